# Optimizing a Trainium2 kernel written in Bass

```python
import math
import jax, jax.numpy as jnp
from jax import lax
import numpy as np

D_MODEL = 2048
BATCH = 4
SEQ = 4096
DEPTH = 4

CHUNK = 64
EPS = 1e-6

S5_GROUPS = 32
S5_GROUP_DIM = 16
S5_WIDTH = S5_GROUPS * S5_GROUP_DIM
S5_STATE = 64
S5_DT_MIN = 1e-3
S5_DT_MAX = 1e-1

HG_HEADS = 6
HG_DK = 128
HG_DV = 128
HG_KEY_WIDTH = HG_HEADS * HG_DK
HG_WIDTH = HG_HEADS * HG_DV

ML_HEADS = 4
ML_DH = 192
ML_WIDTH = ML_HEADS * ML_DH
ML_CONV = 4

MIX_WIDTH = S5_WIDTH + HG_WIDTH + ML_WIDTH
N_BRANCH = 3

FFN_DIM = 5632
FFN_CONV = 3

IN_WIDTHS = (S5_WIDTH,
             HG_KEY_WIDTH, HG_KEY_WIDTH,
             HG_WIDTH, HG_WIDTH,
             ML_WIDTH, ML_WIDTH, ML_WIDTH,
             ML_HEADS, ML_HEADS,
             N_BRANCH * D_MODEL)
IN_TOTAL = 12040

kernel_name = "hybrid_s5_hgrn2_mlstm_convffn"


def _split_points():
    pts, acc = [], 0
    for w in IN_WIDTHS[:-1]:
        acc += w
        pts.append(acc)
    return pts


def rms_norm(x, g):
    xf = x.astype(jnp.float32)
    y = xf * lax.rsqrt(jnp.mean(xf * xf, axis=-1, keepdims=True) + EPS)
    return (y * g.astype(jnp.float32)).astype(x.dtype)


def causal_dwconv(x, w, b):
    k, c = w.shape[0], x.shape[-1]
    y = lax.conv_general_dilated(x, w[:, None, :].astype(x.dtype), window_strides=(1,),
                                 padding=((k - 1, 0),), dimension_numbers=('NWC', 'WIO', 'NWC'),
                                 feature_group_count=c)
    return y + b.astype(x.dtype)


def to_chunks(t, n_heads, d):
    bsz, seq = t.shape[0], t.shape[1]
    return t.reshape(bsz, seq // CHUNK, CHUNK, n_heads, d).transpose(1, 0, 3, 2, 4)


def from_chunks(t):
    nc, bsz, h, l, d = t.shape
    return t.transpose(1, 0, 3, 2, 4).reshape(bsz, nc * l, h, d)


def s5_mixer(u, lam_re, lam_im, log_dt, b_re, b_im, c_re, c_im, d_skip, w_glu, b_glu):
    bsz, seq, _ = u.shape
    f32 = jnp.float32
    uf = u.astype(f32).reshape(bsz, seq, S5_GROUPS, S5_GROUP_DIM)
    dt = jnp.exp(log_dt.astype(f32))[:, None]
    lr, li = lam_re.astype(f32), lam_im.astype(f32)
    mag = jnp.exp(lr * dt)
    ar, ai = mag * jnp.cos(li * dt), mag * jnp.sin(li * dt)
    den = lr * lr + li * li
    cr = ((ar - 1.0) * lr + ai * li) / den
    ci = (ai * lr - (ar - 1.0) * li) / den
    br_, bi_ = b_re.astype(f32), b_im.astype(f32)
    bbr = cr[..., None] * br_ - ci[..., None] * bi_
    bbi = cr[..., None] * bi_ + ci[..., None] * br_
    xr = jnp.einsum('bsgp,gnp->bsgn', uf, bbr)
    xi = jnp.einsum('bsgp,gnp->bsgn', uf, bbi)
    ar_f = jnp.broadcast_to(ar, xr.shape)
    ai_f = jnp.broadcast_to(ai, xr.shape)

    def combine(e1, e2):
        a1r, a1i, x1r, x1i = e1
        a2r, a2i, x2r, x2i = e2
        return (a2r * a1r - a2i * a1i, a2r * a1i + a2i * a1r,
                a2r * x1r - a2i * x1i + x2r, a2r * x1i + a2i * x1r + x2i)

    _, _, sr, si = lax.associative_scan(combine, (ar_f, ai_f, xr, xi), axis=1)
    y = (jnp.einsum('bsgn,gpn->bsgp', sr, c_re.astype(f32))
         - jnp.einsum('bsgn,gpn->bsgp', si, c_im.astype(f32))
         + d_skip.astype(f32).reshape(S5_GROUPS, S5_GROUP_DIM) * uf)
    y = jax.nn.gelu(y.reshape(bsz, seq, S5_WIDTH))
    y = y * jax.nn.sigmoid(y @ w_glu.astype(f32) + b_glu.astype(f32))
    return y.astype(u.dtype)


def hgrn2_mixer(q_in, f_in, i_in, g_in, lb, norm_g):
    bsz, seq, _ = q_in.shape
    f32 = jnp.float32
    q = jax.nn.silu(q_in.astype(f32))
    lbf = lb.astype(f32)
    f = lbf + (1.0 - lbf) * jax.nn.sigmoid(f_in.astype(f32))
    logf = jnp.log(f)
    k = 1.0 - f
    v = i_in.astype(f32)
    qc, kc = to_chunks(q, HG_HEADS, HG_DK), to_chunks(k, HG_HEADS, HG_DK)
    vc, gc = to_chunks(v, HG_HEADS, HG_DV), to_chunks(logf, HG_HEADS, HG_DK)
    causal = jnp.tril(jnp.ones((CHUNK, CHUNK), dtype=bool))

    def step(state, xs):
        qb, kb, vb, gb = xs
        cum = jnp.cumsum(gb, axis=2)
        last = cum[:, :, -1:, :]
        inter = jnp.einsum('bhtk,bhkv->bhtv', qb * jnp.exp(cum), state)
        diff = cum[:, :, :, None, :] - cum[:, :, None, :, :]
        decay = jnp.exp(jnp.where(causal[:, :, None], diff, -jnp.inf))
        scores = jnp.einsum('bhtk,bhsk,bhtsk->bhts', qb, kb, decay)
        out = inter + jnp.einsum('bhts,bhsv->bhtv', scores, vb)
        new_state = (jnp.exp(last[:, :, 0, :])[..., None] * state
                     + jnp.einsum('bhsk,bhsv->bhkv', kb * jnp.exp(last - cum), vb))
        return new_state, out

    s0 = jnp.zeros((bsz, HG_HEADS, HG_DK, HG_DV), f32)
    _, o = lax.scan(step, s0, (qc, kc, vc, gc))
    o = rms_norm(from_chunks(o), norm_g.reshape(HG_HEADS, HG_DV))
    o = o.reshape(bsz, seq, HG_WIDTH) * jax.nn.silu(g_in.astype(f32))
    return o.astype(q_in.dtype)


def mlstm_mixer(cx, v_in, o_in, ig_in, fg_in, conv_w, conv_b, w_qk, b_ig, b_fg, norm_g):
    bsz, seq, _ = cx.shape
    f32 = jnp.float32
    ca = jax.nn.silu(causal_dwconv(cx, conv_w, conv_b)).reshape(bsz, seq, ML_HEADS, ML_DH)
    qk = jnp.einsum('bshd,hde->bshe', ca, w_qk.astype(ca.dtype)).astype(f32)
    q = qk[..., :ML_DH].reshape(bsz, seq, ML_WIDTH)
    k = (qk[..., ML_DH:] * (ML_DH ** -0.5)).reshape(bsz, seq, ML_WIDTH)
    v = v_in.astype(f32)
    ig = (ig_in + b_ig).astype(f32)
    lf = jax.nn.log_sigmoid((fg_in + b_fg).astype(f32))
    qc, kc, vc = to_chunks(q, ML_HEADS, ML_DH), to_chunks(k, ML_HEADS, ML_DH), to_chunks(v, ML_HEADS, ML_DH)
    nch = seq // CHUNK
    icc = ig.reshape(bsz, nch, CHUNK, ML_HEADS).transpose(1, 0, 3, 2)
    fcc = lf.reshape(bsz, nch, CHUNK, ML_HEADS).transpose(1, 0, 3, 2)
    causal = jnp.tril(jnp.ones((CHUNK, CHUNK), dtype=bool))

    def step(carry, xs):
        cmat, nvec, m = carry
        qb, kb, vb, ib, fb = xs
        bcum = jnp.cumsum(fb, axis=-1)
        dmat = jnp.where(causal, bcum[..., :, None] - bcum[..., None, :] + ib[..., None, :], -jnp.inf)
        inter_log = bcum + m[..., None]
        m_t = jnp.maximum(inter_log, jnp.max(dmat, axis=-1))
        w_inter = jnp.exp(inter_log - m_t)
        w_intra = jnp.exp(dmat - m_t[..., None]) * jnp.einsum('bhtd,bhsd->bhts', qb, kb)
        num = (w_inter[..., None] * jnp.einsum('bhtk,bhkv->bhtv', qb, cmat)
               + jnp.einsum('bhts,bhsv->bhtv', w_intra, vb))
        den = w_inter * jnp.einsum('bhtk,bhk->bht', qb, nvec) + jnp.sum(w_intra, axis=-1)
        h = num / jnp.maximum(jnp.abs(den), jnp.exp(-m_t))[..., None]
        m_new = m_t[..., -1]
        decay = jnp.exp(bcum[..., -1] + m - m_new)
        ws = jnp.exp(bcum[..., -1:] - bcum + ib - m_new[..., None])
        c_new = decay[..., None, None] * cmat + jnp.einsum('bhs,bhsk,bhsv->bhkv', ws, kb, vb)
        n_new = decay[..., None] * nvec + jnp.einsum('bhs,bhsk->bhk', ws, kb)
        return (c_new, n_new, m_new), h

    init = (jnp.zeros((bsz, ML_HEADS, ML_DH, ML_DH), f32),
            jnp.zeros((bsz, ML_HEADS, ML_DH), f32),
            jnp.zeros((bsz, ML_HEADS), f32))
    _, h = lax.scan(step, init, (qc, kc, vc, icc, fcc))
    h = rms_norm(from_chunks(h), norm_g.reshape(ML_HEADS, ML_DH))
    h = h.reshape(bsz, seq, ML_WIDTH) * jax.nn.sigmoid(o_in.astype(f32))
    return h.astype(cx.dtype)


def conv_ffn(h, w_up, conv_w, conv_b, w_down):
    up = causal_dwconv(h @ w_up, conv_w, conv_b)
    a, b = up[..., :FFN_DIM], up[..., FFN_DIM:]
    return (jax.nn.silu(a) * b) @ w_down


def setup_inputs(seed: int = 0) -> dict:
    key = jax.random.key(seed)
    ks = jax.random.split(key, 32)
    f32 = jnp.float32
    L, G, N, P = DEPTH, S5_GROUPS, S5_STATE, S5_GROUP_DIM

    def nrm(k, shape, scale):
        return scale * jax.random.normal(k, shape, f32)

    n_idx = jnp.arange(N, dtype=f32)
    return {
        'x': nrm(ks[0], (BATCH, SEQ, D_MODEL), 1.0),
        'mix_norm': 1.0 + nrm(ks[1], (L, D_MODEL), 0.02),
        'w_in': nrm(ks[2], (L, D_MODEL, IN_TOTAL), D_MODEL ** -0.5),
        's5_lam_re': -0.5 + nrm(ks[3], (L, G, N), 0.01),
        's5_lam_im': math.pi * n_idx + nrm(ks[4], (L, G, N), 0.01),
        's5_log_dt': jax.random.uniform(ks[5], (L, G), f32, math.log(S5_DT_MIN), math.log(S5_DT_MAX)),
        's5_b_re': nrm(ks[6], (L, G, N, P), (2 * P) ** -0.5),
        's5_b_im': nrm(ks[7], (L, G, N, P), (2 * P) ** -0.5),
        's5_c_re': nrm(ks[8], (L, G, P, N), N ** -0.5),
        's5_c_im': nrm(ks[9], (L, G, P, N), N ** -0.5),
        's5_d': nrm(ks[10], (L, S5_WIDTH), 1.0),
        's5_w_glu': nrm(ks[11], (L, S5_WIDTH, S5_WIDTH), S5_WIDTH ** -0.5),
        's5_b_glu': nrm(ks[12], (L, S5_WIDTH), 0.02),
        'hg_lower_bounds': nrm(ks[13], (L, HG_KEY_WIDTH), 1.0),
        'hg_norm': 1.0 + nrm(ks[14], (L, HG_WIDTH), 0.02),
        'ml_conv_w': nrm(ks[15], (L, ML_CONV, ML_WIDTH), 0.5),
        'ml_conv_b': nrm(ks[16], (L, ML_WIDTH), 0.02),
        'ml_w_qk': nrm(ks[17], (L, ML_HEADS, ML_DH, 2 * ML_DH), ML_DH ** -0.5),
        'ml_b_ig': nrm(ks[18], (L, ML_HEADS), 0.1),
        'ml_b_fg': jnp.linspace(3.0, 6.0, ML_HEADS, dtype=f32)[None, :] + nrm(ks[19], (L, ML_HEADS), 0.01),
        'ml_norm': 1.0 + nrm(ks[20], (L, ML_WIDTH), 0.02),
        'w_branch': nrm(ks[21], (L, MIX_WIDTH, D_MODEL), (MIX_WIDTH / N_BRANCH) ** -0.5),
        'w_out': nrm(ks[22], (L, D_MODEL, D_MODEL), D_MODEL ** -0.5),
        'ffn_norm': 1.0 + nrm(ks[23], (L, D_MODEL), 0.02),
        'ffn_w_up': nrm(ks[24], (L, D_MODEL, 2 * FFN_DIM), D_MODEL ** -0.5),
        'ffn_conv_w': nrm(ks[25], (L, FFN_CONV, 2 * FFN_DIM), FFN_CONV ** -0.5),
        'ffn_conv_b': nrm(ks[26], (L, 2 * FFN_DIM), 0.02),
        'ffn_w_down': nrm(ks[27], (L, FFN_DIM, D_MODEL), FFN_DIM ** -0.5),
        'final_norm': 1.0 + nrm(ks[28], (D_MODEL,), 0.02),
    }


def reference(x, mix_norm, w_in, s5_lam_re, s5_lam_im, s5_log_dt, s5_b_re, s5_b_im, s5_c_re, s5_c_im,
              s5_d, s5_w_glu, s5_b_glu, hg_lower_bounds, hg_norm, ml_conv_w, ml_conv_b, ml_w_qk,
              ml_b_ig, ml_b_fg, ml_norm, w_branch, w_out, ffn_norm, ffn_w_up, ffn_conv_w, ffn_conv_b,
              ffn_w_down, final_norm):
    lbs = jax.nn.softmax(hg_lower_bounds.astype(jnp.float32), axis=0)
    lbs = jnp.cumsum(lbs, axis=0) - lbs[0:1]
    splits = _split_points()
    r_a, r_b = S5_WIDTH, S5_WIDTH + HG_WIDTH
    for l in range(DEPTH):
        h = rms_norm(x, mix_norm[l])
        (u_a, q_b, f_b, i_b, og_b, cx_c, v_c, o_c, ig_c, fg_c, gates) = jnp.split(h @ w_in[l], splits, axis=-1)
        y_a = s5_mixer(u_a, s5_lam_re[l], s5_lam_im[l], s5_log_dt[l], s5_b_re[l], s5_b_im[l],
                       s5_c_re[l], s5_c_im[l], s5_d[l], s5_w_glu[l], s5_b_glu[l])
        y_b = hgrn2_mixer(q_b, f_b, i_b, og_b, lbs[l], hg_norm[l])
        y_c = mlstm_mixer(cx_c, v_c, o_c, ig_c, fg_c, ml_conv_w[l], ml_conv_b[l], ml_w_qk[l],
                          ml_b_ig[l], ml_b_fg[l], ml_norm[l])
        wb = w_branch[l]
        g_a, g_b, g_c = (gates[..., :D_MODEL], gates[..., D_MODEL:2 * D_MODEL], gates[..., 2 * D_MODEL:])
        merged = (jax.nn.sigmoid(g_a) * (y_a @ wb[:r_a])
                  + jax.nn.sigmoid(g_b) * (y_b @ wb[r_a:r_b])
                  + jax.nn.sigmoid(g_c) * (y_c @ wb[r_b:]))
        x = x + merged @ w_out[l]
        x = x + conv_ffn(rms_norm(x, ffn_norm[l]), ffn_w_up[l], ffn_conv_w[l], ffn_conv_b[l], ffn_w_down[l])
    return rms_norm(x, final_norm)
```

```python
import numpy as np
from contextlib import ExitStack
import concourse.bass as bass
import concourse.mybir as mybir

F32 = mybir.dt.float32
F32R = mybir.dt.float32r
BF = mybir.dt.bfloat16
ALU = mybir.AluOpType
AF = mybir.ActivationFunctionType
AX = mybir.AxisListType

ENGS = ("pe", "act", "dve", "pool", "sp")
NDSEM = 24


class Op:
    __slots__ = ("eng", "fn", "waits", "done", "dma", "idx")


class Prog:
    def __init__(self, nc, es):
        self.nc = nc
        self.ops = {e: [] for e in ENGS}
        self.esem = {e: es.enter_context(nc.semaphore("sem_" + e)) for e in ENGS}
        self.ecnt = {e: 0 for e in ENGS}
        self.dsem = {q: [es.enter_context(nc.semaphore("dq_%s_%d" % (q, i))) for i in range(NDSEM)]
                     for q in ("sp", "pool", "act")}
        self.dcnt = {q: [0] * NDSEM for q in ("sp", "pool", "act")}
        self.dnext = {q: 0 for q in ("sp", "pool", "act")}
        self.waited = {e: {} for e in ENGS}
        self.lastw = {}
        self.readers = {}
        self.pending = {e: [] for e in ENGS}
        self.nops = 0

    def _need(self, eng, waits, dep):
        sem, val, deng, ddma = dep
        if (not ddma) and deng == eng and eng == "pe":
            return
        key = id(sem)
        if self.waited[eng].get(key, 0) >= val:
            return
        waits[key] = (sem, max(val, waits.get(key, (sem, 0))[1]))

    def add(self, eng, fn, reads=(), writes=(), dma=False):
        op = Op()
        op.eng = eng
        op.fn = fn
        op.dma = dma
        waits = {}
        for k in reads:
            w = self.lastw.get(k)
            if w is not None:
                self._need(eng, waits, w)
        for k in writes:
            w = self.lastw.get(k)
            if w is not None:
                self._need(eng, waits, w)
            for r in self.readers.get(k, ()):
                self._need(eng, waits, r)
        for dep in self.pending[eng]:
            self._need(eng, waits, dep)
        self.pending[eng] = []
        if dma:
            q = eng
            i = self.dnext[q]
            self.dnext[q] = (i + 1) % NDSEM
            sem = self.dsem[q][i]
            if self.dcnt[q][i] > 0:
                self._need(eng, waits, (sem, self.dcnt[q][i], eng, True))
            self.dcnt[q][i] += 16
            op.done = (sem, self.dcnt[q][i], eng, True)
        else:
            self.ecnt[eng] += 1
            op.done = (self.esem[eng], self.ecnt[eng], eng, False)
        op.waits = list(waits.values())
        for sem, val in op.waits:
            self.waited[eng][id(sem)] = val
        for k in reads:
            self.readers.setdefault(k, []).append(op.done)
        for k in writes:
            self.lastw[k] = op.done
            self.readers[k] = []
        self.ops[eng].append(op)
        self.nops += 1
        return op

    def barrier(self):
        deps = []
        for e in ENGS:
            if self.ecnt[e] > 0:
                deps.append((self.esem[e], self.ecnt[e], e, True))
        for q in self.dsem:
            for i in range(NDSEM):
                if self.dcnt[q][i] > 0:
                    deps.append((self.dsem[q][i], self.dcnt[q][i], q, True))
        for e in ENGS:
            self.pending[e] = list(deps)
        self.lastw = {}
        self.readers = {}

    def emit(self, final_waits_eng="sp"):
        nc = self.nc
        self.barrier()
        fin = self.pending[final_waits_eng]
        with nc.Block() as block:
            def run(e, eng):
                for op in self.ops[e]:
                    for sem, val in op.waits:
                        eng.wait_ge(sem, val)
                    ins = op.fn(eng)
                    sem, val, _, ddma = op.done
                    ins.then_inc(sem, 16 if ddma else 1)
                if e == final_waits_eng:
                    w = {}
                    for sem, val, _, _ in fin:
                        if self.waited[e].get(id(sem), 0) < val:
                            w[id(sem)] = (sem, max(val, w.get(id(sem), (sem, 0))[1]))
                    for sem, val in w.values():
                        eng.wait_ge(sem, val)

            @block.tensor
            def _(eng):
                run("pe", eng)

            @block.scalar
            def _(eng):
                run("act", eng)

            @block.vector
            def _(eng):
                run("dve", eng)

            @block.gpsimd
            def _(eng):
                run("pool", eng)

            @block.sync
            def _(eng):
                run("sp", eng)

    def mm(self, out, lhsT, rhs, start, stop, r, w):
        return self.add("pe", lambda e: e.matmul(out, lhsT, rhs, start=start, stop=stop), r, w)

    def tr(self, out, in_, ident, r, w):
        return self.add("pe", lambda e: e.transpose(out, in_, ident), r, w)

    def actf(self, out, in_, func, r, w, bias=None, scale=None):
        kw = {}
        if bias is not None:
            kw["bias"] = bias
        if scale is not None:
            kw["scale"] = scale
        return self.add("act", lambda e: e.activation(out, in_, func, **kw), r, w)

    def tt(self, out, a, b, op, r, w, eng="dve"):
        return self.add(eng, lambda e: e.tensor_tensor(out, a, b, op), r, w)

    def ts(self, out, a, s1, s2, op0, op1, r, w, eng="dve"):
        if op1 is None:
            return self.add(eng, lambda e: e.tensor_scalar(out, a, s1, None, op0), r, w)
        return self.add(eng, lambda e: e.tensor_scalar(out, a, s1, s2, op0, op1), r, w)

    def stt(self, out, a, s, b, op0, op1, r, w, eng="dve"):
        return self.add(eng, lambda e: e.scalar_tensor_tensor(out, a, s, b, op0, op1), r, w)

    def cp(self, out, a, r, w, eng="dve"):
        if eng == "act":
            return self.add("act", lambda e: e.copy(out, a), r, w)
        return self.add(eng, lambda e: e.tensor_copy(out, a), r, w)

    def scan(self, out, d0, d1, init, op0, op1, r, w):
        return self.add("dve", lambda e: e.tensor_tensor_scan(out, d0, d1, init, op0, op1), r, w)

    def memset(self, ap, val, w, eng="dve"):
        return self.add(eng, lambda e: e.memset(ap, val), (), w)

    def dma(self, q, out, in_, r, w):
        return self.add(q, lambda e: e.dma_start(out=out, in_=in_), r, w, dma=True)


import math
from concourse.bass_utils import run_bass_kernel_spmd

T = 512
D = 2048
NCH = 16
EPS = 1e-6
MAGIC = 12582912.0
TWO_PI = 2.0 * math.pi
GELU_C = math.sqrt(2.0 / math.pi)
FFN = 5632
IN_TOTAL = 12040
C_IDENT, C_ONES, C_M128, C_M64, C_TT, C_MG, C_EPS, C_ZERO, C_SEL = 0, 128, 256, 384, 448, 960, 962, 963, 964
NCST = 964 + 384
RW = 768
NRING = 8
RCOLS = 26624


def make_consts():
    c = np.zeros((128, NCST), np.float32)
    c[:, C_IDENT:C_IDENT + 128] = np.eye(128)
    c[:, C_ONES:C_ONES + 128] = 1.0
    c[:, C_M128:C_M128 + 128] = np.triu(np.ones((128, 128)))
    c[:64, C_M64:C_M64 + 64] = np.triu(np.ones((64, 64)))
    c[:, C_TT:C_TT + 512] = np.arange(1, 513)[None, :]
    c[:64, C_MG] = 1.0
    c[64:, C_MG + 1] = 1.0
    c[:, C_EPS] = EPS
    for h in range(4):
        c[h, C_SEL + h * 96:C_SEL + (h + 1) * 96] = 1.0
    return c


def build(NL, NT):
    nc = bass.Bass("TRN2", target_bir_lowering=False)
    es = ExitStack()

    def din(name, shape):
        return nc.dram_tensor(name, list(shape), F32, kind="ExternalInput").ap()

    x_d = din("x", [NT * T, D])
    cst_d = din("cst", [128, NCST])
    mix_norm = din("mix_norm", [NL, D]); w_in = din("w_in", [NL, D, IN_TOTAL])
    lam_re = din("s5_lam_re", [NL, 32, 64]); lam_im = din("s5_lam_im", [NL, 32, 64]); log_dt = din("s5_log_dt", [NL, 32])
    b_re = din("s5_b_re", [NL, 32, 64, 16]); b_im = din("s5_b_im", [NL, 32, 64, 16])
    c_re = din("s5_c_re", [NL, 32, 16, 64]); c_im = din("s5_c_im", [NL, 32, 16, 64])
    s5_d = din("s5_d", [NL, 512]); w_glu = din("s5_w_glu", [NL, 512, 512]); b_glu = din("s5_b_glu", [NL, 512])
    hg_lb = din("hg_lower_bounds", [NL, 768]); hg_norm = din("hg_norm", [NL, 768])
    ml_cw = din("ml_conv_w", [NL, 4, 768]); ml_cb = din("ml_conv_b", [NL, 768]); w_qk = din("ml_w_qk", [NL, 4, 192, 384])
    b_ig = din("ml_b_ig", [NL, 4]); b_fg = din("ml_b_fg", [NL, 4]); ml_norm = din("ml_norm", [NL, 768])
    w_br = din("w_branch", [NL, D, D]); w_out = din("w_out", [NL, D, D]); ffn_norm = din("ffn_norm", [NL, D])
    w_up = din("ffn_w_up", [NL, D, 2 * FFN]); f_cw = din("ffn_conv_w", [NL, 3, 2 * FFN]); f_cb = din("ffn_conv_b", [NL, 2 * FFN])
    w_dn = din("ffn_w_down", [NL, FFN, D]); fin_norm = din("final_norm", [D])
    y_d = nc.dram_tensor("y", [NT * T, D], F32, kind="ExternalOutput").ap()
    xs_d = nc.dram_tensor("xs_scr", [NT, 128, NCH * T], F32, kind="Internal").ap()
    lbfc_d = nc.dram_tensor("lbfc_scr", [NL, 16, 128, 512], F32, kind="Internal").ap()

    with es:
        P = Prog(nc, es)
        sbt = lambda n, s, d=F32: es.enter_context(nc.sbuf_tensor(n + "_sb", s, d))
        cst = sbt("cst", [128, NCST])
        xt = sbt("xt", [128, NCH, T])
        ht = sbt("ht", [128, NCH, T], BF)
        htr = ht[:]
        ring = sbt("ring", [128, NRING, RW], BF)
        R = sbt("R", [128, RCOLS])
        vp = sbt("vp", [128, 420])
        vq = sbt("vq", [96, 48])
        lbs = sbt("lbs", [128, 4 * 6 * 2])
        s5st = sbt("s5st", [128, 2, 16])
        s5p = sbt("s5p", [128, 2, 16])
        hst = sbt("hst", [128, 6, 128])
        mlC = sbt("mlC", [96, 4, 2, 192])
        mlN = sbt("mlN", [96, 4, 2, 96])
        mlt = sbt("mlt", [96, 8, 3])
        ftl = sbt("ftl", [128, 88, 2])
        rc = sbt("rc", [4, 4])
        ps = [es.enter_context(nc.psum_tensor("psb%d" % i, [128, 512], F32)) for i in range(8)]
        PK = lambda b: "ps%d" % b

        ident = cst[:, C_IDENT:C_IDENT + 128]
        ones = cst[:, C_ONES:C_ONES + 128]
        m128 = cst[:, C_M128:C_M128 + 128]
        m64 = cst[0:64, C_M64:C_M64 + 64]
        tt_i = cst[:, C_TT:C_TT + 512]
        epsc = cst[:, C_EPS:C_EPS + 1]

        def sel4(h):
            return cst[0:4, C_SEL + 96 * h:C_SEL + 96 * (h + 1)]

        class Carve:
            def __init__(self, base=0):
                self.o = base

            def t2(self, n, parts=128):
                a = R[0:parts, self.o:self.o + n]
                self.o += n
                assert self.o <= RCOLS, self.o
                return a

            def t3(self, a, b, parts=128):
                return self.t2(a * b, parts).rearrange("p (a b) -> p a b", b=b)

            def h2(self, n, parts=128):
                return self.t2((n + 1) // 2, parts).bitcast(BF)[:, 0:n]

            def h3(self, a, b, parts=128):
                return self.t2(a * b // 2, parts).bitcast(BF).rearrange("p (a b) -> p a b", b=b)

        P.dma("sp", cst[:], cst_d, (), ["cst"])
        ring_i = [0]

        def proj(wsrc, kchunks, pieces, tiles, consume, banks=None, ringB=None):
            banks = banks or list(range(len(tiles)))
            nk = len(kchunks)
            for ki, (r0, nr, rhs, rkey) in enumerate(kchunks):
                slots = []
                for (c0, wd) in pieces:
                    s = ring_i[0] % NRING
                    ring_i[0] += 1
                    P.dma("pool", ring[0:nr, s, 0:wd], wsrc[r0:r0 + nr, c0:c0 + wd], (), ["ring%d" % s])
                    slots.append(s)
                for ti, (pi, off, M) in enumerate(tiles):
                    s = slots[pi]
                    P.mm(ps[banks[ti]][0:M, :], ring[0:nr, s, off:off + M], rhs, ki == 0, ki == nk - 1,
                         ["ring%d" % s, rkey], [PK(banks[ti])])
            for ti, (pi, off, M) in enumerate(tiles):
                consume(ti, ps[banks[ti]][0:M, :], PK(banks[ti]))

        def hk(keyprefix="ht"):
            return [(128 * c, 128, htr[:, c, :], "ht") for c in range(NCH)]

        def emit_sin(out, x, tk, kx, kt, kout):
            P.ts(tk, x, 1.0 / TWO_PI, MAGIC, ALU.mult, ALU.add, [kx], [kt])
            P.ts(tk, tk, -MAGIC, None, ALU.add, None, [kt], [kt])
            P.stt(x, tk, -TWO_PI, x, ALU.mult, ALU.add, [kt, kx], [kx])
            P.ts(tk, x, math.pi, TWO_PI, ALU.is_gt, ALU.mult, [kx], [kt])
            P.tt(x, x, tk, ALU.subtract, [kx, kt], [kx])
            P.ts(x, x, math.pi, -math.pi, ALU.min, ALU.max, [kx], [kx])
            P.actf(out, x, AF.Sin, [kx], [kout])

        def load_T(dst, src, nr, w, stage, kst, bank=7):
            P.dma("sp", stage[0:nr, 0:w], src, (), [kst])
            P.tr(ps[bank][0:w, 0:nr], stage[0:nr, 0:w], ident[0:nr, 0:nr], [kst, "cst"], [PK(bank)])
            P.cp(dst, ps[bank][0:w, 0:nr], [PK(bank)], ["vp"])

        def rmsnorm(dst, gcol, sq2, rs, kdst):
            for c in range(NCH):
                sq = sq2[:, c % 2, :]
                P.actf(sq, xt[:, c, :], AF.Square, ["xt%d" % c], ["sq%d" % (c % 2)])
                P.mm(ps[7][:, :], ones, sq, c == 0, c == NCH - 1, ["sq%d" % (c % 2), "cst"], [PK(7)])
            P.actf(rs, ps[7][:, :], AF.Sqrt, [PK(7)], ["rs"], bias=epsc, scale=1.0 / D)
            P.add("dve", lambda e: e.reciprocal(rs, rs), ["rs"], ["rs"])
            for c in range(NCH):
                P.stt(dst[:, c, :], xt[:, c, :], gcol(c), rs, ALU.mult, ALU.mult, ["xt%d" % c, "rs", "vp"], [kdst])

        cv = Carve()
        stg = cv.t2(128)
        lraw = cv.t2(NL * 6)
        for l in range(NL):
            load_T(lraw[:, l * 6:(l + 1) * 6], hg_lb[l].rearrange("(c p) -> c p", p=128), 6, 128, stg, "stg")
        ex = cv.t2(NL * 6)
        tot = cv.t2(6)
        P.actf(ex, lraw, AF.Exp, ["vp"], ["ex"])
        P.cp(tot, ex[:, 0:6], ["ex"], ["tot"])
        for l in range(1, NL):
            P.tt(tot, tot, ex[:, l * 6:(l + 1) * 6], ALU.add, ["tot", "ex"], ["tot"])
        P.add("dve", lambda e: e.reciprocal(tot, tot), ["tot"], ["tot"])
        P.memset(lbs[:, 0:6], 0.0, ["lbs"])
        for l in range(1, NL):
            if l == 1:
                P.cp(lbs[:, 6:12], ex[:, 6:12], ["ex"], ["lbs"])
            else:
                P.tt(lbs[:, l * 6:(l + 1) * 6], lbs[:, (l - 1) * 6:l * 6], ex[:, l * 6:(l + 1) * 6], ALU.add, ["lbs", "ex"], ["lbs"])
        for l in range(NL):
            if l > 0:
                P.tt(lbs[:, l * 6:(l + 1) * 6], lbs[:, l * 6:(l + 1) * 6], tot, ALU.mult, ["lbs", "tot"], ["lbs"])
        for l in range(NL):
            P.ts(lbs[:, 24 + l * 6:24 + (l + 1) * 6], lbs[:, l * 6:(l + 1) * 6], -1.0, 1.0, ALU.mult, ALU.add, ["lbs"], ["lbs"])
        P.barrier()

        for l in range(NL):
            cv = Carve()
            stg = cv.t2(128)
            load_T(vp[:, 0:16], mix_norm[l].rearrange("(c p) -> c p", p=128), 16, 128, stg, "stg")
            load_T(vp[:, 16:32], ffn_norm[l].rearrange("(c p) -> c p", p=128), 16, 128, stg, "stg")
            load_T(vp[:, 32:36], s5_d[l].rearrange("(c p) -> c p", p=128), 4, 128, stg, "stg")
            load_T(vp[:, 36:40], b_glu[l].rearrange("(c p) -> c p", p=128), 4, 128, stg, "stg")
            load_T(vp[:, 40:46], hg_norm[l].rearrange("(c p) -> c p", p=128), 6, 128, stg, "stg")
            load_T(vp[:, 46:134], f_cb[l].rearrange("(c p) -> c p", p=128), 88, 128, stg, "stg")
            for k in range(3):
                load_T(vp[:, 134 + 88 * k:134 + 88 * (k + 1)], f_cw[l, k].rearrange("(c p) -> c p", p=128), 88, 128, stg, "stg")
            load_T(vp[:, 398:414], fin_norm.rearrange("(c p) -> c p", p=128), 16, 128, stg, "stg")
            for k in range(4):
                load_T(vq[:, 8 * k:8 * (k + 1)], ml_cw[l, k].rearrange("(c p) -> c p", p=96), 8, 96, stg, "stg")
            load_T(vq[:, 32:40], ml_cb[l].rearrange("(c p) -> c p", p=96), 8, 96, stg, "stg")
            load_T(vq[:, 40:48], ml_norm[l].rearrange("(c p) -> c p", p=96), 8, 96, stg, "stg")
            P.dma("sp", rc[:, 1:2], b_ig[l].rearrange("(h o) -> h o", o=1), (), ["rc"])
            P.dma("sp", rc[:, 2:3], b_fg[l].rearrange("(h o) -> h o", o=1), (), ["rc"])
            P.memset(s5st[:], 0.0, ["s5st"]); P.memset(hst[:], 0.0, ["hst"]); P.memset(mlC[:], 0.0, ["mlC"])
            P.memset(mlN[:], 0.0, ["mlN"]); P.memset(mlt[:], 0.0, ["mlt"]); P.memset(ftl[:], 0.0, ["ftl"])
            P.memset(rc[:, 0:1], 0.0, ["rc"])

            L16 = cv.t3(3, 128, parts=16)
            LD = cv.t2(2, parts=16)
            P.dma("sp", L16[:, 0, :], lam_re[l].rearrange("(s g) n -> s (g n)", g=2), (), ["L16"])
            P.dma("sp", L16[:, 1, :], lam_im[l].rearrange("(s g) n -> s (g n)", g=2), (), ["L16"])
            P.dma("sp", LD, log_dt[l].rearrange("(s g) -> s g", g=2), (), ["LD"])
            P.cp(L16[:, 2, :].rearrange("p (g n) -> p g n", n=64), LD.unsqueeze(2).to_broadcast([16, 2, 64]), ["LD"], ["L16"])
            sp = cv.t3(12, 16)
            for i in range(3):
                P.tr(ps[7][:, 0:16], L16[:, i, :], ident[0:16, 0:16], ["L16", "cst"], [PK(7)])
                P.cp(sp[:, i, :], ps[7][:, 0:16], [PK(7)], ["sp"])
            lr, li, dt_ = sp[:, 0, :], sp[:, 1, :], sp[:, 2, :]
            tmpa, tmpb = sp[:, 3, :], sp[:, 4, :]
            cosv, sinv, ar, ai, den, cr, ci = (sp[:, i, :] for i in range(5, 12))
            P.actf(dt_, dt_, AF.Exp, ["sp"], ["sp"])
            P.tt(tmpa, lr, dt_, ALU.mult, ["sp"], ["sp"])
            P.actf(s5p[:, 0, :], tmpa, AF.Exp, ["sp"], ["s5p"])
            P.tt(s5p[:, 1, :], li, dt_, ALU.mult, ["sp"], ["s5p"])
            P.cp(tmpa, s5p[:, 1, :], ["s5p"], ["sp"])
            emit_sin(sinv, tmpa, tmpb, "sp", "sp", "sp")
            P.ts(tmpa, s5p[:, 1, :], math.pi / 2, None, ALU.add, None, ["s5p", "sp"], ["sp"])
            emit_sin(cosv, tmpa, tmpb, "sp", "sp", "sp")
            P.tt(ar, s5p[:, 0, :], cosv, ALU.mult, ["sp", "s5p"], ["sp"])
            P.tt(ai, s5p[:, 0, :], sinv, ALU.mult, ["sp", "s5p"], ["sp"])
            P.tt(den, lr, lr, ALU.mult, ["sp"], ["sp"])
            P.tt(tmpa, li, li, ALU.mult, ["sp"], ["sp"])
            P.tt(den, den, tmpa, ALU.add, ["sp"], ["sp"])
            P.add("dve", lambda e: e.reciprocal(den, den), ["sp"], ["sp"])
            P.ts(ar, ar, -1.0, None, ALU.add, None, ["sp"], ["sp"])
            P.tt(cr, ar, lr, ALU.mult, ["sp"], ["sp"])
            P.tt(tmpa, ai, li, ALU.mult, ["sp"], ["sp"])
            P.tt(cr, cr, tmpa, ALU.add, ["sp"], ["sp"])
            P.tt(cr, cr, den, ALU.mult, ["sp"], ["sp"])
            P.tt(ci, ai, lr, ALU.mult, ["sp"], ["sp"])
            P.tt(tmpa, ar, li, ALU.mult, ["sp"], ["sp"])
            P.tt(ci, ci, tmpa, ALU.subtract, ["sp"], ["sp"])
            P.tt(ci, ci, den, ALU.mult, ["sp"], ["sp"])
            BR = cv.t3(16, 16); BI = cv.t3(16, 16); bbr = cv.t3(16, 16); bbi = cv.t3(16, 16); btmp = cv.t3(16, 16)
            P.dma("sp", BR, b_re[l].rearrange("(s g) n p -> (g n) s p", g=2), (), ["BR"])
            P.dma("sp", BI, b_im[l].rearrange("(s g) n p -> (g n) s p", g=2), (), ["BI"])
            crb = cr.unsqueeze(2).to_broadcast([128, 16, 16]); cib = ci.unsqueeze(2).to_broadcast([128, 16, 16])
            P.tt(bbr, BR, crb, ALU.mult, ["BR", "sp"], ["bbr"])
            P.tt(btmp, BI, cib, ALU.mult, ["BI", "sp"], ["btmp"])
            P.tt(bbr, bbr, btmp, ALU.subtract, ["bbr", "btmp"], ["bbr"])
            P.tt(bbi, BI, crb, ALU.mult, ["BI", "sp"], ["bbi"])
            P.tt(btmp, BR, cib, ALU.mult, ["BR", "sp"], ["btmp"])
            P.tt(bbi, bbi, btmp, ALU.add, ["bbi", "btmp"], ["bbi"])
            CI = cv.t3(2 * 16, 128, parts=16)
            cre = cv.t3(16, 16); cim = cv.t3(16, 16)
            P.dma("sp", CI[:, 0:16, :].rearrange("p s (g n) -> p s g n", g=2), c_re[l].rearrange("(s g) p n -> p s g n", g=2), (), ["CI"])
            P.dma("sp", CI[:, 16:32, :].rearrange("p s (g n) -> p s g n", g=2), c_im[l].rearrange("(s g) p n -> p s g n", g=2), (), ["CI"])
            for s in range(32):
                P.tr(ps[6][:, (s % 16) * 16:(s % 16 + 1) * 16], CI[:, s, :], ident[0:16, 0:16], ["CI", "cst"], [PK(6)])
                if s == 15:
                    P.cp(cre.rearrange("p a b -> p (a b)"), ps[6][:, 0:256], [PK(6)], ["cre"])
                if s == 31:
                    P.ts(cim.rearrange("p a b -> p (a b)"), ps[6][:, 0:256], -1.0, None, ALU.mult, None, [PK(6)], ["cim"])
            Fm = cv.t3(16, 128)
            Fst = cv.t3(4, 128)
            for mi, (src, ksrc, needT) in enumerate([(bbr, "bbr", True), (bbi, "bbi", True), (cre, "cre", False), (cim, "cim", False)]):
                P.memset(Fm, 0.0, ["Fm"])
                F4 = Fm.rearrange("p (a m) r -> p a m r", m=4)
                s4 = src.rearrange("p (a m) q -> p a m q", m=4)
                for m in range(4):
                    for gl in range(2):
                        P.ts(F4[:, :, m, 32 * m + 16 * gl:32 * m + 16 * gl + 16], s4[:, :, m, :],
                             cst[:, C_MG + gl:C_MG + gl + 1], None, ALU.mult, None, [ksrc, "cst"], ["Fm"])
                for s in range(16):
                    if needT:
                        P.tr(ps[s % 2][:, 0:128], Fm[:, s, :], ident, ["Fm", "cst"], [PK(s % 2)])
                        P.cp(Fst[:, s % 4, :], ps[s % 2][:, 0:128], [PK(s % 2)], ["Fst%d" % (s % 4)])
                        P.dma("sp", lbfc_d[l, s, :, 128 * mi:128 * (mi + 1)], Fst[:, s % 4, :], ["Fst%d" % (s % 4)], ["lbfc%d" % s])
                    else:
                        P.dma("sp", lbfc_d[l, s, :, 128 * mi:128 * (mi + 1)], Fm[:, s, :], ["Fm"], ["lbfc%d" % s])
            P.barrier()

            for t in range(NT):
                tok0 = t * T
                if l == 0:
                    cv = Carve()
                    xin = cv.t2(D)
                    for tb in range(4):
                        P.dma("sp", xin, x_d[tok0 + tb * 128:tok0 + (tb + 1) * 128, :], (), ["xin"])
                        for c in range(NCH):
                            P.tr(ps[c % 8][:, 0:128], xin[:, c * 128:(c + 1) * 128], ident, ["xin", "cst"], [PK(c % 8)])
                            P.cp(xt[:, c, tb * 128:(tb + 1) * 128], ps[c % 8][:, 0:128], [PK(c % 8)], ["xt%d" % c],
                                 eng=("act" if c % 2 else "dve"))
                else:
                    P.dma("sp", xt[:].rearrange("p a b -> p (a b)"), xs_d[t], ["xs%d" % t], ["xt%d" % c for c in range(NCH)])
                P.barrier()

                cv = Carve()
                merged = cv.t3(NCH, T)
                mergedr = cv.h3(NCH, T)
                Ybr = cv.h3(8, T)
                abase = cv.o
                SG = Carve(abase).t3(6, T)

                ca_ = Carve(abase)
                sq2 = ca_.t3(2, T); rs = ca_.t2(T)
                rmsnorm(htr, lambda c: vp[:, c:c + 1], sq2, rs, "ht")
                P.barrier()

                def branch(kchunks, first, last_br):
                    gcol0 = {0: 5896, 1: 5896 + D, 2: 5896 + 2 * D}[first[0]]
                    row0 = first[1]
                    for g0, ng in ((0, 6), (6, 6), (12, 4)):
                        def cons_g(i, pap, pk):
                            P.actf(SG[:, i, :], pap, AF.Sigmoid, [pk], ["SG%d" % i])
                        proj(w_in[l], hk(), [(gcol0 + 128 * g0, 128 * ng)], [(0, 128 * i, 128) for i in range(ng)], cons_g)

                        def cons_z(i, pap, pk, g0=g0):
                            j = g0 + i
                            if first[0] == 0:
                                P.tt(merged[:, j, :], SG[:, i, :], pap, ALU.mult, ["SG%d" % i, pk], ["mg%d" % j])
                            else:
                                P.tt(SG[:, i, :], SG[:, i, :], pap, ALU.mult, ["SG%d" % i, pk], ["SG%d" % i])
                                dstm = mergedr if last_br else merged
                                P.tt(dstm[:, j, :], merged[:, j, :], SG[:, i, :], ALU.add, ["SG%d" % i, "mg%d" % j],
                                     ["mgr%d" % j if last_br else "mg%d" % j])
                        kc = [(row0 + r0, nr, rhs, rk) for (r0, nr, rhs, rk) in kchunks]
                        proj(w_br[l], kc, [(128 * g0, 128 * ng)], [(0, 128 * i, 128) for i in range(ng)], cons_z)

                ca_ = Carve(abase)
                U = ca_.t3(4, T); G = ca_.t3(4, T); Gr_ = ca_.h3(4, T)
                cs = ca_.t2(T); sn = ca_.t2(T); zr = ca_.t2(T); zi = ca_.t2(T); wr = ca_.t2(T); wi = ca_.t2(T)
                t1 = ca_.t2(T); t2_ = ca_.t2(T)
                LF = ca_.t3(2, 512)

                def cons_u(i, pap, pk):
                    P.cp(U[:, i, :], pap, [pk], ["U"], eng="act")
                proj(w_in[l], hk(), [(0, 512)], [(0, 128 * i, 128) for i in range(4)], cons_u, banks=[4, 5, 6, 7])
                for s in range(16):
                    c = s // 4
                    lf = LF[:, s % 2, :]
                    lfk = "LF%d" % (s % 2)
                    P.dma("sp", lf, lbfc_d[l, s], ["lbfc%d" % s], [lfk])
                    P.ts(t1, tt_i, s5p[:, 1, s:s + 1], None, ALU.mult, None, ["cst", "s5p"], ["t1"])
                    emit_sin(sn, t1, t2_, "t1", "t2", "sn")
                    P.ts(t1, tt_i, s5p[:, 1, s:s + 1], math.pi / 2, ALU.mult, ALU.add, ["cst", "s5p"], ["t1"])
                    emit_sin(cs, t1, t2_, "t1", "t2", "cs")
                    bre, bim = ps[4 + 2 * (s % 2)], ps[5 + 2 * (s % 2)]
                    kre, kim = PK(4 + 2 * (s % 2)), PK(5 + 2 * (s % 2))
                    P.mm(bre[:, :], lf[:, 0:128], U[:, c, :], True, True, [lfk, "U"], [kre])
                    P.mm(bim[:, :], lf[:, 128:256], U[:, c, :], True, True, [lfk, "U"], [kim])
                    P.tt(t1, bre[:, :], cs, ALU.mult, [kre, "cs"], ["t1"])
                    P.tt(t2_, bim[:, :], sn, ALU.mult, [kim, "sn"], ["t2"])
                    P.tt(zr, t1, t2_, ALU.add, ["t1", "t2"], ["zr"])
                    P.tt(t1, bim[:, :], cs, ALU.mult, [kim, "cs"], ["t1"])
                    P.tt(t2_, bre[:, :], sn, ALU.mult, [kre, "sn"], ["t2"])
                    P.tt(zi, t1, t2_, ALU.subtract, ["t1", "t2"], ["zi"])
                    magb = s5p[:, 0, s:s + 1].to_broadcast([128, T])
                    P.scan(wr, magb, zr, s5st[:, 0, s:s + 1], ALU.mult, ALU.add, ["zr", "s5p", "s5st"], ["wr"])
                    P.scan(wi, magb, zi, s5st[:, 1, s:s + 1], ALU.mult, ALU.add, ["zi", "s5p", "s5st"], ["wi"])
                    P.tt(t1, wr, cs, ALU.mult, ["wr", "cs"], ["t1"])
                    P.tt(t2_, wi, sn, ALU.mult, ["wi", "sn"], ["t2"])
                    P.tt(zr, t1, t2_, ALU.subtract, ["t1", "t2"], ["zr"])
                    P.tt(t1, wr, sn, ALU.mult, ["wr", "sn"], ["t1"])
                    P.tt(t2_, wi, cs, ALU.mult, ["wi", "cs"], ["t2"])
                    P.tt(zi, t1, t2_, ALU.add, ["t1", "t2"], ["zi"])
                    P.cp(s5st[:, 0, s:s + 1], zr[:, T - 1:T], ["zr"], ["s5st"])
                    P.cp(s5st[:, 1, s:s + 1], zi[:, T - 1:T], ["zi"], ["s5st"])
                    P.mm(ps[c][:, :], lf[:, 256:384], zr, s % 4 == 0, False, [lfk, "zr"], [PK(c)])
                    P.mm(ps[c][:, :], lf[:, 384:512], zi, False, s % 4 == 3, [lfk, "zi"], [PK(c)])
                for c in range(4):
                    P.stt(t1, U[:, c, :], vp[:, 32 + c:33 + c], ps[c][:, :], ALU.mult, ALU.add, ["U", "vp", PK(c)], ["t1"])
                    P.tt(t2_, t1, t1, ALU.mult, ["t1"], ["t2"])
                    P.ts(t2_, t2_, 0.044715, 1.0, ALU.mult, ALU.add, ["t2"], ["t2"])
                    P.tt(t2_, t2_, t1, ALU.mult, ["t1", "t2"], ["t2"])
                    P.actf(t2_, t2_, AF.Sigmoid, ["t2"], ["t2"], scale=2.0 * GELU_C)
                    P.tt(G[:, c, :], t1, t2_, ALU.mult, ["t1", "t2"], ["G"])
                    P.cp(Gr_[:, c, :], G[:, c, :], ["G"], ["Gr"], eng="act")

                def cons_glu(i, pap, pk):
                    P.actf(t1, pap, AF.Sigmoid, [pk], ["t1"], bias=vp[:, 36 + i:37 + i], scale=1.0)
                    P.tt(Ybr[:, i, :], G[:, i, :], t1, ALU.mult, ["G", "t1"], ["Y"])
                proj(w_glu[l], [(128 * c, 128, Gr_[:, c, :], "Gr") for c in range(4)], [(0, 512)],
                     [(0, 128 * i, 128) for i in range(4)], cons_glu)
                P.barrier()
                branch([(128 * c, 128, Ybr[:, c, :], "Y") for c in range(4)], (0, 0), False)
                P.barrier()

                for half in range(2):
                    ca_ = Carve(abase)
                    Qb = ca_.t3(3, T); Fb = ca_.t3(3, T); Vb = ca_.t3(3, T); OGb = ca_.t3(3, T)
                    T1 = ca_.t2(T); T2 = ca_.t2(T); CM = ca_.t2(T); D3 = ca_.t2(T)
                    A = ca_.t2(T); Ash = ca_.t2(T); Bm = ca_.t2(T); Kd = ca_.t2(T)
                    VT = ca_.t2(128, parts=64); KT = ca_.t2(128, parts=64); SM = ca_.t2(64, parts=64)
                    EL = ca_.t2(8)

                    def cons1(i, pap, pk):
                        if i < 3:
                            P.actf(Qb[:, i, :], pap, AF.Silu, [pk], ["Qb%d" % i])
                        else:
                            P.actf(Fb[:, i - 3, :], pap, AF.Sigmoid, [pk], ["Fb%d" % (i - 3)])
                    proj(w_in[l], hk(), [(512 + 384 * half, 384), (1280 + 384 * half, 384)],
                         [(0, 0, 128), (0, 128, 128), (0, 256, 128), (1, 0, 128), (1, 128, 128), (1, 256, 128)], cons1)

                    def cons2(i, pap, pk):
                        if i < 3:
                            P.cp(Vb[:, i, :], pap, [pk], ["Vb%d" % i], eng="act")
                        else:
                            P.actf(OGb[:, i - 3, :], pap, AF.Silu, [pk], ["OGb%d" % (i - 3)])
                    proj(w_in[l], hk(), [(2048 + 384 * half, 384), (2816 + 384 * half, 384)],
                         [(0, 0, 128), (0, 128, 128), (0, 256, 128), (1, 0, 128), (1, 128, 128), (1, 256, 128)], cons2)
                    for hh in range(3):
                        h = 3 * half + hh
                        q = Qb[:, hh, :]; f = Fb[:, hh, :]; v = Vb[:, hh, :]; og = OGb[:, hh, :]
                        kq, kf, kv, ko = "Qb%d" % hh, "Fb%d" % hh, "Vb%d" % hh, "OGb%d" % hh
                        P.ts(f, f, lbs[:, 24 + l * 6 + h:24 + l * 6 + h + 1], lbs[:, l * 6 + h:l * 6 + h + 1], ALU.mult, ALU.add, [kf, "lbs"], [kf])
                        P.actf(T1, f, AF.Ln, [kf], ["T1"])
                        P.ts(f, f, -1.0, 1.0, ALU.mult, ALU.add, [kf], [kf])
                        P.scan(T2, ones[:, 0:1].to_broadcast([128, T]), T1, 0.0, ALU.mult, ALU.add, ["T1", "cst"], ["T2"])
                        T23 = T2.rearrange("p (a b) -> p a b", b=64); CM3 = CM.rearrange("p (a b) -> p a b", b=64)
                        P.cp(CM3[:, 0, :], T23[:, 0, :], ["T2"], ["CM"])
                        P.tt(CM3[:, 1:8, :], T23[:, 1:8, :], T23[:, 0:7, 63:64].to_broadcast([128, 7, 64]), ALU.subtract, ["T2"], ["CM"])
                        lastb = CM3[:, :, 63:64].to_broadcast([128, 8, 64])
                        D33 = D3.rearrange("p (a b) -> p a b", b=64)
                        P.stt(D33, lastb, -0.5, CM3, ALU.mult, ALU.add, ["CM"], ["D3"])
                        P.actf(T1, CM, AF.Exp, ["CM"], ["T1"])
                        P.tt(A, q, T1, ALU.mult, [kq, "T1"], ["A"])
                        P.actf(T1, D3, AF.Exp, ["D3"], ["T1"])
                        P.tt(Ash, q, T1, ALU.mult, [kq, "T1"], ["Ash"])
                        P.actf(T2, D3, AF.Exp, ["D3"], ["T2"], scale=-1.0)
                        P.tt(Bm, f, T2, ALU.mult, [kf, "T2"], ["Bm"])
                        P.tt(D33, lastb, CM3, ALU.subtract, ["CM"], ["D3"])
                        P.actf(T1, D3, AF.Exp, ["D3"], ["T1"])
                        P.tt(Kd, f, T1, ALU.mult, [kf, "T1"], ["Kd"])
                        P.actf(EL, CM3[:, :, 63], AF.Exp, ["CM"], ["EL"])
                        S = hst[:, h, :]
                        ks = "hst%d" % h
                        for c in range(8):
                            cols = slice(64 * c, 64 * (c + 1))
                            P.mm(ps[0][0:64, 0:64], Bm[:, cols], Ash[:, cols], True, True, ["Bm", "Ash"], [PK(0)])
                            P.tt(SM, ps[0][0:64, 0:64], m64, ALU.mult, [PK(0), "cst"], ["SM"])
                            P.tr(ps[1][0:64, 0:128], v[:, cols], ident, [kv, "cst"], [PK(1)])
                            P.cp(VT, ps[1][0:64, 0:128], [PK(1)], ["VT"], eng="act")
                            P.tr(ps[2][0:64, 0:128], Kd[:, cols], ident, ["Kd", "cst"], [PK(2)])
                            P.cp(KT, ps[2][0:64, 0:128], [PK(2)], ["KT"], eng="act")
                            P.mm(ps[3][:, cols], VT, SM, True, False, ["VT", "SM"], [PK(3)])
                            P.mm(ps[3][:, cols], S, A[:, cols], False, True, [ks, "A"], [PK(3)])
                            P.mm(ps[4][:, 0:128], KT, VT, True, True, ["KT", "VT"], [PK(4)])
                            P.stt(S, S, EL[:, c:c + 1], ps[4][:, 0:128], ALU.mult, ALU.add, [ks, "EL", PK(4)], [ks])
                        P.cp(T1, ps[3][:, :], [PK(3)], ["T1"])
                        P.actf(T2, T1, AF.Square, ["T1"], ["T2"])
                        P.mm(ps[5][:, :], ones, T2, True, True, ["T2", "cst"], [PK(5)])
                        P.actf(T2, ps[5][:, :], AF.Sqrt, [PK(5)], ["T2"], bias=epsc, scale=1.0 / 128)
                        P.add("dve", lambda e, T2=T2: e.reciprocal(T2, T2), ["T2"], ["T2"])
                        P.stt(T1, T1, vp[:, 40 + h:41 + h], T2, ALU.mult, ALU.mult, ["T1", "T2", "vp"], ["T1"])
                        P.tt(Ybr[:, h, :], T1, og, ALU.mult, ["T1", ko], ["Y"])
                P.barrier()
                branch([(128 * c, 128, Ybr[:, c, :], "Y") for c in range(6)], (1, 512), False)
                P.barrier()

                ca_ = Carve(abase)
                IG = ca_.t2(T, parts=4); LFr = ca_.t2(T, parts=4); Bc = ca_.t2(T, parts=4); Rr = ca_.t2(T, parts=4)
                AC = ca_.t3(4, 4); CS = ca_.t2(4, parts=4); DEC = ca_.t2(4, parts=4); DB = ca_.t2(4, parts=96)
                CXs = ca_.t3(2, 515, parts=96); Vh = ca_.t3(2, T, parts=96); OGs = ca_.t3(2, T, parts=96)
                acc = ca_.t3(2, T, parts=96); CAr = ca_.h3(2, T, parts=96)
                Qm = ca_.t3(2, T, parts=96); Km = ca_.t3(2, T, parts=96); HS = ca_.t3(2, T, parts=96)
                DS = ca_.t2(T, parts=96); DT2 = ca_.t2(T, parts=96)
                PT = ca_.t2(128); VTm = ca_.t2(192); KTa = ca_.t2(192); CTm = ca_.t2(384, parts=96)
                Ycr = Ybr[0:96, :, :]

                def cons_g4(i, pap, pk):
                    if i == 0:
                        P.actf(IG, pap, AF.Identity, [pk], ["IG"], bias=rc[:, 1:2], scale=1.0)
                    else:
                        P.actf(LFr, pap, AF.Sigmoid, [pk], ["LFr"], bias=rc[:, 2:3], scale=1.0)
                        P.actf(LFr, LFr, AF.Ln, ["LFr"], ["LFr"])
                proj(w_in[l], hk(), [(5888, 8)], [(0, 0, 4), (0, 4, 4)], cons_g4)
                ones4 = ones[0:4, 0:1].to_broadcast([4, T])
                P.scan(Bc, ones4, LFr, 0.0, ALU.mult, ALU.add, ["LFr", "cst"], ["Bc"])
                P.tt(IG, IG, Bc, ALU.subtract, ["IG", "Bc"], ["IG"])
                P.scan(Rr, ones4, IG, rc[:, 0:1], ALU.mult, ALU.max, ["IG", "rc", "cst"], ["Rr"])
                R3 = Rr.rearrange("p (a b) -> p a b", b=128)
                P.cp(CS[:, 0:1], rc[:, 0:1], ["rc"], ["CS"])
                P.cp(CS[:, 1:4], R3[:, 0:3, 127], ["Rr"], ["CS"])
                P.tt(DEC, CS, R3[:, :, 127], ALU.subtract, ["CS", "Rr"], ["DEC"])
                P.actf(DEC, DEC, AF.Exp, ["DEC"], ["DEC"])
                csb = CS.unsqueeze(2).to_broadcast([4, 4, 128])
                P.tt(LFr.rearrange("p (a b) -> p a b", b=128), IG.rearrange("p (a b) -> p a b", b=128), csb, ALU.subtract, ["IG", "CS"], ["LFr"])
                P.actf(LFr, LFr, AF.Exp, ["LFr"], ["LFr"])
                P.tt(IG.rearrange("p (a b) -> p a b", b=128), Bc.rearrange("p (a b) -> p a b", b=128), csb, ALU.add, ["Bc", "CS", "IG"], ["IG"])
                P.actf(IG, IG, AF.Exp, ["IG"], ["IG"], scale=-1.0)
                P.tt(rc[:, 0:1], Bc[:, T - 1:T], Rr[:, T - 1:T], ALU.add, ["Bc", "Rr"], ["rc"])
                for ch in range(4):
                    P.mm(ps[7][:, 4 * ch:4 * ch + 4], LFr[:, 128 * ch:128 * (ch + 1)], ident[0:4, 0:4], True, True, ["LFr", "cst"], [PK(7)])
                P.cp(AC.rearrange("p a b -> p (a b)"), ps[7][:, 0:16], [PK(7)], ["AC"])
                for h in range(4):
                    def cons_m(i, pap, pk, h=h):
                        j = i % 2
                        if i < 2:
                            P.cp(CXs[:, j, 3:515], pap, [pk], ["CXs%d" % j], eng="act")
                        elif i < 4:
                            P.cp(Vh[:, j, :], pap, [pk], ["Vh"], eng="act")
                        else:
                            P.actf(OGs[:, j, :], pap, AF.Sigmoid, [pk], ["OGs"])
                    proj(w_in[l], hk(), [(3584 + 192 * h, 192), (4352 + 192 * h, 192), (5120 + 192 * h, 192)],
                         [(0, 0, 96), (0, 96, 96), (1, 0, 96), (1, 96, 96), (2, 0, 96), (2, 96, 96)], cons_m)
                    for j in range(2):
                        fj = 2 * h + j
                        P.cp(CXs[:, j, 0:3], mlt[:, fj, :], ["mlt"], ["CXs%d" % j])
                        P.actf(acc[:, j, :], CXs[:, j, 3:515], AF.Identity, ["CXs%d" % j, "vq"], ["acc"],
                               bias=vq[:, 32 + fj:33 + fj], scale=vq[:, 24 + fj:25 + fj])
                        for k in range(3):
                            P.stt(acc[:, j, :], CXs[:, j, k:k + T], vq[:, 8 * k + fj:8 * k + fj + 1], acc[:, j, :], ALU.mult, ALU.add,
                                  ["CXs%d" % j, "vq", "acc"], ["acc"])
                        P.cp(mlt[:, fj, :], CXs[:, j, 512:515], ["CXs%d" % j], ["mlt"])
                        P.actf(CAr[:, j, :], acc[:, j, :], AF.Silu, ["acc"], ["CA"])

                    def cons_qk(i, pap, pk):
                        if i < 2:
                            P.cp(Qm[:, i, :], pap, [pk], ["Qm"], eng="act")
                        else:
                            P.ts(Km[:, i - 2, :], pap, 192.0 ** -0.5, None, ALU.mult, None, [pk], ["Km"])
                    proj(w_qk[l, h], [(0, 96, CAr[:, 0, :], "CA"), (96, 96, CAr[:, 1, :], "CA")], [(0, 384)],
                         [(0, 96 * i, 96) for i in range(4)], cons_qk)
                    P.mm(ps[7][0:96, 0:4], sel4(h), DEC, True, True, ["DEC", "cst"], [PK(7)])
                    P.cp(DB, ps[7][0:96, 0:4], [PK(7)], ["DB"])
                    kC, kN = "mlC%d" % h, "mlN%d" % h
                    for ch in range(4):
                        cols = slice(128 * ch, 128 * (ch + 1))
                        P.mm(ps[0][:, 0:128], Km[:, 0, cols], Qm[:, 0, cols], True, False, ["Km", "Qm"], [PK(0)])
                        P.mm(ps[0][:, 0:128], Km[:, 1, cols], Qm[:, 1, cols], False, True, ["Km", "Qm"], [PK(0)])
                        P.stt(PT, ps[0][:, 0:128], AC[:, ch, h:h + 1], m128, ALU.mult, ALU.mult, [PK(0), "AC", "cst"], ["PT"])
                        for j in range(2):
                            P.tr(ps[1][:, 96 * j:96 * (j + 1)], Vh[:, j, cols], ident[0:96, 0:96], ["Vh", "cst"], [PK(1)])
                        P.cp(VTm, ps[1][:, 0:192], [PK(1)], ["VTm"], eng="act")
                        for j in range(2):
                            P.tr(ps[1][:, 192 + 96 * j:192 + 96 * (j + 1)], Km[:, j, cols], ident[0:96, 0:96], ["Km", "cst"], [PK(1)])
                        P.ts(KTa, ps[1][:, 192:384], AC[:, ch, h:h + 1], None, ALU.mult, None, [PK(1), "AC"], ["KTa"])
                        for j in range(2):
                            pn = ps[2 + j]
                            P.mm(pn[0:96, cols], VTm[:, 96 * j:96 * (j + 1)], PT, True, False, ["VTm", "PT"], [PK(2 + j)])
                            P.mm(pn[0:96, cols], mlC[:, h, 0, 96 * j:96 * (j + 1)], Qm[:, 0, cols], False, False, [kC, "Qm"], [PK(2 + j)])
                            P.mm(pn[0:96, cols], mlC[:, h, 1, 96 * j:96 * (j + 1)], Qm[:, 1, cols], False, True, [kC, "Qm"], [PK(2 + j)])
                        P.mm(ps[4][0:96, cols], ones[:, 0:96], PT, True, False, ["cst", "PT"], [PK(4)])
                        P.mm(ps[4][0:96, cols], mlN[:, h, 0, :], Qm[:, 0, cols], False, False, [kN, "Qm"], [PK(4)])
                        P.mm(ps[4][0:96, cols], mlN[:, h, 1, :], Qm[:, 1, cols], False, True, [kN, "Qm"], [PK(4)])
                        for kt in range(2):
                            P.mm(ps[5][0:96, 192 * kt:192 * (kt + 1)], KTa[:, 96 * kt:96 * (kt + 1)], VTm, True, True, ["KTa", "VTm"], [PK(5)])
                            P.mm(ps[6][0:96, 96 * kt:96 * (kt + 1)], KTa[:, 96 * kt:96 * (kt + 1)], ones[:, 0:96], True, True, ["KTa", "cst"], [PK(6)])
                        Cf = mlC[:, h, :, :].rearrange("p a b -> p (a b)")
                        Nf = mlN[:, h, :, :].rearrange("p a b -> p (a b)")
                        P.tt(CTm, ps[5][0:96, 0:384], Cf, ALU.add, [PK(5), kC], ["CTm"])
                        P.ts(Cf, CTm, DB[:, ch:ch + 1], None, ALU.mult, None, ["CTm", "DB"], [kC])
                        P.tt(CTm[:, 0:192], ps[6][0:96, 0:192], Nf, ALU.add, [PK(6), kN], ["CTm"])
                        P.ts(Nf, CTm[:, 0:192], DB[:, ch:ch + 1], None, ALU.mult, None, ["CTm", "DB"], [kN])
                    P.mm(ps[7][0:96, :], sel4(h), IG, True, True, ["IG", "cst"], [PK(7)])
                    P.cp(DS, ps[4][0:96, :], [PK(4)], ["DS"])
                    P.stt(DT2, DS, -1.0, DS, ALU.mult, ALU.max, ["DS"], ["DT2"])
                    P.tt(DT2, DT2, ps[7][0:96, :], ALU.max, ["DT2", PK(7)], ["DT2"])
                    P.add("dve", lambda e, DT2=DT2: e.reciprocal(DT2, DT2), ["DT2"], ["DT2"])
                    for j in range(2):
                        P.tt(HS[:, j, :], ps[2 + j][0:96, :], DT2, ALU.mult, [PK(2 + j), "DT2"], ["HS"])
                        P.actf(acc[:, j, :], HS[:, j, :], AF.Square, ["HS"], ["acc"])
                        P.mm(ps[7][0:96, :], ones[0:96, 0:96], acc[:, j, :], j == 0, j == 1, ["acc", "cst"], [PK(7)])
                    P.actf(DS, ps[7][0:96, :], AF.Sqrt, [PK(7)], ["DS"], bias=epsc[0:96, :], scale=1.0 / 192)
                    P.add("dve", lambda e, DS=DS: e.reciprocal(DS, DS), ["DS"], ["DS"])
                    for j in range(2):
                        fj = 2 * h + j
                        P.stt(HS[:, j, :], HS[:, j, :], vq[:, 40 + fj:41 + fj], DS, ALU.mult, ALU.mult, ["HS", "DS", "vq"], ["HS"])
                        P.tt(Ycr[:, fj, :], HS[:, j, :], OGs[:, j, :], ALU.mult, ["HS", "OGs"], ["Y"])
                P.barrier()
                branch([(96 * i, 96, Ycr[:, i, :], "Y") for i in range(8)], (2, 1280), True)

                for g0, ng in ((0, 6), (6, 6), (12, 4)):
                    def cons_o(i, pap, pk, g0=g0):
                        j = g0 + i
                        P.tt(xt[:, j, :], xt[:, j, :], pap, ALU.add, ["xt%d" % j, pk], ["xt%d" % j])
                    proj(w_out[l], [(128 * c, 128, mergedr[:, c, :], "mgr%d" % c) for c in range(NCH)],
                         [(128 * g0, 128 * ng)], [(0, 128 * i, 128) for i in range(ng)], cons_o)
                P.barrier()

                cv = Carve()
                sq2 = cv.t3(2, T); rs = cv.t2(T)
                rmsnorm(htr, lambda c: vp[:, 16 + c:17 + c], sq2, rs, "ht")
                ringB = cv.h3(3, D)
                actgr = cv.h3(6, T)
                ST = cv.t3(2, 514); accA = cv.t2(T); accB = cv.t2(T)
                gi = 0
                g0 = 0
                while g0 < 44:
                    ng = min(3, 44 - g0)
                    ab = (gi % 2) * 3

                    def cons_f(i, pap, pk, g0=g0, ng=ng, ab=ab):
                        isb = i % 2
                        ii = i // 2
                        cidx = g0 + ii + (44 if isb else 0)
                        st = ST[:, isb, :]
                        kst = "ST%d" % isb
                        dst = accB if isb else accA
                        kd = "accB" if isb else "accA"
                        P.cp(st[:, 2:514], pap, [pk], [kst], eng="act")
                        P.cp(st[:, 0:2], ftl[:, cidx, :], ["ftl"], [kst])
                        P.actf(dst, st[:, 2:514], AF.Identity, [kst, "vp"], [kd],
                               bias=vp[:, 46 + cidx:47 + cidx], scale=vp[:, 134 + 88 * 2 + cidx:135 + 88 * 2 + cidx])
                        for k in range(2):
                            P.stt(dst, st[:, k:k + T], vp[:, 134 + 88 * k + cidx:135 + 88 * k + cidx], dst, ALU.mult, ALU.add, [kst, "vp", kd], [kd])
                        P.cp(ftl[:, cidx, :], st[:, 512:514], [kst], ["ftl"])
                        if isb == 0:
                            P.actf(accA, accA, AF.Silu, ["accA"], ["accA"])
                        else:
                            P.tt(actgr[:, ab + ii, :], accA, accB, ALU.mult, ["accA", "accB"], ["actg%d" % (ab + ii)])
                    tiles = []
                    for ii in range(ng):
                        tiles.append((0, 128 * ii, 128))
                        tiles.append((1, 128 * ii, 128))
                    proj(w_up[l], hk(), [(128 * g0, 128 * ng), (FFN + 128 * g0, 128 * ng)], tiles, cons_f)
                    for ii in range(ng):
                        P.dma("pool", ringB[:, ii, :], w_dn[l, 128 * (g0 + ii):128 * (g0 + ii + 1), :], (), ["ringB%d" % ii])
                    for j in range(NCH):
                        b = 6 + (j % 2)
                        for ii in range(ng):
                            P.mm(ps[b][:, :], ringB[:, ii, 128 * j:128 * (j + 1)], actgr[:, ab + ii, :], ii == 0, ii == ng - 1,
                                 ["ringB%d" % ii, "actg%d" % (ab + ii)], [PK(b)])
                        P.tt(xt[:, j, :], xt[:, j, :], ps[b][:, :], ALU.add, ["xt%d" % j, PK(b)], ["xt%d" % j])
                    g0 += ng
                    gi += 1
                P.barrier()

                if l < NL - 1:
                    P.dma("sp", xs_d[t], xt[:].rearrange("p a b -> p (a b)"), ["xt%d" % c for c in range(NCH)], ["xs%d" % t])
                else:
                    cv = Carve()
                    sq2 = cv.t3(2, T); rs = cv.t2(T)
                    xo = cv.t2(D)
                    hf = cv.t3(NCH, T)
                    rmsnorm(hf, lambda c: vp[:, 398 + c:399 + c], sq2, rs, "hf")
                    for tb in range(4):
                        for c in range(NCH):
                            P.tr(ps[c % 8][:, 0:128], hf[:, c, tb * 128:(tb + 1) * 128], ident, ["hf", "cst"], [PK(c % 8)])
                            P.cp(xo[:, c * 128:(c + 1) * 128], ps[c % 8][:, 0:128], [PK(c % 8)], ["xo"], eng=("act" if c % 2 else "dve"))
                        P.dma("sp", y_d[tok0 + tb * 128:tok0 + (tb + 1) * 128, :], xo, ["xo"], ["y"])
                P.barrier()
        P.emit()
    return nc


WNAMES = ["mix_norm", "w_in", "s5_lam_re", "s5_lam_im", "s5_log_dt", "s5_b_re", "s5_b_im", "s5_c_re", "s5_c_im",
          "s5_d", "s5_w_glu", "s5_b_glu", "hg_lower_bounds", "hg_norm", "ml_conv_w", "ml_conv_b", "ml_w_qk",
          "ml_b_ig", "ml_b_fg", "ml_norm", "w_branch", "w_out", "ffn_norm", "ffn_w_up", "ffn_conv_w", "ffn_conv_b",
          "ffn_w_down", "final_norm"]


def run(inputs, NL, NT, ncores):
    nc = build(NL, NT)
    x = np.asarray(inputs["x"], np.float32)
    B = x.shape[0]
    cstv = make_consts()
    shared = {k: np.ascontiguousarray(np.asarray(inputs[k], np.float32)) for k in WNAMES}
    in_maps = []
    for c in range(ncores):
        m = dict(shared)
        m["x"] = np.ascontiguousarray(x[c % B])
        m["cst"] = cstv
        in_maps.append(m)
    res = run_bass_kernel_spmd(nc, in_maps, core_ids=list(range(ncores)))
    return np.stack([res.results[b]["y"] for b in range(B)], axis=0).astype(np.float32)


def kernel(**inputs):
    return run(inputs, 4, 8, 8)
```

```python
import numpy as np
from contextlib import ExitStack
import concourse.bass as bass
import concourse.mybir as mybir

F32 = mybir.dt.float32
F32R = mybir.dt.float32r
BF = mybir.dt.bfloat16
ALU = mybir.AluOpType
AF = mybir.ActivationFunctionType
AX = mybir.AxisListType

ENGS = ("pe", "act", "dve", "pool", "sp")
NDSEM = 24


class Op:
    __slots__ = ("eng", "fn", "waits", "done", "dma", "idx")


class Prog:
    def __init__(self, nc, es):
        self.nc = nc
        self.ops = {e: [] for e in ENGS}
        self.esem = {e: es.enter_context(nc.semaphore("sem_" + e)) for e in ENGS}
        self.ecnt = {e: 0 for e in ENGS}
        self.dsem = {q: [es.enter_context(nc.semaphore("dq_%s_%d" % (q, i))) for i in range(NDSEM)]
                     for q in ("sp", "pool", "act")}
        self.dcnt = {q: [0] * NDSEM for q in ("sp", "pool", "act")}
        self.dnext = {q: 0 for q in ("sp", "pool", "act")}
        self.waited = {e: {} for e in ENGS}
        self.lastw = {}
        self.readers = {}
        self.pending = {e: [] for e in ENGS}
        self.nops = 0

    def _need(self, eng, waits, dep):
        sem, val, deng, ddma = dep
        if (not ddma) and deng == eng and eng == "pe":
            return
        key = id(sem)
        if self.waited[eng].get(key, 0) >= val:
            return
        waits[key] = (sem, max(val, waits.get(key, (sem, 0))[1]))

    def add(self, eng, fn, reads=(), writes=(), dma=False):
        op = Op()
        op.eng = eng
        op.fn = fn
        op.dma = dma
        waits = {}
        for k in reads:
            w = self.lastw.get(k)
            if w is not None:
                self._need(eng, waits, w)
        for k in writes:
            w = self.lastw.get(k)
            if w is not None:
                self._need(eng, waits, w)
            for r in self.readers.get(k, ()):
                self._need(eng, waits, r)
        for dep in self.pending[eng]:
            self._need(eng, waits, dep)
        self.pending[eng] = []
        if dma:
            q = eng
            i = self.dnext[q]
            self.dnext[q] = (i + 1) % NDSEM
            sem = self.dsem[q][i]
            if self.dcnt[q][i] > 0:
                self._need(eng, waits, (sem, self.dcnt[q][i], eng, True))
            self.dcnt[q][i] += 16
            op.done = (sem, self.dcnt[q][i], eng, True)
        else:
            self.ecnt[eng] += 1
            op.done = (self.esem[eng], self.ecnt[eng], eng, False)
        op.waits = list(waits.values())
        for sem, val in op.waits:
            self.waited[eng][id(sem)] = val
        for k in reads:
            self.readers.setdefault(k, []).append(op.done)
        for k in writes:
            self.lastw[k] = op.done
            self.readers[k] = []
        self.ops[eng].append(op)
        self.nops += 1
        return op

    def barrier(self):
        deps = []
        for e in ENGS:
            if self.ecnt[e] > 0:
                deps.append((self.esem[e], self.ecnt[e], e, True))
        for q in self.dsem:
            for i in range(NDSEM):
                if self.dcnt[q][i] > 0:
                    deps.append((self.dsem[q][i], self.dcnt[q][i], q, True))
        for e in ENGS:
            self.pending[e] = list(deps)
        self.lastw = {}
        self.readers = {}

    def emit(self, final_waits_eng="sp"):
        nc = self.nc
        self.barrier()
        fin = self.pending[final_waits_eng]
        with nc.Block() as block:
            def run(e, eng):
                for op in self.ops[e]:
                    for sem, val in op.waits:
                        eng.wait_ge(sem, val)
                    ins = op.fn(eng)
                    sem, val, _, ddma = op.done
                    ins.then_inc(sem, 16 if ddma else 1)
                if e == final_waits_eng:
                    w = {}
                    for sem, val, _, _ in fin:
                        if self.waited[e].get(id(sem), 0) < val:
                            w[id(sem)] = (sem, max(val, w.get(id(sem), (sem, 0))[1]))
                    for sem, val in w.values():
                        eng.wait_ge(sem, val)

            @block.tensor
            def _(eng):
                run("pe", eng)

            @block.scalar
            def _(eng):
                run("act", eng)

            @block.vector
            def _(eng):
                run("dve", eng)

            @block.gpsimd
            def _(eng):
                run("pool", eng)

            @block.sync
            def _(eng):
                run("sp", eng)

    def mm(self, out, lhsT, rhs, start, stop, r, w):
        return self.add("pe", lambda e: e.matmul(out, lhsT, rhs, start=start, stop=stop), r, w)

    def tr(self, out, in_, ident, r, w):
        return self.add("pe", lambda e: e.transpose(out, in_, ident), r, w)

    def actf(self, out, in_, func, r, w, bias=None, scale=None):
        kw = {}
        if bias is not None:
            kw["bias"] = bias
        if scale is not None:
            kw["scale"] = scale
        return self.add("act", lambda e: e.activation(out, in_, func, **kw), r, w)

    def tt(self, out, a, b, op, r, w, eng="dve"):
        return self.add(eng, lambda e: e.tensor_tensor(out, a, b, op), r, w)

    def ts(self, out, a, s1, s2, op0, op1, r, w, eng="dve"):
        if op1 is None:
            return self.add(eng, lambda e: e.tensor_scalar(out, a, s1, None, op0), r, w)
        return self.add(eng, lambda e: e.tensor_scalar(out, a, s1, s2, op0, op1), r, w)

    def stt(self, out, a, s, b, op0, op1, r, w, eng="dve"):
        return self.add(eng, lambda e: e.scalar_tensor_tensor(out, a, s, b, op0, op1), r, w)

    def cp(self, out, a, r, w, eng="dve"):
        if eng == "act":
            return self.add("act", lambda e: e.copy(out, a), r, w)
        return self.add(eng, lambda e: e.tensor_copy(out, a), r, w)

    def scan(self, out, d0, d1, init, op0, op1, r, w):
        return self.add("dve", lambda e: e.tensor_tensor_scan(out, d0, d1, init, op0, op1), r, w)

    def memset(self, ap, val, w, eng="dve"):
        return self.add(eng, lambda e: e.memset(ap, val), (), w)

    def dma(self, q, out, in_, r, w):
        return self.add(q, lambda e: e.dma_start(out=out, in_=in_), r, w, dma=True)


import math
from concourse.bass_utils import run_bass_kernel_spmd

T = 512
D = 2048
NCH = 16
EPS = 1e-6
MAGIC = 12582912.0
TWO_PI = 2.0 * math.pi
GELU_C = math.sqrt(2.0 / math.pi)
FFN = 5632
IN_TOTAL = 12040
C_IDENT, C_ONES, C_M128, C_M64, C_TT, C_MG, C_EPS, C_ZERO, C_SEL = 0, 128, 256, 384, 448, 960, 962, 963, 964
NCST = 964 + 384
RW = 768
NRING = 8
RCOLS = 26624


def make_consts():
    c = np.zeros((128, NCST), np.float32)
    c[:, C_IDENT:C_IDENT + 128] = np.eye(128)
    c[:, C_ONES:C_ONES + 128] = 1.0
    c[:, C_M128:C_M128 + 128] = np.triu(np.ones((128, 128)))
    c[:64, C_M64:C_M64 + 64] = np.triu(np.ones((64, 64)))
    c[:, C_TT:C_TT + 512] = np.arange(1, 513)[None, :]
    c[:64, C_MG] = 1.0
    c[64:, C_MG + 1] = 1.0
    c[:, C_EPS] = EPS
    for h in range(4):
        c[h, C_SEL + h * 96:C_SEL + (h + 1) * 96] = 1.0
    return c


def build(NL, NT):
    nc = bass.Bass("TRN2", target_bir_lowering=False)
    es = ExitStack()

    def din(name, shape):
        return nc.dram_tensor(name, list(shape), F32, kind="ExternalInput").ap()

    x_d = din("x", [NT * T, D])
    cst_d = din("cst", [128, NCST])
    mix_norm = din("mix_norm", [NL, D]); w_in = din("w_in", [NL, D, IN_TOTAL])
    lam_re = din("s5_lam_re", [NL, 32, 64]); lam_im = din("s5_lam_im", [NL, 32, 64]); log_dt = din("s5_log_dt", [NL, 32])
    b_re = din("s5_b_re", [NL, 32, 64, 16]); b_im = din("s5_b_im", [NL, 32, 64, 16])
    c_re = din("s5_c_re", [NL, 32, 16, 64]); c_im = din("s5_c_im", [NL, 32, 16, 64])
    s5_d = din("s5_d", [NL, 512]); w_glu = din("s5_w_glu", [NL, 512, 512]); b_glu = din("s5_b_glu", [NL, 512])
    hg_lb = din("hg_lower_bounds", [NL, 768]); hg_norm = din("hg_norm", [NL, 768])
    ml_cw = din("ml_conv_w", [NL, 4, 768]); ml_cb = din("ml_conv_b", [NL, 768]); w_qk = din("ml_w_qk", [NL, 4, 192, 384])
    b_ig = din("ml_b_ig", [NL, 4]); b_fg = din("ml_b_fg", [NL, 4]); ml_norm = din("ml_norm", [NL, 768])
    w_br = din("w_branch", [NL, D, D]); w_out = din("w_out", [NL, D, D]); ffn_norm = din("ffn_norm", [NL, D])
    w_up = din("ffn_w_up", [NL, D, 2 * FFN]); f_cw = din("ffn_conv_w", [NL, 3, 2 * FFN]); f_cb = din("ffn_conv_b", [NL, 2 * FFN])
    w_dn = din("ffn_w_down", [NL, FFN, D]); fin_norm = din("final_norm", [D])
    y_d = nc.dram_tensor("y", [NT * T, D], F32, kind="ExternalOutput").ap()
    xs_d = nc.dram_tensor("xs_scr", [NT, 128, NCH * T], F32, kind="Internal").ap()
    lbfc_d = nc.dram_tensor("lbfc_scr", [NL, 16, 128, 512], F32, kind="Internal").ap()
    tab_d = nc.dram_tensor("tab_scr", [NL, 16, 128, 1024], F32, kind="Internal").ap()

    with es:
        P = Prog(nc, es)
        sbt = lambda n, s, d=F32: es.enter_context(nc.sbuf_tensor(n + "_sb", s, d))
        cst = sbt("cst", [128, NCST])
        xt = sbt("xt", [128, NCH, T])
        ht = sbt("ht", [128, NCH, T], BF)
        htr = ht[:]
        ring = sbt("ring", [128, NRING, 2, RW], BF)
        R = sbt("R", [128, RCOLS])
        vp = sbt("vp", [128, 420])
        vq = sbt("vq", [96, 48])
        lbs = sbt("lbs", [128, 4 * 6 * 2])
        s5st = sbt("s5st", [128, 2, 16])
        s5p = sbt("s5p", [128, 2, 16])
        hst = sbt("hst", [128, 6, 128])
        mlC = sbt("mlC", [96, 4, 2, 192])
        mlN = sbt("mlN", [96, 4, 2, 96])
        mlt = sbt("mlt", [96, 8, 3])
        ftl = sbt("ftl", [128, 88, 2])
        rc = sbt("rc", [4, 4])
        ps = [es.enter_context(nc.psum_tensor("psb%d" % i, [128, 512], F32)) for i in range(8)]
        PK = lambda b: "ps%d" % b

        ident = cst[:, C_IDENT:C_IDENT + 128]
        ones = cst[:, C_ONES:C_ONES + 128]
        m128 = cst[:, C_M128:C_M128 + 128]
        m64 = cst[0:64, C_M64:C_M64 + 64]
        tt_i = cst[:, C_TT:C_TT + 512]
        epsc = cst[:, C_EPS:C_EPS + 1]

        def sel4(h):
            return cst[0:4, C_SEL + 96 * h:C_SEL + 96 * (h + 1)]

        class Carve:
            def __init__(self, base=0):
                self.o = base

            def t2(self, n, parts=128):
                a = R[0:parts, self.o:self.o + n]
                self.o += n
                assert self.o <= RCOLS, self.o
                return a

            def t3(self, a, b, parts=128):
                return self.t2(a * b, parts).rearrange("p (a b) -> p a b", b=b)

            def h2(self, n, parts=128):
                return self.t2((n + 1) // 2, parts).bitcast(BF)[:, 0:n]

            def h3(self, a, b, parts=128):
                return self.t2(a * b // 2, parts).bitcast(BF).rearrange("p (a b) -> p a b", b=b)

        P.dma("sp", cst[:], cst_d, (), ["cst"])
        ring_i = [0]

        def proj(wsrc, kchunks, pieces, tiles, consume, banks=None, ringB=None):
            banks = banks or list(range(len(tiles)))
            nk = len(kchunks)
            ki = 0
            while ki < nk:
                r0, nr, rhs, rkey = kchunks[ki]
                pack = 1
                if nr == 128 and ki + 1 < nk and kchunks[ki + 1][1] == 128 and kchunks[ki + 1][0] == r0 + 128:
                    pack = 2
                slots = []
                for (c0, wd) in pieces:
                    s = ring_i[0] % NRING
                    ring_i[0] += 1
                    if pack == 2:
                        P.dma("pool", ring[:, s, :, 0:wd], wsrc[r0:r0 + 256, c0:c0 + wd].rearrange("(a p) c -> p a c", p=128),
                              (), ["ring%d" % s])
                    else:
                        P.dma("pool", ring[0:nr, s, 0, 0:wd], wsrc[r0:r0 + nr, c0:c0 + wd], (), ["ring%d" % s])
                    slots.append(s)
                for a in range(pack):
                    _, nr_a, rhs_a, rkey_a = kchunks[ki + a]
                    for ti, (pi, off, M) in enumerate(tiles):
                        s = slots[pi]
                        P.mm(ps[banks[ti]][0:M, :], ring[0:nr_a, s, a, off:off + M], rhs_a, ki + a == 0, ki + a == nk - 1,
                             ["ring%d" % s, rkey_a], [PK(banks[ti])])
                ki += pack
            for ti, (pi, off, M) in enumerate(tiles):
                consume(ti, ps[banks[ti]][0:M, :], PK(banks[ti]))

        def hk(keyprefix="ht"):
            return [(128 * c, 128, htr[:, c, :], "ht") for c in range(NCH)]

        def emit_sin(out, x, tk, kx, kt, kout):
            P.ts(tk, x, 1.0 / TWO_PI, MAGIC, ALU.mult, ALU.add, [kx], [kt])
            P.ts(tk, tk, -MAGIC, None, ALU.add, None, [kt], [kt])
            P.stt(x, tk, -TWO_PI, x, ALU.mult, ALU.add, [kt, kx], [kx])
            P.ts(tk, x, math.pi, TWO_PI, ALU.is_gt, ALU.mult, [kx], [kt])
            P.tt(x, x, tk, ALU.subtract, [kx, kt], [kx])
            P.ts(x, x, math.pi, -math.pi, ALU.min, ALU.max, [kx], [kx])
            P.actf(out, x, AF.Sin, [kx], [kout])

        def load_T(dst, src, nr, w, stage, kst, bank=7):
            P.dma("sp", stage[0:nr, 0:w], src, (), [kst])
            P.tr(ps[bank][0:w, 0:nr], stage[0:nr, 0:w], ident[0:nr, 0:nr], [kst, "cst"], [PK(bank)])
            P.cp(dst, ps[bank][0:w, 0:nr], [PK(bank)], ["vp"])

        def rmsnorm(dst, gcol, sq2, rs, kdst):
            for c in range(NCH):
                sq = sq2[:, c % 2, :]
                P.actf(sq, xt[:, c, :], AF.Square, ["xt%d" % c], ["sq%d" % (c % 2)])
                P.mm(ps[7][:, :], ones, sq, c == 0, c == NCH - 1, ["sq%d" % (c % 2), "cst"], [PK(7)])
            P.actf(rs, ps[7][:, :], AF.Sqrt, [PK(7)], ["rs"], bias=epsc, scale=1.0 / D)
            P.add("dve", lambda e: e.reciprocal(rs, rs), ["rs"], ["rs"])
            for c in range(NCH):
                P.stt(dst[:, c, :], xt[:, c, :], gcol(c), rs, ALU.mult, ALU.mult, ["xt%d" % c, "rs", "vp"], [kdst])

        cv = Carve()
        stg = cv.t2(128)
        lraw = cv.t2(NL * 6)
        for l in range(NL):
            load_T(lraw[:, l * 6:(l + 1) * 6], hg_lb[l].rearrange("(c p) -> c p", p=128), 6, 128, stg, "stg")
        ex = cv.t2(NL * 6)
        tot = cv.t2(6)
        P.actf(ex, lraw, AF.Exp, ["vp"], ["ex"])
        P.cp(tot, ex[:, 0:6], ["ex"], ["tot"])
        for l in range(1, NL):
            P.tt(tot, tot, ex[:, l * 6:(l + 1) * 6], ALU.add, ["tot", "ex"], ["tot"])
        P.add("dve", lambda e: e.reciprocal(tot, tot), ["tot"], ["tot"])
        P.memset(lbs[:, 0:6], 0.0, ["lbs"])
        for l in range(1, NL):
            if l == 1:
                P.cp(lbs[:, 6:12], ex[:, 6:12], ["ex"], ["lbs"])
            else:
                P.tt(lbs[:, l * 6:(l + 1) * 6], lbs[:, (l - 1) * 6:l * 6], ex[:, l * 6:(l + 1) * 6], ALU.add, ["lbs", "ex"], ["lbs"])
        for l in range(NL):
            if l > 0:
                P.tt(lbs[:, l * 6:(l + 1) * 6], lbs[:, l * 6:(l + 1) * 6], tot, ALU.mult, ["lbs", "tot"], ["lbs"])
        for l in range(NL):
            P.ts(lbs[:, 24 + l * 6:24 + (l + 1) * 6], lbs[:, l * 6:(l + 1) * 6], -1.0, 1.0, ALU.mult, ALU.add, ["lbs"], ["lbs"])
        P.barrier()

        for l in range(NL):
            cv = Carve()
            stg = cv.t2(128)
            load_T(vp[:, 0:16], mix_norm[l].rearrange("(c p) -> c p", p=128), 16, 128, stg, "stg")
            load_T(vp[:, 16:32], ffn_norm[l].rearrange("(c p) -> c p", p=128), 16, 128, stg, "stg")
            load_T(vp[:, 32:36], s5_d[l].rearrange("(c p) -> c p", p=128), 4, 128, stg, "stg")
            load_T(vp[:, 36:40], b_glu[l].rearrange("(c p) -> c p", p=128), 4, 128, stg, "stg")
            load_T(vp[:, 40:46], hg_norm[l].rearrange("(c p) -> c p", p=128), 6, 128, stg, "stg")
            load_T(vp[:, 46:134], f_cb[l].rearrange("(c p) -> c p", p=128), 88, 128, stg, "stg")
            for k in range(3):
                load_T(vp[:, 134 + 88 * k:134 + 88 * (k + 1)], f_cw[l, k].rearrange("(c p) -> c p", p=128), 88, 128, stg, "stg")
            load_T(vp[:, 398:414], fin_norm.rearrange("(c p) -> c p", p=128), 16, 128, stg, "stg")
            for k in range(4):
                load_T(vq[:, 8 * k:8 * (k + 1)], ml_cw[l, k].rearrange("(c p) -> c p", p=96), 8, 96, stg, "stg")
            load_T(vq[:, 32:40], ml_cb[l].rearrange("(c p) -> c p", p=96), 8, 96, stg, "stg")
            load_T(vq[:, 40:48], ml_norm[l].rearrange("(c p) -> c p", p=96), 8, 96, stg, "stg")
            P.dma("sp", rc[:, 1:2], b_ig[l].rearrange("(h o) -> h o", o=1), (), ["rc"])
            P.dma("sp", rc[:, 2:3], b_fg[l].rearrange("(h o) -> h o", o=1), (), ["rc"])
            P.memset(s5st[:], 0.0, ["s5st"]); P.memset(hst[:], 0.0, ["hst"]); P.memset(mlC[:], 0.0, ["mlC"])
            P.memset(mlN[:], 0.0, ["mlN"]); P.memset(mlt[:], 0.0, ["mlt"]); P.memset(ftl[:], 0.0, ["ftl"])
            P.memset(rc[:, 0:1], 0.0, ["rc"])

            L16 = cv.t3(3, 128, parts=16)
            LD = cv.t2(2, parts=16)
            P.dma("sp", L16[:, 0, :], lam_re[l].rearrange("(s g) n -> s (g n)", g=2), (), ["L16"])
            P.dma("sp", L16[:, 1, :], lam_im[l].rearrange("(s g) n -> s (g n)", g=2), (), ["L16"])
            P.dma("sp", LD, log_dt[l].rearrange("(s g) -> s g", g=2), (), ["LD"])
            P.cp(L16[:, 2, :].rearrange("p (g n) -> p g n", n=64), LD.unsqueeze(2).to_broadcast([16, 2, 64]), ["LD"], ["L16"])
            sp = cv.t3(12, 16)
            for i in range(3):
                P.tr(ps[7][:, 0:16], L16[:, i, :], ident[0:16, 0:16], ["L16", "cst"], [PK(7)])
                P.cp(sp[:, i, :], ps[7][:, 0:16], [PK(7)], ["sp"])
            lr, li, dt_ = sp[:, 0, :], sp[:, 1, :], sp[:, 2, :]
            tmpa, tmpb = sp[:, 3, :], sp[:, 4, :]
            cosv, sinv, ar, ai, den, cr, ci = (sp[:, i, :] for i in range(5, 12))
            P.actf(dt_, dt_, AF.Exp, ["sp"], ["sp"])
            P.tt(tmpa, lr, dt_, ALU.mult, ["sp"], ["sp"])
            P.actf(s5p[:, 0, :], tmpa, AF.Exp, ["sp"], ["s5p"])
            P.tt(s5p[:, 1, :], li, dt_, ALU.mult, ["sp"], ["s5p"])
            P.cp(tmpa, s5p[:, 1, :], ["s5p"], ["sp"])
            emit_sin(sinv, tmpa, tmpb, "sp", "sp", "sp")
            P.ts(tmpa, s5p[:, 1, :], math.pi / 2, None, ALU.add, None, ["s5p", "sp"], ["sp"])
            emit_sin(cosv, tmpa, tmpb, "sp", "sp", "sp")
            P.tt(ar, s5p[:, 0, :], cosv, ALU.mult, ["sp", "s5p"], ["sp"])
            P.tt(ai, s5p[:, 0, :], sinv, ALU.mult, ["sp", "s5p"], ["sp"])
            P.tt(den, lr, lr, ALU.mult, ["sp"], ["sp"])
            P.tt(tmpa, li, li, ALU.mult, ["sp"], ["sp"])
            P.tt(den, den, tmpa, ALU.add, ["sp"], ["sp"])
            P.add("dve", lambda e: e.reciprocal(den, den), ["sp"], ["sp"])
            P.ts(ar, ar, -1.0, None, ALU.add, None, ["sp"], ["sp"])
            P.tt(cr, ar, lr, ALU.mult, ["sp"], ["sp"])
            P.tt(tmpa, ai, li, ALU.mult, ["sp"], ["sp"])
            P.tt(cr, cr, tmpa, ALU.add, ["sp"], ["sp"])
            P.tt(cr, cr, den, ALU.mult, ["sp"], ["sp"])
            P.tt(ci, ai, lr, ALU.mult, ["sp"], ["sp"])
            P.tt(tmpa, ar, li, ALU.mult, ["sp"], ["sp"])
            P.tt(ci, ci, tmpa, ALU.subtract, ["sp"], ["sp"])
            P.tt(ci, ci, den, ALU.mult, ["sp"], ["sp"])
            TB = cv.t3(2, 1024); tx = cv.t2(T); tk_ = cv.t2(T)
            for s in range(16):
                tb = TB[:, s % 2, :]
                P.ts(tx, tt_i, s5p[:, 1, s:s + 1], math.pi / 2, ALU.mult, ALU.add, ["cst", "s5p"], ["tx"])
                emit_sin(tb[:, 0:T], tx, tk_, "tx", "tk", "TB%d" % (s % 2))
                P.ts(tx, tt_i, s5p[:, 1, s:s + 1], None, ALU.mult, None, ["cst", "s5p"], ["tx"])
                emit_sin(tb[:, T:2 * T], tx, tk_, "tx", "tk", "TB%d" % (s % 2))
                P.dma("sp", tab_d[l, s], tb, ["TB%d" % (s % 2)], ["tab%d" % s])
            BR = cv.t3(16, 16); BI = cv.t3(16, 16); bbr = cv.t3(16, 16); bbi = cv.t3(16, 16); btmp = cv.t3(16, 16)
            P.dma("sp", BR, b_re[l].rearrange("(s g) n p -> (g n) s p", g=2), (), ["BR"])
            P.dma("sp", BI, b_im[l].rearrange("(s g) n p -> (g n) s p", g=2), (), ["BI"])
            crb = cr.unsqueeze(2).to_broadcast([128, 16, 16]); cib = ci.unsqueeze(2).to_broadcast([128, 16, 16])
            P.tt(bbr, BR, crb, ALU.mult, ["BR", "sp"], ["bbr"])
            P.tt(btmp, BI, cib, ALU.mult, ["BI", "sp"], ["btmp"])
            P.tt(bbr, bbr, btmp, ALU.subtract, ["bbr", "btmp"], ["bbr"])
            P.tt(bbi, BI, crb, ALU.mult, ["BI", "sp"], ["bbi"])
            P.tt(btmp, BR, cib, ALU.mult, ["BR", "sp"], ["btmp"])
            P.tt(bbi, bbi, btmp, ALU.add, ["bbi", "btmp"], ["bbi"])
            CI = cv.t3(2 * 16, 128, parts=16)
            cre = cv.t3(16, 16); cim = cv.t3(16, 16)
            P.dma("sp", CI[:, 0:16, :].rearrange("p s (g n) -> p s g n", g=2), c_re[l].rearrange("(s g) p n -> p s g n", g=2), (), ["CI"])
            P.dma("sp", CI[:, 16:32, :].rearrange("p s (g n) -> p s g n", g=2), c_im[l].rearrange("(s g) p n -> p s g n", g=2), (), ["CI"])
            for s in range(32):
                P.tr(ps[6][:, (s % 16) * 16:(s % 16 + 1) * 16], CI[:, s, :], ident[0:16, 0:16], ["CI", "cst"], [PK(6)])
                if s == 15:
                    P.cp(cre.rearrange("p a b -> p (a b)"), ps[6][:, 0:256], [PK(6)], ["cre"])
                if s == 31:
                    P.ts(cim.rearrange("p a b -> p (a b)"), ps[6][:, 0:256], -1.0, None, ALU.mult, None, [PK(6)], ["cim"])
            Fm = cv.t3(16, 128)
            Fst = cv.t3(4, 128)
            for mi, (src, ksrc, needT) in enumerate([(bbr, "bbr", True), (bbi, "bbi", True), (cre, "cre", False), (cim, "cim", False)]):
                P.memset(Fm, 0.0, ["Fm"])
                F4 = Fm.rearrange("p (a m) r -> p a m r", m=4)
                s4 = src.rearrange("p (a m) q -> p a m q", m=4)
                for m in range(4):
                    for gl in range(2):
                        P.ts(F4[:, :, m, 32 * m + 16 * gl:32 * m + 16 * gl + 16], s4[:, :, m, :],
                             cst[:, C_MG + gl:C_MG + gl + 1], None, ALU.mult, None, [ksrc, "cst"], ["Fm"])
                for s in range(16):
                    if needT:
                        P.tr(ps[s % 2][:, 0:128], Fm[:, s, :], ident, ["Fm", "cst"], [PK(s % 2)])
                        P.cp(Fst[:, s % 4, :], ps[s % 2][:, 0:128], [PK(s % 2)], ["Fst%d" % (s % 4)])
                        P.dma("sp", lbfc_d[l, s, :, 128 * mi:128 * (mi + 1)], Fst[:, s % 4, :], ["Fst%d" % (s % 4)], ["lbfc%d" % s])
                    else:
                        P.dma("sp", lbfc_d[l, s, :, 128 * mi:128 * (mi + 1)], Fm[:, s, :], ["Fm"], ["lbfc%d" % s])
            P.barrier()

            for t in range(NT):
                tok0 = t * T
                if l == 0:
                    cv = Carve()
                    xin = cv.t2(D)
                    for tb in range(4):
                        P.dma("sp", xin, x_d[tok0 + tb * 128:tok0 + (tb + 1) * 128, :], (), ["xin"])
                        for c in range(NCH):
                            P.tr(ps[c % 8][:, 0:128], xin[:, c * 128:(c + 1) * 128], ident, ["xin", "cst"], [PK(c % 8)])
                            P.cp(xt[:, c, tb * 128:(tb + 1) * 128], ps[c % 8][:, 0:128], [PK(c % 8)], ["xt%d" % c],
                                 eng=("act" if c % 2 else "dve"))
                else:
                    P.dma("sp", xt[:].rearrange("p a b -> p (a b)"), xs_d[t], ["xs%d" % t], ["xt%d" % c for c in range(NCH)])
                P.barrier()

                cv = Carve()
                merged = cv.t3(NCH, T)
                mergedr = cv.h3(NCH, T)
                Ybr = cv.h3(8, T)
                abase = cv.o
                SG = Carve(abase).t3(6, T)

                ca_ = Carve(abase)
                sq2 = ca_.t3(2, T); rs = ca_.t2(T)
                rmsnorm(htr, lambda c: vp[:, c:c + 1], sq2, rs, "ht")
                P.barrier()

                def branch(kchunks, first, last_br):
                    gcol0 = {0: 5896, 1: 5896 + D, 2: 5896 + 2 * D}[first[0]]
                    row0 = first[1]
                    for g0, ng in ((0, 6), (6, 6), (12, 4)):
                        def cons_g(i, pap, pk):
                            P.actf(SG[:, i, :], pap, AF.Sigmoid, [pk], ["SG%d" % i])
                        proj(w_in[l], hk(), [(gcol0 + 128 * g0, 128 * ng)], [(0, 128 * i, 128) for i in range(ng)], cons_g)

                        def cons_z(i, pap, pk, g0=g0):
                            j = g0 + i
                            if first[0] == 0:
                                P.tt(merged[:, j, :], SG[:, i, :], pap, ALU.mult, ["SG%d" % i, pk], ["mg%d" % j])
                            else:
                                P.tt(SG[:, i, :], SG[:, i, :], pap, ALU.mult, ["SG%d" % i, pk], ["SG%d" % i])
                                dstm = mergedr if last_br else merged
                                P.tt(dstm[:, j, :], merged[:, j, :], SG[:, i, :], ALU.add, ["SG%d" % i, "mg%d" % j],
                                     ["mgr%d" % j if last_br else "mg%d" % j])
                        kc = [(row0 + r0, nr, rhs, rk) for (r0, nr, rhs, rk) in kchunks]
                        proj(w_br[l], kc, [(128 * g0, 128 * ng)], [(0, 128 * i, 128) for i in range(ng)], cons_z)

                ca_ = Carve(abase)
                U = ca_.t3(4, T); G = ca_.t3(4, T); Gr_ = ca_.h3(4, T)
                CSN = ca_.t3(2, 2 * T); zr = ca_.t2(T); zi = ca_.t2(T); wr = ca_.t2(T); wi = ca_.t2(T)
                t1 = ca_.t2(T); t2_ = ca_.t2(T)
                LF = ca_.t3(2, 512)

                def cons_u(i, pap, pk):
                    P.cp(U[:, i, :], pap, [pk], ["U"], eng="act")
                proj(w_in[l], hk(), [(0, 512)], [(0, 128 * i, 128) for i in range(4)], cons_u, banks=[4, 5, 6, 7])
                for s in range(16):
                    c = s // 4
                    lf = LF[:, s % 2, :]
                    lfk = "LF%d" % (s % 2)
                    P.dma("sp", lf, lbfc_d[l, s], ["lbfc%d" % s], [lfk])
                    cs = CSN[:, s % 2, 0:T]; sn = CSN[:, s % 2, T:2 * T]
                    kcs = "csn%d" % (s % 2)
                    P.dma("sp", CSN[:, s % 2, :], tab_d[l, s], ["tab%d" % s], [kcs])
                    bre, bim = ps[4 + 2 * (s % 2)], ps[5 + 2 * (s % 2)]
                    kre, kim = PK(4 + 2 * (s % 2)), PK(5 + 2 * (s % 2))
                    P.mm(bre[:, :], lf[:, 0:128], U[:, c, :], True, True, [lfk, "U"], [kre])
                    P.mm(bim[:, :], lf[:, 128:256], U[:, c, :], True, True, [lfk, "U"], [kim])
                    P.tt(t1, bre[:, :], cs, ALU.mult, [kre, kcs], ["t1"])
                    P.tt(t2_, bim[:, :], sn, ALU.mult, [kim, kcs], ["t2"])
                    P.tt(zr, t1, t2_, ALU.add, ["t1", "t2"], ["zr"])
                    P.tt(t1, bim[:, :], cs, ALU.mult, [kim, kcs], ["t1"])
                    P.tt(t2_, bre[:, :], sn, ALU.mult, [kre, kcs], ["t2"])
                    P.tt(zi, t1, t2_, ALU.subtract, ["t1", "t2"], ["zi"])
                    magb = s5p[:, 0, s:s + 1].to_broadcast([128, T])
                    P.scan(wr, magb, zr, s5st[:, 0, s:s + 1], ALU.mult, ALU.add, ["zr", "s5p", "s5st"], ["wr"])
                    P.scan(wi, magb, zi, s5st[:, 1, s:s + 1], ALU.mult, ALU.add, ["zi", "s5p", "s5st"], ["wi"])
                    P.tt(t1, wr, cs, ALU.mult, ["wr", kcs], ["t1"])
                    P.tt(t2_, wi, sn, ALU.mult, ["wi", kcs], ["t2"])
                    P.tt(zr, t1, t2_, ALU.subtract, ["t1", "t2"], ["zr"])
                    P.tt(t1, wr, sn, ALU.mult, ["wr", kcs], ["t1"])
                    P.tt(t2_, wi, cs, ALU.mult, ["wi", kcs], ["t2"])
                    P.tt(zi, t1, t2_, ALU.add, ["t1", "t2"], ["zi"])
                    P.cp(s5st[:, 0, s:s + 1], zr[:, T - 1:T], ["zr"], ["s5st"])
                    P.cp(s5st[:, 1, s:s + 1], zi[:, T - 1:T], ["zi"], ["s5st"])
                    P.mm(ps[c][:, :], lf[:, 256:384], zr, s % 4 == 0, False, [lfk, "zr"], [PK(c)])
                    P.mm(ps[c][:, :], lf[:, 384:512], zi, False, s % 4 == 3, [lfk, "zi"], [PK(c)])
                for c in range(4):
                    P.stt(t1, U[:, c, :], vp[:, 32 + c:33 + c], ps[c][:, :], ALU.mult, ALU.add, ["U", "vp", PK(c)], ["t1"])
                    P.tt(t2_, t1, t1, ALU.mult, ["t1"], ["t2"])
                    P.ts(t2_, t2_, 0.044715, 1.0, ALU.mult, ALU.add, ["t2"], ["t2"])
                    P.tt(t2_, t2_, t1, ALU.mult, ["t1", "t2"], ["t2"])
                    P.actf(t2_, t2_, AF.Sigmoid, ["t2"], ["t2"], scale=2.0 * GELU_C)
                    P.tt(G[:, c, :], t1, t2_, ALU.mult, ["t1", "t2"], ["G"])
                    P.cp(Gr_[:, c, :], G[:, c, :], ["G"], ["Gr"], eng="act")

                def cons_glu(i, pap, pk):
                    P.actf(t1, pap, AF.Sigmoid, [pk], ["t1"], bias=vp[:, 36 + i:37 + i], scale=1.0)
                    P.tt(Ybr[:, i, :], G[:, i, :], t1, ALU.mult, ["G", "t1"], ["Y"])
                proj(w_glu[l], [(128 * c, 128, Gr_[:, c, :], "Gr") for c in range(4)], [(0, 512)],
                     [(0, 128 * i, 128) for i in range(4)], cons_glu)
                P.barrier()
                branch([(128 * c, 128, Ybr[:, c, :], "Y") for c in range(4)], (0, 0), False)
                P.barrier()

                for half in range(2):
                    ca_ = Carve(abase)
                    Qb = ca_.t3(3, T); Fb = ca_.t3(3, T); Vb = ca_.t3(3, T); OGb = ca_.t3(3, T)
                    T1 = ca_.t2(T); T2 = ca_.t2(T); CM = ca_.t2(T); D3 = ca_.t2(T)
                    A = ca_.t2(T); Ash = ca_.t2(T); Bm = ca_.t2(T); Kd = ca_.t2(T)
                    VT = ca_.t2(128, parts=64); KT = ca_.t2(128, parts=64); SM = ca_.t2(64, parts=64)
                    EL = ca_.t2(8)

                    def cons1(i, pap, pk):
                        if i < 3:
                            P.actf(Qb[:, i, :], pap, AF.Silu, [pk], ["Qb%d" % i])
                        else:
                            P.actf(Fb[:, i - 3, :], pap, AF.Sigmoid, [pk], ["Fb%d" % (i - 3)])
                    proj(w_in[l], hk(), [(512 + 384 * half, 384), (1280 + 384 * half, 384)],
                         [(0, 0, 128), (0, 128, 128), (0, 256, 128), (1, 0, 128), (1, 128, 128), (1, 256, 128)], cons1)

                    def cons2(i, pap, pk):
                        if i < 3:
                            P.cp(Vb[:, i, :], pap, [pk], ["Vb%d" % i], eng="act")
                        else:
                            P.actf(OGb[:, i - 3, :], pap, AF.Silu, [pk], ["OGb%d" % (i - 3)])
                    proj(w_in[l], hk(), [(2048 + 384 * half, 384), (2816 + 384 * half, 384)],
                         [(0, 0, 128), (0, 128, 128), (0, 256, 128), (1, 0, 128), (1, 128, 128), (1, 256, 128)], cons2)
                    for hh in range(3):
                        h = 3 * half + hh
                        q = Qb[:, hh, :]; f = Fb[:, hh, :]; v = Vb[:, hh, :]; og = OGb[:, hh, :]
                        kq, kf, kv, ko = "Qb%d" % hh, "Fb%d" % hh, "Vb%d" % hh, "OGb%d" % hh
                        P.ts(f, f, lbs[:, 24 + l * 6 + h:24 + l * 6 + h + 1], lbs[:, l * 6 + h:l * 6 + h + 1], ALU.mult, ALU.add, [kf, "lbs"], [kf])
                        P.actf(T1, f, AF.Ln, [kf], ["T1"])
                        P.ts(f, f, -1.0, 1.0, ALU.mult, ALU.add, [kf], [kf])
                        P.scan(T2, ones[:, 0:1].to_broadcast([128, T]), T1, 0.0, ALU.mult, ALU.add, ["T1", "cst"], ["T2"])
                        T23 = T2.rearrange("p (a b) -> p a b", b=64); CM3 = CM.rearrange("p (a b) -> p a b", b=64)
                        P.cp(CM3[:, 0, :], T23[:, 0, :], ["T2"], ["CM"])
                        P.tt(CM3[:, 1:8, :], T23[:, 1:8, :], T23[:, 0:7, 63:64].to_broadcast([128, 7, 64]), ALU.subtract, ["T2"], ["CM"])
                        lastb = CM3[:, :, 63:64].to_broadcast([128, 8, 64])
                        D33 = D3.rearrange("p (a b) -> p a b", b=64)
                        P.stt(D33, lastb, -0.5, CM3, ALU.mult, ALU.add, ["CM"], ["D3"])
                        P.actf(T1, CM, AF.Exp, ["CM"], ["T1"])
                        P.tt(A, q, T1, ALU.mult, [kq, "T1"], ["A"])
                        P.actf(T1, D3, AF.Exp, ["D3"], ["T1"])
                        P.tt(Ash, q, T1, ALU.mult, [kq, "T1"], ["Ash"])
                        P.actf(T2, D3, AF.Exp, ["D3"], ["T2"], scale=-1.0)
                        P.tt(Bm, f, T2, ALU.mult, [kf, "T2"], ["Bm"])
                        P.tt(D33, lastb, CM3, ALU.subtract, ["CM"], ["D3"])
                        P.actf(T1, D3, AF.Exp, ["D3"], ["T1"])
                        P.tt(Kd, f, T1, ALU.mult, [kf, "T1"], ["Kd"])
                        P.actf(EL, CM3[:, :, 63], AF.Exp, ["CM"], ["EL"])
                        S = hst[:, h, :]
                        ks = "hst%d" % h
                        for c in range(8):
                            cols = slice(64 * c, 64 * (c + 1))
                            P.mm(ps[0][0:64, 0:64], Bm[:, cols], Ash[:, cols], True, True, ["Bm", "Ash"], [PK(0)])
                            P.tt(SM, ps[0][0:64, 0:64], m64, ALU.mult, [PK(0), "cst"], ["SM"])
                            P.tr(ps[1][0:64, 0:128], v[:, cols], ident, [kv, "cst"], [PK(1)])
                            P.cp(VT, ps[1][0:64, 0:128], [PK(1)], ["VT"], eng="act")
                            P.tr(ps[2][0:64, 0:128], Kd[:, cols], ident, ["Kd", "cst"], [PK(2)])
                            P.cp(KT, ps[2][0:64, 0:128], [PK(2)], ["KT"], eng="act")
                            P.mm(ps[3][:, cols], VT, SM, True, False, ["VT", "SM"], [PK(3)])
                            P.mm(ps[3][:, cols], S, A[:, cols], False, True, [ks, "A"], [PK(3)])
                            P.mm(ps[4][:, 0:128], KT, VT, True, True, ["KT", "VT"], [PK(4)])
                            P.stt(S, S, EL[:, c:c + 1], ps[4][:, 0:128], ALU.mult, ALU.add, [ks, "EL", PK(4)], [ks])
                        P.cp(T1, ps[3][:, :], [PK(3)], ["T1"])
                        P.actf(T2, T1, AF.Square, ["T1"], ["T2"])
                        P.mm(ps[5][:, :], ones, T2, True, True, ["T2", "cst"], [PK(5)])
                        P.actf(T2, ps[5][:, :], AF.Sqrt, [PK(5)], ["T2"], bias=epsc, scale=1.0 / 128)
                        P.add("dve", lambda e, T2=T2: e.reciprocal(T2, T2), ["T2"], ["T2"])
                        P.stt(T1, T1, vp[:, 40 + h:41 + h], T2, ALU.mult, ALU.mult, ["T1", "T2", "vp"], ["T1"])
                        P.tt(Ybr[:, h, :], T1, og, ALU.mult, ["T1", ko], ["Y"])
                P.barrier()
                branch([(128 * c, 128, Ybr[:, c, :], "Y") for c in range(6)], (1, 512), False)
                P.barrier()

                ca_ = Carve(abase)
                IG = ca_.t2(T, parts=4); LFr = ca_.t2(T, parts=4); Bc = ca_.t2(T, parts=4); Rr = ca_.t2(T, parts=4)
                AC = ca_.t3(4, 4); CS = ca_.t2(4, parts=4); DEC = ca_.t2(4, parts=4); DB = ca_.t2(4, parts=96)
                CXs = ca_.t3(2, 515, parts=96); Vh = ca_.t3(2, T, parts=96); OGs = ca_.t3(2, T, parts=96)
                acc = ca_.t3(2, T, parts=96); CAr = ca_.h3(2, T, parts=96)
                Qm = ca_.t3(2, T, parts=96); Km = ca_.t3(2, T, parts=96); HS = ca_.t3(2, T, parts=96)
                DS = ca_.t2(T, parts=96); DT2 = ca_.t2(T, parts=96)
                PT = ca_.t2(128); VTm = ca_.t2(192); KTa = ca_.t2(192); CTm = ca_.t2(384, parts=96)
                Ycr = Ybr[0:96, :, :]

                def cons_g4(i, pap, pk):
                    if i == 0:
                        P.actf(IG, pap, AF.Identity, [pk], ["IG"], bias=rc[:, 1:2], scale=1.0)
                    else:
                        P.actf(LFr, pap, AF.Sigmoid, [pk], ["LFr"], bias=rc[:, 2:3], scale=1.0)
                        P.actf(LFr, LFr, AF.Ln, ["LFr"], ["LFr"])
                proj(w_in[l], hk(), [(5888, 8)], [(0, 0, 4), (0, 4, 4)], cons_g4)
                ones4 = ones[0:4, 0:1].to_broadcast([4, T])
                P.scan(Bc, ones4, LFr, 0.0, ALU.mult, ALU.add, ["LFr", "cst"], ["Bc"])
                P.tt(IG, IG, Bc, ALU.subtract, ["IG", "Bc"], ["IG"])
                P.scan(Rr, ones4, IG, rc[:, 0:1], ALU.mult, ALU.max, ["IG", "rc", "cst"], ["Rr"])
                R3 = Rr.rearrange("p (a b) -> p a b", b=128)
                P.cp(CS[:, 0:1], rc[:, 0:1], ["rc"], ["CS"])
                P.cp(CS[:, 1:4], R3[:, 0:3, 127], ["Rr"], ["CS"])
                P.tt(DEC, CS, R3[:, :, 127], ALU.subtract, ["CS", "Rr"], ["DEC"])
                P.actf(DEC, DEC, AF.Exp, ["DEC"], ["DEC"])
                csb = CS.unsqueeze(2).to_broadcast([4, 4, 128])
                P.tt(LFr.rearrange("p (a b) -> p a b", b=128), IG.rearrange("p (a b) -> p a b", b=128), csb, ALU.subtract, ["IG", "CS"], ["LFr"])
                P.actf(LFr, LFr, AF.Exp, ["LFr"], ["LFr"])
                P.tt(IG.rearrange("p (a b) -> p a b", b=128), Bc.rearrange("p (a b) -> p a b", b=128), csb, ALU.add, ["Bc", "CS", "IG"], ["IG"])
                P.actf(IG, IG, AF.Exp, ["IG"], ["IG"], scale=-1.0)
                P.tt(rc[:, 0:1], Bc[:, T - 1:T], Rr[:, T - 1:T], ALU.add, ["Bc", "Rr"], ["rc"])
                for ch in range(4):
                    P.mm(ps[7][:, 4 * ch:4 * ch + 4], LFr[:, 128 * ch:128 * (ch + 1)], ident[0:4, 0:4], True, True, ["LFr", "cst"], [PK(7)])
                P.cp(AC.rearrange("p a b -> p (a b)"), ps[7][:, 0:16], [PK(7)], ["AC"])
                for h in range(4):
                    def cons_m(i, pap, pk, h=h):
                        j = i % 2
                        if i < 2:
                            P.cp(CXs[:, j, 3:515], pap, [pk], ["CXs%d" % j], eng="act")
                        elif i < 4:
                            P.cp(Vh[:, j, :], pap, [pk], ["Vh"], eng="act")
                        else:
                            P.actf(OGs[:, j, :], pap, AF.Sigmoid, [pk], ["OGs"])
                    proj(w_in[l], hk(), [(3584 + 192 * h, 192), (4352 + 192 * h, 192), (5120 + 192 * h, 192)],
                         [(0, 0, 96), (0, 96, 96), (1, 0, 96), (1, 96, 96), (2, 0, 96), (2, 96, 96)], cons_m)
                    for j in range(2):
                        fj = 2 * h + j
                        P.cp(CXs[:, j, 0:3], mlt[:, fj, :], ["mlt"], ["CXs%d" % j])
                        P.actf(acc[:, j, :], CXs[:, j, 3:515], AF.Identity, ["CXs%d" % j, "vq"], ["acc"],
                               bias=vq[:, 32 + fj:33 + fj], scale=vq[:, 24 + fj:25 + fj])
                        for k in range(3):
                            P.stt(acc[:, j, :], CXs[:, j, k:k + T], vq[:, 8 * k + fj:8 * k + fj + 1], acc[:, j, :], ALU.mult, ALU.add,
                                  ["CXs%d" % j, "vq", "acc"], ["acc"])
                        P.cp(mlt[:, fj, :], CXs[:, j, 512:515], ["CXs%d" % j], ["mlt"])
                        P.actf(CAr[:, j, :], acc[:, j, :], AF.Silu, ["acc"], ["CA"])

                    def cons_qk(i, pap, pk):
                        if i < 2:
                            P.cp(Qm[:, i, :], pap, [pk], ["Qm"], eng="act")
                        else:
                            P.ts(Km[:, i - 2, :], pap, 192.0 ** -0.5, None, ALU.mult, None, [pk], ["Km"])
                    proj(w_qk[l, h], [(0, 96, CAr[:, 0, :], "CA"), (96, 96, CAr[:, 1, :], "CA")], [(0, 384)],
                         [(0, 96 * i, 96) for i in range(4)], cons_qk)
                    P.mm(ps[7][0:96, 0:4], sel4(h), DEC, True, True, ["DEC", "cst"], [PK(7)])
                    P.cp(DB, ps[7][0:96, 0:4], [PK(7)], ["DB"])
                    kC, kN = "mlC%d" % h, "mlN%d" % h
                    for ch in range(4):
                        cols = slice(128 * ch, 128 * (ch + 1))
                        P.mm(ps[0][:, 0:128], Km[:, 0, cols], Qm[:, 0, cols], True, False, ["Km", "Qm"], [PK(0)])
                        P.mm(ps[0][:, 0:128], Km[:, 1, cols], Qm[:, 1, cols], False, True, ["Km", "Qm"], [PK(0)])
                        P.stt(PT, ps[0][:, 0:128], AC[:, ch, h:h + 1], m128, ALU.mult, ALU.mult, [PK(0), "AC", "cst"], ["PT"])
                        for j in range(2):
                            P.tr(ps[1][:, 96 * j:96 * (j + 1)], Vh[:, j, cols], ident[0:96, 0:96], ["Vh", "cst"], [PK(1)])
                        P.cp(VTm, ps[1][:, 0:192], [PK(1)], ["VTm"], eng="act")
                        for j in range(2):
                            P.tr(ps[1][:, 192 + 96 * j:192 + 96 * (j + 1)], Km[:, j, cols], ident[0:96, 0:96], ["Km", "cst"], [PK(1)])
                        P.ts(KTa, ps[1][:, 192:384], AC[:, ch, h:h + 1], None, ALU.mult, None, [PK(1), "AC"], ["KTa"])
                        for j in range(2):
                            pn = ps[2 + j]
                            P.mm(pn[0:96, cols], VTm[:, 96 * j:96 * (j + 1)], PT, True, False, ["VTm", "PT"], [PK(2 + j)])
                            P.mm(pn[0:96, cols], mlC[:, h, 0, 96 * j:96 * (j + 1)], Qm[:, 0, cols], False, False, [kC, "Qm"], [PK(2 + j)])
                            P.mm(pn[0:96, cols], mlC[:, h, 1, 96 * j:96 * (j + 1)], Qm[:, 1, cols], False, True, [kC, "Qm"], [PK(2 + j)])
                        P.mm(ps[4][0:96, cols], ones[:, 0:96], PT, True, False, ["cst", "PT"], [PK(4)])
                        P.mm(ps[4][0:96, cols], mlN[:, h, 0, :], Qm[:, 0, cols], False, False, [kN, "Qm"], [PK(4)])
                        P.mm(ps[4][0:96, cols], mlN[:, h, 1, :], Qm[:, 1, cols], False, True, [kN, "Qm"], [PK(4)])
                        for kt in range(2):
                            P.mm(ps[5][0:96, 192 * kt:192 * (kt + 1)], KTa[:, 96 * kt:96 * (kt + 1)], VTm, True, True, ["KTa", "VTm"], [PK(5)])
                            P.mm(ps[6][0:96, 96 * kt:96 * (kt + 1)], KTa[:, 96 * kt:96 * (kt + 1)], ones[:, 0:96], True, True, ["KTa", "cst"], [PK(6)])
                        Cf = mlC[:, h, :, :].rearrange("p a b -> p (a b)")
                        Nf = mlN[:, h, :, :].rearrange("p a b -> p (a b)")
                        P.tt(CTm, ps[5][0:96, 0:384], Cf, ALU.add, [PK(5), kC], ["CTm"])
                        P.ts(Cf, CTm, DB[:, ch:ch + 1], None, ALU.mult, None, ["CTm", "DB"], [kC])
                        P.tt(CTm[:, 0:192], ps[6][0:96, 0:192], Nf, ALU.add, [PK(6), kN], ["CTm"])
                        P.ts(Nf, CTm[:, 0:192], DB[:, ch:ch + 1], None, ALU.mult, None, ["CTm", "DB"], [kN])
                    P.mm(ps[7][0:96, :], sel4(h), IG, True, True, ["IG", "cst"], [PK(7)])
                    P.cp(DS, ps[4][0:96, :], [PK(4)], ["DS"])
                    P.stt(DT2, DS, -1.0, DS, ALU.mult, ALU.max, ["DS"], ["DT2"])
                    P.tt(DT2, DT2, ps[7][0:96, :], ALU.max, ["DT2", PK(7)], ["DT2"])
                    P.add("dve", lambda e, DT2=DT2: e.reciprocal(DT2, DT2), ["DT2"], ["DT2"])
                    for j in range(2):
                        P.tt(HS[:, j, :], ps[2 + j][0:96, :], DT2, ALU.mult, [PK(2 + j), "DT2"], ["HS"])
                        P.actf(acc[:, j, :], HS[:, j, :], AF.Square, ["HS"], ["acc"])
                        P.mm(ps[7][0:96, :], ones[0:96, 0:96], acc[:, j, :], j == 0, j == 1, ["acc", "cst"], [PK(7)])
                    P.actf(DS, ps[7][0:96, :], AF.Sqrt, [PK(7)], ["DS"], bias=epsc[0:96, :], scale=1.0 / 192)
                    P.add("dve", lambda e, DS=DS: e.reciprocal(DS, DS), ["DS"], ["DS"])
                    for j in range(2):
                        fj = 2 * h + j
                        P.stt(HS[:, j, :], HS[:, j, :], vq[:, 40 + fj:41 + fj], DS, ALU.mult, ALU.mult, ["HS", "DS", "vq"], ["HS"])
                        P.tt(Ycr[:, fj, :], HS[:, j, :], OGs[:, j, :], ALU.mult, ["HS", "OGs"], ["Y"])
                P.barrier()
                branch([(96 * i, 96, Ycr[:, i, :], "Y") for i in range(8)], (2, 1280), True)

                for g0, ng in ((0, 6), (6, 6), (12, 4)):
                    def cons_o(i, pap, pk, g0=g0):
                        j = g0 + i
                        P.tt(xt[:, j, :], xt[:, j, :], pap, ALU.add, ["xt%d" % j, pk], ["xt%d" % j])
                    proj(w_out[l], [(128 * c, 128, mergedr[:, c, :], "mgr%d" % c) for c in range(NCH)],
                         [(128 * g0, 128 * ng)], [(0, 128 * i, 128) for i in range(ng)], cons_o)
                P.barrier()

                cv = Carve()
                sq2 = cv.t3(2, T); rs = cv.t2(T)
                rmsnorm(htr, lambda c: vp[:, 16 + c:17 + c], sq2, rs, "ht")
                ringB = cv.h3(6, D)
                actgr = cv.h3(12, T)
                SA = cv.t3(6, T)
                ST = cv.t3(2, 514); accB = cv.t2(T)
                gi = 0
                g0 = 0
                while g0 < 44:
                    ng = min(6, 44 - g0)
                    ab = (gi % 2) * 6

                    def conv_tile(pap, pk, cidx, dst, kd, par):
                        st = ST[:, par, :]
                        kst = "ST%d" % par
                        P.cp(st[:, 2:514], pap, [pk], [kst], eng="act")
                        P.cp(st[:, 0:2], ftl[:, cidx, :], ["ftl"], [kst])
                        P.actf(dst, st[:, 2:514], AF.Identity, [kst, "vp"], [kd],
                               bias=vp[:, 46 + cidx:47 + cidx], scale=vp[:, 134 + 88 * 2 + cidx:135 + 88 * 2 + cidx])
                        for k in range(2):
                            P.stt(dst, st[:, k:k + T], vp[:, 134 + 88 * k + cidx:135 + 88 * k + cidx], dst, ALU.mult, ALU.add, [kst, "vp", kd], [kd])
                        P.cp(ftl[:, cidx, :], st[:, 512:514], [kst], ["ftl"])

                    def cons_a(i, pap, pk, g0=g0):
                        conv_tile(pap, pk, g0 + i, SA[:, i, :], "SA%d" % i, i % 2)
                        P.actf(SA[:, i, :], SA[:, i, :], AF.Silu, ["SA%d" % i], ["SA%d" % i])

                    def cons_b(i, pap, pk, g0=g0, ab=ab):
                        conv_tile(pap, pk, 44 + g0 + i, accB, "accB", i % 2)
                        P.tt(actgr[:, ab + i, :], SA[:, i, :], accB, ALU.mult, ["SA%d" % i, "accB"], ["actg%d" % (ab + i)])
                    tl = [(0, 128 * ii, 128) for ii in range(ng)]
                    proj(w_up[l], hk(), [(128 * g0, 128 * ng)], tl, cons_a)
                    proj(w_up[l], hk(), [(FFN + 128 * g0, 128 * ng)], tl, cons_b)
                    for ii in range(ng):
                        P.dma("pool", ringB[:, ii, :], w_dn[l, 128 * (g0 + ii):128 * (g0 + ii + 1), :], (), ["ringB%d" % ii])
                    for j in range(NCH):
                        b = 6 + (j % 2)
                        for ii in range(ng):
                            P.mm(ps[b][:, :], ringB[:, ii, 128 * j:128 * (j + 1)], actgr[:, ab + ii, :], ii == 0, ii == ng - 1,
                                 ["ringB%d" % ii, "actg%d" % (ab + ii)], [PK(b)])
                        P.tt(xt[:, j, :], xt[:, j, :], ps[b][:, :], ALU.add, ["xt%d" % j, PK(b)], ["xt%d" % j])
                    g0 += ng
                    gi += 1
                P.barrier()

                if l < NL - 1:
                    P.dma("sp", xs_d[t], xt[:].rearrange("p a b -> p (a b)"), ["xt%d" % c for c in range(NCH)], ["xs%d" % t])
                else:
                    cv = Carve()
                    sq2 = cv.t3(2, T); rs = cv.t2(T)
                    xo = cv.t2(D)
                    hf = cv.t3(NCH, T)
                    rmsnorm(hf, lambda c: vp[:, 398 + c:399 + c], sq2, rs, "hf")
                    for tb in range(4):
                        for c in range(NCH):
                            P.tr(ps[c % 8][:, 0:128], hf[:, c, tb * 128:(tb + 1) * 128], ident, ["hf", "cst"], [PK(c % 8)])
                            P.cp(xo[:, c * 128:(c + 1) * 128], ps[c % 8][:, 0:128], [PK(c % 8)], ["xo"], eng=("act" if c % 2 else "dve"))
                        P.dma("sp", y_d[tok0 + tb * 128:tok0 + (tb + 1) * 128, :], xo, ["xo"], ["y"])
                P.barrier()
        P.emit()
    return nc


WNAMES = ["mix_norm", "w_in", "s5_lam_re", "s5_lam_im", "s5_log_dt", "s5_b_re", "s5_b_im", "s5_c_re", "s5_c_im",
          "s5_d", "s5_w_glu", "s5_b_glu", "hg_lower_bounds", "hg_norm", "ml_conv_w", "ml_conv_b", "ml_w_qk",
          "ml_b_ig", "ml_b_fg", "ml_norm", "w_branch", "w_out", "ffn_norm", "ffn_w_up", "ffn_conv_w", "ffn_conv_b",
          "ffn_w_down", "final_norm"]


def run(inputs, NL, NT, ncores):
    nc = build(NL, NT)
    x = np.asarray(inputs["x"], np.float32)
    B = x.shape[0]
    cstv = make_consts()
    shared = {k: np.ascontiguousarray(np.asarray(inputs[k], np.float32)) for k in WNAMES}
    in_maps = []
    for c in range(ncores):
        m = dict(shared)
        m["x"] = np.ascontiguousarray(x[c % B])
        m["cst"] = cstv
        in_maps.append(m)
    res = run_bass_kernel_spmd(nc, in_maps, core_ids=list(range(ncores)))
    return np.stack([res.results[b]["y"] for b in range(B)], axis=0).astype(np.float32)


def kernel(**inputs):
    return run(inputs, 4, 8, 8)
```

```python
import numpy as np
from contextlib import ExitStack
import concourse.bass as bass
import concourse.mybir as mybir

F32 = mybir.dt.float32
F32R = mybir.dt.float32r
BF = mybir.dt.bfloat16
ALU = mybir.AluOpType
AF = mybir.ActivationFunctionType
AX = mybir.AxisListType

ENGS = ("pe", "act", "dve", "pool", "sp")
NDSEM = 24


class Op:
    __slots__ = ("eng", "fn", "waits", "done", "dma", "idx")


class Prog:
    def __init__(self, nc, es):
        self.nc = nc
        self.ops = {e: [] for e in ENGS}
        self.esem = {e: es.enter_context(nc.semaphore("sem_" + e)) for e in ENGS}
        self.ecnt = {e: 0 for e in ENGS}
        self.dsem = {q: [es.enter_context(nc.semaphore("dq_%s_%d" % (q, i))) for i in range(NDSEM)]
                     for q in ("sp", "pool", "act")}
        self.dcnt = {q: [0] * NDSEM for q in ("sp", "pool", "act")}
        self.dnext = {q: 0 for q in ("sp", "pool", "act")}
        self.waited = {e: {} for e in ENGS}
        self.lastw = {}
        self.readers = {}
        self.pending = {e: [] for e in ENGS}
        self.nops = 0

    def _need(self, eng, waits, dep):
        sem, val, deng, ddma = dep
        if (not ddma) and deng == eng and eng == "pe":
            return
        key = id(sem)
        if self.waited[eng].get(key, 0) >= val:
            return
        waits[key] = (sem, max(val, waits.get(key, (sem, 0))[1]))

    def add(self, eng, fn, reads=(), writes=(), dma=False):
        op = Op()
        op.eng = eng
        op.fn = fn
        op.dma = dma
        waits = {}
        for k in reads:
            w = self.lastw.get(k)
            if w is not None:
                self._need(eng, waits, w)
        for k in writes:
            w = self.lastw.get(k)
            if w is not None:
                self._need(eng, waits, w)
            for r in self.readers.get(k, ()):
                self._need(eng, waits, r)
        for dep in self.pending[eng]:
            self._need(eng, waits, dep)
        self.pending[eng] = []
        if dma:
            q = eng
            i = self.dnext[q]
            self.dnext[q] = (i + 1) % NDSEM
            sem = self.dsem[q][i]
            if self.dcnt[q][i] > 0:
                self._need(eng, waits, (sem, self.dcnt[q][i], eng, True))
            self.dcnt[q][i] += 16
            op.done = (sem, self.dcnt[q][i], eng, True)
        else:
            self.ecnt[eng] += 1
            op.done = (self.esem[eng], self.ecnt[eng], eng, False)
        op.waits = list(waits.values())
        for sem, val in op.waits:
            self.waited[eng][id(sem)] = val
        for k in reads:
            self.readers.setdefault(k, []).append(op.done)
        for k in writes:
            self.lastw[k] = op.done
            self.readers[k] = []
        self.ops[eng].append(op)
        self.nops += 1
        return op

    def barrier(self):
        deps = []
        for e in ENGS:
            if self.ecnt[e] > 0:
                deps.append((self.esem[e], self.ecnt[e], e, True))
        for q in self.dsem:
            for i in range(NDSEM):
                if self.dcnt[q][i] > 0:
                    deps.append((self.dsem[q][i], self.dcnt[q][i], q, True))
        for e in ENGS:
            self.pending[e] = list(deps)
        self.lastw = {}
        self.readers = {}

    def emit(self, final_waits_eng="sp"):
        nc = self.nc
        self.barrier()
        fin = self.pending[final_waits_eng]
        with nc.Block() as block:
            def run(e, eng):
                for op in self.ops[e]:
                    for sem, val in op.waits:
                        eng.wait_ge(sem, val)
                    ins = op.fn(eng)
                    sem, val, _, ddma = op.done
                    ins.then_inc(sem, 16 if ddma else 1)
                if e == final_waits_eng:
                    w = {}
                    for sem, val, _, _ in fin:
                        if self.waited[e].get(id(sem), 0) < val:
                            w[id(sem)] = (sem, max(val, w.get(id(sem), (sem, 0))[1]))
                    for sem, val in w.values():
                        eng.wait_ge(sem, val)

            @block.tensor
            def _(eng):
                run("pe", eng)

            @block.scalar
            def _(eng):
                run("act", eng)

            @block.vector
            def _(eng):
                run("dve", eng)

            @block.gpsimd
            def _(eng):
                run("pool", eng)

            @block.sync
            def _(eng):
                run("sp", eng)

    def mm(self, out, lhsT, rhs, start, stop, r, w):
        return self.add("pe", lambda e: e.matmul(out, lhsT, rhs, start=start, stop=stop), r, w)

    def tr(self, out, in_, ident, r, w):
        return self.add("pe", lambda e: e.transpose(out, in_, ident), r, w)

    def actf(self, out, in_, func, r, w, bias=None, scale=None):
        kw = {}
        if bias is not None:
            kw["bias"] = bias
        if scale is not None:
            kw["scale"] = scale
        return self.add("act", lambda e: e.activation(out, in_, func, **kw), r, w)

    def tt(self, out, a, b, op, r, w, eng="dve"):
        return self.add(eng, lambda e: e.tensor_tensor(out, a, b, op), r, w)

    def ts(self, out, a, s1, s2, op0, op1, r, w, eng="dve"):
        if op1 is None:
            return self.add(eng, lambda e: e.tensor_scalar(out, a, s1, None, op0), r, w)
        return self.add(eng, lambda e: e.tensor_scalar(out, a, s1, s2, op0, op1), r, w)

    def stt(self, out, a, s, b, op0, op1, r, w, eng="dve"):
        return self.add(eng, lambda e: e.scalar_tensor_tensor(out, a, s, b, op0, op1), r, w)

    def cp(self, out, a, r, w, eng="dve"):
        if eng == "act":
            return self.add("act", lambda e: e.copy(out, a), r, w)
        return self.add(eng, lambda e: e.tensor_copy(out, a), r, w)

    def scan(self, out, d0, d1, init, op0, op1, r, w):
        return self.add("dve", lambda e: e.tensor_tensor_scan(out, d0, d1, init, op0, op1), r, w)

    def memset(self, ap, val, w, eng="dve"):
        return self.add(eng, lambda e: e.memset(ap, val), (), w)

    def dma(self, q, out, in_, r, w):
        return self.add(q, lambda e: e.dma_start(out=out, in_=in_), r, w, dma=True)


import math
from concourse.bass_utils import run_bass_kernel_spmd

T = 512
D = 2048
NCH = 16
EPS = 1e-6
MAGIC = 12582912.0
TWO_PI = 2.0 * math.pi
GELU_C = math.sqrt(2.0 / math.pi)
FFN = 5632
IN_TOTAL = 12040
C_IDENT, C_ONES, C_M128, C_M64, C_TT, C_MG, C_EPS, C_ZERO, C_SEL = 0, 128, 256, 384, 448, 960, 962, 963, 964
NCST = 964 + 384
RW = 768
NRING = 8
RCOLS = 26624


def make_consts():
    c = np.zeros((128, NCST), np.float32)
    c[:, C_IDENT:C_IDENT + 128] = np.eye(128)
    c[:, C_ONES:C_ONES + 128] = 1.0
    c[:, C_M128:C_M128 + 128] = np.triu(np.ones((128, 128)))
    c[:64, C_M64:C_M64 + 64] = np.triu(np.ones((64, 64)))
    c[:, C_TT:C_TT + 512] = np.arange(1, 513)[None, :]
    c[:64, C_MG] = 1.0
    c[64:, C_MG + 1] = 1.0
    c[:, C_EPS] = EPS
    for h in range(4):
        c[h, C_SEL + h * 96:C_SEL + (h + 1) * 96] = 1.0
    return c


def build(NL, NT):
    nc = bass.Bass("TRN2", target_bir_lowering=False)
    es = ExitStack()

    def din(name, shape):
        return nc.dram_tensor(name, list(shape), F32, kind="ExternalInput").ap()

    x_d = din("x", [NT * T, D])
    cst_d = din("cst", [128, NCST])
    mix_norm = din("mix_norm", [NL, D]); w_in = din("w_in", [NL, D, IN_TOTAL])
    lam_re = din("s5_lam_re", [NL, 32, 64]); lam_im = din("s5_lam_im", [NL, 32, 64]); log_dt = din("s5_log_dt", [NL, 32])
    b_re = din("s5_b_re", [NL, 32, 64, 16]); b_im = din("s5_b_im", [NL, 32, 64, 16])
    c_re = din("s5_c_re", [NL, 32, 16, 64]); c_im = din("s5_c_im", [NL, 32, 16, 64])
    s5_d = din("s5_d", [NL, 512]); w_glu = din("s5_w_glu", [NL, 512, 512]); b_glu = din("s5_b_glu", [NL, 512])
    hg_lb = din("hg_lower_bounds", [NL, 768]); hg_norm = din("hg_norm", [NL, 768])
    ml_cw = din("ml_conv_w", [NL, 4, 768]); ml_cb = din("ml_conv_b", [NL, 768]); w_qk = din("ml_w_qk", [NL, 4, 192, 384])
    b_ig = din("ml_b_ig", [NL, 4]); b_fg = din("ml_b_fg", [NL, 4]); ml_norm = din("ml_norm", [NL, 768])
    w_br = din("w_branch", [NL, D, D]); w_out = din("w_out", [NL, D, D]); ffn_norm = din("ffn_norm", [NL, D])
    w_up = din("ffn_w_up", [NL, D, 2 * FFN]); f_cw = din("ffn_conv_w", [NL, 3, 2 * FFN]); f_cb = din("ffn_conv_b", [NL, 2 * FFN])
    w_dn = din("ffn_w_down", [NL, FFN, D]); fin_norm = din("final_norm", [D])
    y_d = nc.dram_tensor("y", [NT * T, D], F32, kind="ExternalOutput").ap()
    xs_d = nc.dram_tensor("xs_scr", [NT, 128, NCH * T], F32, kind="Internal").ap()
    lbfc_d = nc.dram_tensor("lbfc_scr", [NL, 16, 128, 512], F32, kind="Internal").ap()
    tab_d = nc.dram_tensor("tab_scr", [NL, 16, 128, 1024], F32, kind="Internal").ap()

    with es:
        P = Prog(nc, es)
        sbt = lambda n, s, d=F32: es.enter_context(nc.sbuf_tensor(n + "_sb", s, d))
        cst = sbt("cst", [128, NCST])
        xt = sbt("xt", [128, NCH, T])
        ht = sbt("ht", [128, NCH, T], BF)
        htr = ht[:]
        ring = sbt("ring", [128, NRING, 2, RW], BF)
        R = sbt("R", [128, RCOLS])
        vp = sbt("vp", [128, 420])
        vq = sbt("vq", [96, 48])
        lbs = sbt("lbs", [128, 4 * 6 * 2])
        s5st = sbt("s5st", [128, 2, 16])
        s5p = sbt("s5p", [128, 2, 16])
        hst = sbt("hst", [128, 6, 128])
        mlC = sbt("mlC", [96, 4, 2, 192])
        mlN = sbt("mlN", [96, 4, 2, 96])
        mlt = sbt("mlt", [96, 8, 3])
        ftl = sbt("ftl", [128, 88, 2])
        rc = sbt("rc", [4, 4])
        ps = [es.enter_context(nc.psum_tensor("psb%d" % i, [128, 512], F32)) for i in range(8)]
        PK = lambda b: "ps%d" % b

        ident = cst[:, C_IDENT:C_IDENT + 128]
        ones = cst[:, C_ONES:C_ONES + 128]
        m128 = cst[:, C_M128:C_M128 + 128]
        m64 = cst[0:64, C_M64:C_M64 + 64]
        tt_i = cst[:, C_TT:C_TT + 512]
        epsc = cst[:, C_EPS:C_EPS + 1]

        def sel4(h):
            return cst[0:4, C_SEL + 96 * h:C_SEL + 96 * (h + 1)]

        class Carve:
            def __init__(self, base=0):
                self.o = base

            def t2(self, n, parts=128):
                a = R[0:parts, self.o:self.o + n]
                self.o += n
                assert self.o <= RCOLS, self.o
                return a

            def t3(self, a, b, parts=128):
                return self.t2(a * b, parts).rearrange("p (a b) -> p a b", b=b)

            def h2(self, n, parts=128):
                return self.t2((n + 1) // 2, parts).bitcast(BF)[:, 0:n]

            def h3(self, a, b, parts=128):
                return self.t2(a * b // 2, parts).bitcast(BF).rearrange("p (a b) -> p a b", b=b)

        P.dma("sp", cst[:], cst_d, (), ["cst"])
        ring_i = [0]

        def proj(wsrc, kchunks, pieces, tiles, consume, banks=None, ringB=None):
            banks = banks or list(range(len(tiles)))
            nk = len(kchunks)
            ki = 0
            while ki < nk:
                r0, nr, rhs, rkey = kchunks[ki]
                pack = 1
                if nr == 128 and ki + 1 < nk and kchunks[ki + 1][1] == 128 and kchunks[ki + 1][0] == r0 + 128:
                    pack = 2
                slots = []
                for (c0, wd) in pieces:
                    s = ring_i[0] % NRING
                    ring_i[0] += 1
                    if pack == 2:
                        P.dma("pool", ring[:, s, :, 0:wd], wsrc[r0:r0 + 256, c0:c0 + wd].rearrange("(a p) c -> p a c", p=128),
                              (), ["ring%d" % s])
                    else:
                        P.dma("pool", ring[0:nr, s, 0, 0:wd], wsrc[r0:r0 + nr, c0:c0 + wd], (), ["ring%d" % s])
                    slots.append(s)
                for a in range(pack):
                    _, nr_a, rhs_a, rkey_a = kchunks[ki + a]
                    for ti, (pi, off, M) in enumerate(tiles):
                        s = slots[pi]
                        P.mm(ps[banks[ti]][0:M, :], ring[0:nr_a, s, a, off:off + M], rhs_a, ki + a == 0, ki + a == nk - 1,
                             ["ring%d" % s, rkey_a], [PK(banks[ti])])
                ki += pack
            for ti, (pi, off, M) in enumerate(tiles):
                consume(ti, ps[banks[ti]][0:M, :], PK(banks[ti]))

        def hk(keyprefix="ht"):
            return [(128 * c, 128, htr[:, c, :], "ht") for c in range(NCH)]

        def emit_sin(out, x, tk, kx, kt, kout):
            P.ts(tk, x, 1.0 / TWO_PI, MAGIC, ALU.mult, ALU.add, [kx], [kt])
            P.ts(tk, tk, -MAGIC, None, ALU.add, None, [kt], [kt])
            P.stt(x, tk, -TWO_PI, x, ALU.mult, ALU.add, [kt, kx], [kx])
            P.ts(tk, x, math.pi, TWO_PI, ALU.is_gt, ALU.mult, [kx], [kt])
            P.tt(x, x, tk, ALU.subtract, [kx, kt], [kx])
            P.ts(x, x, math.pi, -math.pi, ALU.min, ALU.max, [kx], [kx])
            P.actf(out, x, AF.Sin, [kx], [kout])

        def load_T(dst, src, nr, w, stage, kst, bank=7):
            P.dma("sp", stage[0:nr, 0:w], src, (), [kst])
            P.tr(ps[bank][0:w, 0:nr], stage[0:nr, 0:w], ident[0:nr, 0:nr], [kst, "cst"], [PK(bank)])
            P.cp(dst, ps[bank][0:w, 0:nr], [PK(bank)], ["vp"])

        def rmsnorm(dst, gcol, sq2, rs, kdst):
            for c in range(NCH):
                sq = sq2[:, c % 2, :]
                P.actf(sq, xt[:, c, :], AF.Square, ["xt%d" % c], ["sq%d" % (c % 2)])
                P.mm(ps[7][:, :], ones, sq, c == 0, c == NCH - 1, ["sq%d" % (c % 2), "cst"], [PK(7)])
            P.actf(rs, ps[7][:, :], AF.Sqrt, [PK(7)], ["rs"], bias=epsc, scale=1.0 / D)
            P.add("dve", lambda e: e.reciprocal(rs, rs), ["rs"], ["rs"])
            for c in range(NCH):
                P.stt(dst[:, c, :], xt[:, c, :], gcol(c), rs, ALU.mult, ALU.mult, ["xt%d" % c, "rs", "vp"], [kdst])

        cv = Carve()
        stg = cv.t2(128)
        lraw = cv.t2(NL * 6)
        for l in range(NL):
            load_T(lraw[:, l * 6:(l + 1) * 6], hg_lb[l].rearrange("(c p) -> c p", p=128), 6, 128, stg, "stg")
        ex = cv.t2(NL * 6)
        tot = cv.t2(6)
        P.actf(ex, lraw, AF.Exp, ["vp"], ["ex"])
        P.cp(tot, ex[:, 0:6], ["ex"], ["tot"])
        for l in range(1, NL):
            P.tt(tot, tot, ex[:, l * 6:(l + 1) * 6], ALU.add, ["tot", "ex"], ["tot"])
        P.add("dve", lambda e: e.reciprocal(tot, tot), ["tot"], ["tot"])
        P.memset(lbs[:, 0:6], 0.0, ["lbs"])
        for l in range(1, NL):
            if l == 1:
                P.cp(lbs[:, 6:12], ex[:, 6:12], ["ex"], ["lbs"])
            else:
                P.tt(lbs[:, l * 6:(l + 1) * 6], lbs[:, (l - 1) * 6:l * 6], ex[:, l * 6:(l + 1) * 6], ALU.add, ["lbs", "ex"], ["lbs"])
        for l in range(NL):
            if l > 0:
                P.tt(lbs[:, l * 6:(l + 1) * 6], lbs[:, l * 6:(l + 1) * 6], tot, ALU.mult, ["lbs", "tot"], ["lbs"])
        for l in range(NL):
            P.ts(lbs[:, 24 + l * 6:24 + (l + 1) * 6], lbs[:, l * 6:(l + 1) * 6], -1.0, 1.0, ALU.mult, ALU.add, ["lbs"], ["lbs"])
        P.barrier()

        for l in range(NL):
            cv = Carve()
            stg = cv.t2(128)
            load_T(vp[:, 0:16], mix_norm[l].rearrange("(c p) -> c p", p=128), 16, 128, stg, "stg")
            load_T(vp[:, 16:32], ffn_norm[l].rearrange("(c p) -> c p", p=128), 16, 128, stg, "stg")
            load_T(vp[:, 32:36], s5_d[l].rearrange("(c p) -> c p", p=128), 4, 128, stg, "stg")
            load_T(vp[:, 36:40], b_glu[l].rearrange("(c p) -> c p", p=128), 4, 128, stg, "stg")
            load_T(vp[:, 40:46], hg_norm[l].rearrange("(c p) -> c p", p=128), 6, 128, stg, "stg")
            load_T(vp[:, 46:134], f_cb[l].rearrange("(c p) -> c p", p=128), 88, 128, stg, "stg")
            for k in range(3):
                load_T(vp[:, 134 + 88 * k:134 + 88 * (k + 1)], f_cw[l, k].rearrange("(c p) -> c p", p=128), 88, 128, stg, "stg")
            load_T(vp[:, 398:414], fin_norm.rearrange("(c p) -> c p", p=128), 16, 128, stg, "stg")
            for k in range(4):
                load_T(vq[:, 8 * k:8 * (k + 1)], ml_cw[l, k].rearrange("(c p) -> c p", p=96), 8, 96, stg, "stg")
            load_T(vq[:, 32:40], ml_cb[l].rearrange("(c p) -> c p", p=96), 8, 96, stg, "stg")
            load_T(vq[:, 40:48], ml_norm[l].rearrange("(c p) -> c p", p=96), 8, 96, stg, "stg")
            P.dma("sp", rc[:, 1:2], b_ig[l].rearrange("(h o) -> h o", o=1), (), ["rc"])
            P.dma("sp", rc[:, 2:3], b_fg[l].rearrange("(h o) -> h o", o=1), (), ["rc"])
            P.memset(s5st[:], 0.0, ["s5st"]); P.memset(hst[:], 0.0, ["hst"]); P.memset(mlC[:], 0.0, ["mlC"])
            P.memset(mlN[:], 0.0, ["mlN"]); P.memset(mlt[:], 0.0, ["mlt"]); P.memset(ftl[:], 0.0, ["ftl"])
            P.memset(rc[:, 0:1], 0.0, ["rc"])

            L16 = cv.t3(3, 128, parts=16)
            LD = cv.t2(2, parts=16)
            P.dma("sp", L16[:, 0, :], lam_re[l].rearrange("(s g) n -> s (g n)", g=2), (), ["L16"])
            P.dma("sp", L16[:, 1, :], lam_im[l].rearrange("(s g) n -> s (g n)", g=2), (), ["L16"])
            P.dma("sp", LD, log_dt[l].rearrange("(s g) -> s g", g=2), (), ["LD"])
            P.cp(L16[:, 2, :].rearrange("p (g n) -> p g n", n=64), LD.unsqueeze(2).to_broadcast([16, 2, 64]), ["LD"], ["L16"])
            sp = cv.t3(12, 16)
            for i in range(3):
                P.tr(ps[7][:, 0:16], L16[:, i, :], ident[0:16, 0:16], ["L16", "cst"], [PK(7)])
                P.cp(sp[:, i, :], ps[7][:, 0:16], [PK(7)], ["sp"])
            lr, li, dt_ = sp[:, 0, :], sp[:, 1, :], sp[:, 2, :]
            tmpa, tmpb = sp[:, 3, :], sp[:, 4, :]
            cosv, sinv, ar, ai, den, cr, ci = (sp[:, i, :] for i in range(5, 12))
            P.actf(dt_, dt_, AF.Exp, ["sp"], ["sp"])
            P.tt(tmpa, lr, dt_, ALU.mult, ["sp"], ["sp"])
            P.actf(s5p[:, 0, :], tmpa, AF.Exp, ["sp"], ["s5p"])
            P.tt(s5p[:, 1, :], li, dt_, ALU.mult, ["sp"], ["s5p"])
            P.cp(tmpa, s5p[:, 1, :], ["s5p"], ["sp"])
            emit_sin(sinv, tmpa, tmpb, "sp", "sp", "sp")
            P.ts(tmpa, s5p[:, 1, :], math.pi / 2, None, ALU.add, None, ["s5p", "sp"], ["sp"])
            emit_sin(cosv, tmpa, tmpb, "sp", "sp", "sp")
            P.tt(ar, s5p[:, 0, :], cosv, ALU.mult, ["sp", "s5p"], ["sp"])
            P.tt(ai, s5p[:, 0, :], sinv, ALU.mult, ["sp", "s5p"], ["sp"])
            P.tt(den, lr, lr, ALU.mult, ["sp"], ["sp"])
            P.tt(tmpa, li, li, ALU.mult, ["sp"], ["sp"])
            P.tt(den, den, tmpa, ALU.add, ["sp"], ["sp"])
            P.add("dve", lambda e: e.reciprocal(den, den), ["sp"], ["sp"])
            P.ts(ar, ar, -1.0, None, ALU.add, None, ["sp"], ["sp"])
            P.tt(cr, ar, lr, ALU.mult, ["sp"], ["sp"])
            P.tt(tmpa, ai, li, ALU.mult, ["sp"], ["sp"])
            P.tt(cr, cr, tmpa, ALU.add, ["sp"], ["sp"])
            P.tt(cr, cr, den, ALU.mult, ["sp"], ["sp"])
            P.tt(ci, ai, lr, ALU.mult, ["sp"], ["sp"])
            P.tt(tmpa, ar, li, ALU.mult, ["sp"], ["sp"])
            P.tt(ci, ci, tmpa, ALU.subtract, ["sp"], ["sp"])
            P.tt(ci, ci, den, ALU.mult, ["sp"], ["sp"])
            TB = cv.t3(2, 1024); tx = cv.t2(T); tk_ = cv.t2(T)
            for s in range(16):
                tb = TB[:, s % 2, :]
                P.ts(tx, tt_i, s5p[:, 1, s:s + 1], math.pi / 2, ALU.mult, ALU.add, ["cst", "s5p"], ["tx"])
                emit_sin(tb[:, 0:T], tx, tk_, "tx", "tk", "TB%d" % (s % 2))
                P.ts(tx, tt_i, s5p[:, 1, s:s + 1], None, ALU.mult, None, ["cst", "s5p"], ["tx"])
                emit_sin(tb[:, T:2 * T], tx, tk_, "tx", "tk", "TB%d" % (s % 2))
                P.dma("sp", tab_d[l, s], tb, ["TB%d" % (s % 2)], ["tab%d" % s])
            BR = cv.t3(16, 16); BI = cv.t3(16, 16); bbr = cv.t3(16, 16); bbi = cv.t3(16, 16); btmp = cv.t3(16, 16)
            P.dma("sp", BR, b_re[l].rearrange("(s g) n p -> (g n) s p", g=2), (), ["BR"])
            P.dma("sp", BI, b_im[l].rearrange("(s g) n p -> (g n) s p", g=2), (), ["BI"])
            crb = cr.unsqueeze(2).to_broadcast([128, 16, 16]); cib = ci.unsqueeze(2).to_broadcast([128, 16, 16])
            P.tt(bbr, BR, crb, ALU.mult, ["BR", "sp"], ["bbr"])
            P.tt(btmp, BI, cib, ALU.mult, ["BI", "sp"], ["btmp"])
            P.tt(bbr, bbr, btmp, ALU.subtract, ["bbr", "btmp"], ["bbr"])
            P.tt(bbi, BI, crb, ALU.mult, ["BI", "sp"], ["bbi"])
            P.tt(btmp, BR, cib, ALU.mult, ["BR", "sp"], ["btmp"])
            P.tt(bbi, bbi, btmp, ALU.add, ["bbi", "btmp"], ["bbi"])
            CI = cv.t3(2 * 16, 128, parts=16)
            cre = cv.t3(16, 16); cim = cv.t3(16, 16)
            P.dma("sp", CI[:, 0:16, :].rearrange("p s (g n) -> p s g n", g=2), c_re[l].rearrange("(s g) p n -> p s g n", g=2), (), ["CI"])
            P.dma("sp", CI[:, 16:32, :].rearrange("p s (g n) -> p s g n", g=2), c_im[l].rearrange("(s g) p n -> p s g n", g=2), (), ["CI"])
            for s in range(32):
                P.tr(ps[6][:, (s % 16) * 16:(s % 16 + 1) * 16], CI[:, s, :], ident[0:16, 0:16], ["CI", "cst"], [PK(6)])
                if s == 15:
                    P.cp(cre.rearrange("p a b -> p (a b)"), ps[6][:, 0:256], [PK(6)], ["cre"])
                if s == 31:
                    P.ts(cim.rearrange("p a b -> p (a b)"), ps[6][:, 0:256], -1.0, None, ALU.mult, None, [PK(6)], ["cim"])
            Fm = cv.t3(16, 128)
            Fst = cv.t3(4, 128)
            for mi, (src, ksrc, needT) in enumerate([(bbr, "bbr", True), (bbi, "bbi", True), (cre, "cre", False), (cim, "cim", False)]):
                P.memset(Fm, 0.0, ["Fm"])
                F4 = Fm.rearrange("p (a m) r -> p a m r", m=4)
                s4 = src.rearrange("p (a m) q -> p a m q", m=4)
                for m in range(4):
                    for gl in range(2):
                        P.ts(F4[:, :, m, 32 * m + 16 * gl:32 * m + 16 * gl + 16], s4[:, :, m, :],
                             cst[:, C_MG + gl:C_MG + gl + 1], None, ALU.mult, None, [ksrc, "cst"], ["Fm"])
                for s in range(16):
                    if needT:
                        P.tr(ps[s % 2][:, 0:128], Fm[:, s, :], ident, ["Fm", "cst"], [PK(s % 2)])
                        P.cp(Fst[:, s % 4, :], ps[s % 2][:, 0:128], [PK(s % 2)], ["Fst%d" % (s % 4)])
                        P.dma("sp", lbfc_d[l, s, :, 128 * mi:128 * (mi + 1)], Fst[:, s % 4, :], ["Fst%d" % (s % 4)], ["lbfc%d" % s])
                    else:
                        P.dma("sp", lbfc_d[l, s, :, 128 * mi:128 * (mi + 1)], Fm[:, s, :], ["Fm"], ["lbfc%d" % s])
            P.barrier()

            for t in range(NT):
                tok0 = t * T
                if l == 0:
                    cv = Carve()
                    xin = cv.t2(D)
                    for tb in range(4):
                        P.dma("sp", xin, x_d[tok0 + tb * 128:tok0 + (tb + 1) * 128, :], (), ["xin"])
                        for c in range(NCH):
                            P.tr(ps[c % 8][:, 0:128], xin[:, c * 128:(c + 1) * 128], ident, ["xin", "cst"], [PK(c % 8)])
                            P.cp(xt[:, c, tb * 128:(tb + 1) * 128], ps[c % 8][:, 0:128], [PK(c % 8)], ["xt%d" % c],
                                 eng=("act" if c % 2 else "dve"))
                else:
                    P.dma("sp", xt[:].rearrange("p a b -> p (a b)"), xs_d[t], ["xs%d" % t], ["xt%d" % c for c in range(NCH)])
                P.barrier()

                cv = Carve()
                merged = cv.t3(NCH, T)
                mergedr = cv.h3(NCH, T)
                Ybr = cv.h3(8, T)
                abase = cv.o
                SG = Carve(abase).t3(6, T)

                ca_ = Carve(abase)
                sq2 = ca_.t3(2, T); rs = ca_.t2(T)
                rmsnorm(htr, lambda c: vp[:, c:c + 1], sq2, rs, "ht")
                P.barrier()

                def branch(kchunks, first, last_br):
                    gcol0 = {0: 5896, 1: 5896 + D, 2: 5896 + 2 * D}[first[0]]
                    row0 = first[1]
                    for g0, ng in ((0, 6), (6, 6), (12, 4)):
                        def cons_g(i, pap, pk):
                            P.actf(SG[:, i, :], pap, AF.Sigmoid, [pk], ["SG%d" % i])
                        proj(w_in[l], hk(), [(gcol0 + 128 * g0, 128 * ng)], [(0, 128 * i, 128) for i in range(ng)], cons_g)

                        def cons_z(i, pap, pk, g0=g0):
                            j = g0 + i
                            if first[0] == 0:
                                P.tt(merged[:, j, :], SG[:, i, :], pap, ALU.mult, ["SG%d" % i, pk], ["mg%d" % j])
                            else:
                                P.tt(SG[:, i, :], SG[:, i, :], pap, ALU.mult, ["SG%d" % i, pk], ["SG%d" % i])
                                dstm = mergedr if last_br else merged
                                P.tt(dstm[:, j, :], merged[:, j, :], SG[:, i, :], ALU.add, ["SG%d" % i, "mg%d" % j],
                                     ["mgr%d" % j if last_br else "mg%d" % j])
                        kc = [(row0 + r0, nr, rhs, rk) for (r0, nr, rhs, rk) in kchunks]
                        proj(w_br[l], kc, [(128 * g0, 128 * ng)], [(0, 128 * i, 128) for i in range(ng)], cons_z)

                ca_ = Carve(abase)
                U = ca_.t3(4, T); G = ca_.t3(4, T); Gr_ = ca_.h3(4, T)
                CSN = ca_.t3(2, 2 * T); zr = ca_.t2(T); zi = ca_.t2(T); wr = ca_.t2(T); wi = ca_.t2(T)
                t1 = ca_.t2(T); t2_ = ca_.t2(T)
                LF = ca_.t3(2, 512)

                def cons_u(i, pap, pk):
                    P.cp(U[:, i, :], pap, [pk], ["U"], eng="act")
                proj(w_in[l], hk(), [(0, 512)], [(0, 128 * i, 128) for i in range(4)], cons_u, banks=[4, 5, 6, 7])
                for s in range(16):
                    c = s // 4
                    lf = LF[:, s % 2, :]
                    lfk = "LF%d" % (s % 2)
                    P.dma("sp", lf, lbfc_d[l, s], ["lbfc%d" % s], [lfk])
                    cs = CSN[:, s % 2, 0:T]; sn = CSN[:, s % 2, T:2 * T]
                    kcs = "csn%d" % (s % 2)
                    P.dma("sp", CSN[:, s % 2, :], tab_d[l, s], ["tab%d" % s], [kcs])
                    bre, bim = ps[4 + 2 * (s % 2)], ps[5 + 2 * (s % 2)]
                    kre, kim = PK(4 + 2 * (s % 2)), PK(5 + 2 * (s % 2))
                    P.mm(bre[:, :], lf[:, 0:128], U[:, c, :], True, True, [lfk, "U"], [kre])
                    P.mm(bim[:, :], lf[:, 128:256], U[:, c, :], True, True, [lfk, "U"], [kim])
                    P.tt(t1, bre[:, :], cs, ALU.mult, [kre, kcs], ["t1"])
                    P.tt(t2_, bim[:, :], sn, ALU.mult, [kim, kcs], ["t2"])
                    P.tt(zr, t1, t2_, ALU.add, ["t1", "t2"], ["zr"])
                    P.tt(t1, bim[:, :], cs, ALU.mult, [kim, kcs], ["t1"])
                    P.tt(t2_, bre[:, :], sn, ALU.mult, [kre, kcs], ["t2"])
                    P.tt(zi, t1, t2_, ALU.subtract, ["t1", "t2"], ["zi"])
                    magb = s5p[:, 0, s:s + 1].to_broadcast([128, T])
                    P.scan(wr, magb, zr, s5st[:, 0, s:s + 1], ALU.mult, ALU.add, ["zr", "s5p", "s5st"], ["wr"])
                    P.scan(wi, magb, zi, s5st[:, 1, s:s + 1], ALU.mult, ALU.add, ["zi", "s5p", "s5st"], ["wi"])
                    P.tt(t1, wr, cs, ALU.mult, ["wr", kcs], ["t1"])
                    P.tt(t2_, wi, sn, ALU.mult, ["wi", kcs], ["t2"])
                    P.tt(zr, t1, t2_, ALU.subtract, ["t1", "t2"], ["zr"])
                    P.tt(t1, wr, sn, ALU.mult, ["wr", kcs], ["t1"])
                    P.tt(t2_, wi, cs, ALU.mult, ["wi", kcs], ["t2"])
                    P.tt(zi, t1, t2_, ALU.add, ["t1", "t2"], ["zi"])
                    P.cp(s5st[:, 0, s:s + 1], zr[:, T - 1:T], ["zr"], ["s5st"])
                    P.cp(s5st[:, 1, s:s + 1], zi[:, T - 1:T], ["zi"], ["s5st"])
                    P.mm(ps[c][:, :], lf[:, 256:384], zr, s % 4 == 0, False, [lfk, "zr"], [PK(c)])
                    P.mm(ps[c][:, :], lf[:, 384:512], zi, False, s % 4 == 3, [lfk, "zi"], [PK(c)])
                for c in range(4):
                    P.stt(t1, U[:, c, :], vp[:, 32 + c:33 + c], ps[c][:, :], ALU.mult, ALU.add, ["U", "vp", PK(c)], ["t1"])
                    P.tt(t2_, t1, t1, ALU.mult, ["t1"], ["t2"])
                    P.ts(t2_, t2_, 0.044715, 1.0, ALU.mult, ALU.add, ["t2"], ["t2"])
                    P.tt(t2_, t2_, t1, ALU.mult, ["t1", "t2"], ["t2"])
                    P.actf(t2_, t2_, AF.Sigmoid, ["t2"], ["t2"], scale=2.0 * GELU_C)
                    P.tt(G[:, c, :], t1, t2_, ALU.mult, ["t1", "t2"], ["G"])
                    P.cp(Gr_[:, c, :], G[:, c, :], ["G"], ["Gr"], eng="act")

                def cons_glu(i, pap, pk):
                    P.actf(t1, pap, AF.Sigmoid, [pk], ["t1"], bias=vp[:, 36 + i:37 + i], scale=1.0)
                    P.tt(Ybr[:, i, :], G[:, i, :], t1, ALU.mult, ["G", "t1"], ["Y"])
                proj(w_glu[l], [(128 * c, 128, Gr_[:, c, :], "Gr") for c in range(4)], [(0, 512)],
                     [(0, 128 * i, 128) for i in range(4)], cons_glu)
                P.barrier()
                branch([(128 * c, 128, Ybr[:, c, :], "Y") for c in range(4)], (0, 0), False)
                P.barrier()

                for half in range(2):
                    ca_ = Carve(abase)
                    Qb = ca_.t3(3, T); Fb = ca_.t3(3, T); Vb = ca_.t3(3, T); OGb = ca_.t3(3, T)
                    T1 = ca_.t2(T); T2 = ca_.t2(T); CM = ca_.t2(T); D3 = ca_.t2(T)
                    A = ca_.t2(T); Ash = ca_.t2(T); Bm = ca_.t2(T); Kd = ca_.t2(T)
                    VT = ca_.t2(128, parts=64); KT = ca_.t2(128, parts=64); SM = ca_.t2(64, parts=64)
                    EL = ca_.t2(8)

                    def cons1(i, pap, pk):
                        if i < 3:
                            P.actf(Qb[:, i, :], pap, AF.Silu, [pk], ["Qb%d" % i])
                        else:
                            P.actf(Fb[:, i - 3, :], pap, AF.Sigmoid, [pk], ["Fb%d" % (i - 3)])
                    proj(w_in[l], hk(), [(512 + 384 * half, 384), (1280 + 384 * half, 384)],
                         [(0, 0, 128), (0, 128, 128), (0, 256, 128), (1, 0, 128), (1, 128, 128), (1, 256, 128)], cons1)

                    def cons2(i, pap, pk):
                        if i < 3:
                            P.cp(Vb[:, i, :], pap, [pk], ["Vb%d" % i], eng="act")
                        else:
                            P.actf(OGb[:, i - 3, :], pap, AF.Silu, [pk], ["OGb%d" % (i - 3)])
                    proj(w_in[l], hk(), [(2048 + 384 * half, 384), (2816 + 384 * half, 384)],
                         [(0, 0, 128), (0, 128, 128), (0, 256, 128), (1, 0, 128), (1, 128, 128), (1, 256, 128)], cons2)
                    for hh in range(3):
                        h = 3 * half + hh
                        q = Qb[:, hh, :]; f = Fb[:, hh, :]; v = Vb[:, hh, :]; og = OGb[:, hh, :]
                        kq, kf, kv, ko = "Qb%d" % hh, "Fb%d" % hh, "Vb%d" % hh, "OGb%d" % hh
                        P.ts(f, f, lbs[:, 24 + l * 6 + h:24 + l * 6 + h + 1], lbs[:, l * 6 + h:l * 6 + h + 1], ALU.mult, ALU.add, [kf, "lbs"], [kf])
                        P.actf(T1, f, AF.Ln, [kf], ["T1"])
                        P.ts(f, f, -1.0, 1.0, ALU.mult, ALU.add, [kf], [kf])
                        P.scan(T2, ones[:, 0:1].to_broadcast([128, T]), T1, 0.0, ALU.mult, ALU.add, ["T1", "cst"], ["T2"])
                        T23 = T2.rearrange("p (a b) -> p a b", b=64); CM3 = CM.rearrange("p (a b) -> p a b", b=64)
                        P.cp(CM3[:, 0, :], T23[:, 0, :], ["T2"], ["CM"])
                        P.tt(CM3[:, 1:8, :], T23[:, 1:8, :], T23[:, 0:7, 63:64].to_broadcast([128, 7, 64]), ALU.subtract, ["T2"], ["CM"])
                        lastb = CM3[:, :, 63:64].to_broadcast([128, 8, 64])
                        D33 = D3.rearrange("p (a b) -> p a b", b=64)
                        P.stt(D33, lastb, -0.5, CM3, ALU.mult, ALU.add, ["CM"], ["D3"])
                        P.actf(T1, CM, AF.Exp, ["CM"], ["T1"])
                        P.tt(A, q, T1, ALU.mult, [kq, "T1"], ["A"])
                        P.actf(T1, D3, AF.Exp, ["D3"], ["T1"])
                        P.tt(Ash, q, T1, ALU.mult, [kq, "T1"], ["Ash"])
                        P.actf(T2, D3, AF.Exp, ["D3"], ["T2"], scale=-1.0)
                        P.tt(Bm, f, T2, ALU.mult, [kf, "T2"], ["Bm"])
                        P.tt(D33, lastb, CM3, ALU.subtract, ["CM"], ["D3"])
                        P.actf(T1, D3, AF.Exp, ["D3"], ["T1"])
                        P.tt(Kd, f, T1, ALU.mult, [kf, "T1"], ["Kd"])
                        P.actf(EL, CM3[:, :, 63], AF.Exp, ["CM"], ["EL"])
                        S = hst[:, h, :]
                        ks = "hst%d" % h
                        for c in range(8):
                            cols = slice(64 * c, 64 * (c + 1))
                            P.mm(ps[0][0:64, 0:64], Bm[:, cols], Ash[:, cols], True, True, ["Bm", "Ash"], [PK(0)])
                            P.tt(SM, ps[0][0:64, 0:64], m64, ALU.mult, [PK(0), "cst"], ["SM"])
                            P.tr(ps[1][0:64, 0:128], v[:, cols], ident, [kv, "cst"], [PK(1)])
                            P.cp(VT, ps[1][0:64, 0:128], [PK(1)], ["VT"], eng="act")
                            P.tr(ps[2][0:64, 0:128], Kd[:, cols], ident, ["Kd", "cst"], [PK(2)])
                            P.cp(KT, ps[2][0:64, 0:128], [PK(2)], ["KT"], eng="act")
                            P.mm(ps[3][:, cols], VT, SM, True, False, ["VT", "SM"], [PK(3)])
                            P.mm(ps[3][:, cols], S, A[:, cols], False, True, [ks, "A"], [PK(3)])
                            P.mm(ps[4][:, 0:128], KT, VT, True, True, ["KT", "VT"], [PK(4)])
                            P.stt(S, S, EL[:, c:c + 1], ps[4][:, 0:128], ALU.mult, ALU.add, [ks, "EL", PK(4)], [ks])
                        P.cp(T1, ps[3][:, :], [PK(3)], ["T1"])
                        P.actf(T2, T1, AF.Square, ["T1"], ["T2"])
                        P.mm(ps[5][:, :], ones, T2, True, True, ["T2", "cst"], [PK(5)])
                        P.actf(T2, ps[5][:, :], AF.Sqrt, [PK(5)], ["T2"], bias=epsc, scale=1.0 / 128)
                        P.add("dve", lambda e, T2=T2: e.reciprocal(T2, T2), ["T2"], ["T2"])
                        P.stt(T1, T1, vp[:, 40 + h:41 + h], T2, ALU.mult, ALU.mult, ["T1", "T2", "vp"], ["T1"])
                        P.tt(Ybr[:, h, :], T1, og, ALU.mult, ["T1", ko], ["Y"])
                P.barrier()
                branch([(128 * c, 128, Ybr[:, c, :], "Y") for c in range(6)], (1, 512), False)
                P.barrier()

                ca_ = Carve(abase)
                IG = ca_.t2(T, parts=4); LFr = ca_.t2(T, parts=4); Bc = ca_.t2(T, parts=4); Rr = ca_.t2(T, parts=4)
                AC = ca_.t3(4, 4); CS = ca_.t2(4, parts=4); DEC = ca_.t2(4, parts=4); DB = ca_.t2(4, parts=96)
                CXs = ca_.t3(2, 515, parts=96); Vh = ca_.t3(2, T, parts=96); OGs = ca_.t3(2, T, parts=96)
                acc = ca_.t3(2, T, parts=96); CAr = ca_.h3(2, T, parts=96)
                Qm = ca_.t3(2, T, parts=96); Km = ca_.t3(2, T, parts=96); HS = ca_.t3(2, T, parts=96)
                DS = ca_.t2(T, parts=96); DT2 = ca_.t2(T, parts=96)
                PT = ca_.t2(128); VTm = ca_.t2(192); KTa = ca_.t2(192); CTm = ca_.t2(384, parts=96)
                Ycr = Ybr[0:96, :, :]

                def cons_g4(i, pap, pk):
                    if i == 0:
                        P.actf(IG, pap, AF.Identity, [pk], ["IG"], bias=rc[:, 1:2], scale=1.0)
                    else:
                        P.actf(LFr, pap, AF.Sigmoid, [pk], ["LFr"], bias=rc[:, 2:3], scale=1.0)
                        P.actf(LFr, LFr, AF.Ln, ["LFr"], ["LFr"])
                proj(w_in[l], hk(), [(5888, 8)], [(0, 0, 4), (0, 4, 4)], cons_g4)
                ones4 = ones[0:4, 0:1].to_broadcast([4, T])
                P.scan(Bc, ones4, LFr, 0.0, ALU.mult, ALU.add, ["LFr", "cst"], ["Bc"])
                P.tt(IG, IG, Bc, ALU.subtract, ["IG", "Bc"], ["IG"])
                P.scan(Rr, ones4, IG, rc[:, 0:1], ALU.mult, ALU.max, ["IG", "rc", "cst"], ["Rr"])
                R3 = Rr.rearrange("p (a b) -> p a b", b=128)
                P.cp(CS[:, 0:1], rc[:, 0:1], ["rc"], ["CS"])
                P.cp(CS[:, 1:4], R3[:, 0:3, 127], ["Rr"], ["CS"])
                P.tt(DEC, CS, R3[:, :, 127], ALU.subtract, ["CS", "Rr"], ["DEC"])
                P.actf(DEC, DEC, AF.Exp, ["DEC"], ["DEC"])
                csb = CS.unsqueeze(2).to_broadcast([4, 4, 128])
                P.tt(LFr.rearrange("p (a b) -> p a b", b=128), IG.rearrange("p (a b) -> p a b", b=128), csb, ALU.subtract, ["IG", "CS"], ["LFr"])
                P.actf(LFr, LFr, AF.Exp, ["LFr"], ["LFr"])
                P.tt(IG.rearrange("p (a b) -> p a b", b=128), Bc.rearrange("p (a b) -> p a b", b=128), csb, ALU.add, ["Bc", "CS", "IG"], ["IG"])
                P.actf(IG, IG, AF.Exp, ["IG"], ["IG"], scale=-1.0)
                P.tt(rc[:, 0:1], Bc[:, T - 1:T], Rr[:, T - 1:T], ALU.add, ["Bc", "Rr"], ["rc"])
                for ch in range(4):
                    P.mm(ps[7][:, 4 * ch:4 * ch + 4], LFr[:, 128 * ch:128 * (ch + 1)], ident[0:4, 0:4], True, True, ["LFr", "cst"], [PK(7)])
                P.cp(AC.rearrange("p a b -> p (a b)"), ps[7][:, 0:16], [PK(7)], ["AC"])
                for h in range(4):
                    def cons_m(i, pap, pk, h=h):
                        j = i % 2
                        if i < 2:
                            P.cp(CXs[:, j, 3:515], pap, [pk], ["CXs%d" % j], eng="act")
                        elif i < 4:
                            P.cp(Vh[:, j, :], pap, [pk], ["Vh"], eng="act")
                        else:
                            P.actf(OGs[:, j, :], pap, AF.Sigmoid, [pk], ["OGs"])
                    proj(w_in[l], hk(), [(3584 + 192 * h, 192), (4352 + 192 * h, 192), (5120 + 192 * h, 192)],
                         [(0, 0, 96), (0, 96, 96), (1, 0, 96), (1, 96, 96), (2, 0, 96), (2, 96, 96)], cons_m)
                    for j in range(2):
                        fj = 2 * h + j
                        P.cp(CXs[:, j, 0:3], mlt[:, fj, :], ["mlt"], ["CXs%d" % j])
                        P.actf(acc[:, j, :], CXs[:, j, 3:515], AF.Identity, ["CXs%d" % j, "vq"], ["acc"],
                               bias=vq[:, 32 + fj:33 + fj], scale=vq[:, 24 + fj:25 + fj])
                        for k in range(3):
                            P.stt(acc[:, j, :], CXs[:, j, k:k + T], vq[:, 8 * k + fj:8 * k + fj + 1], acc[:, j, :], ALU.mult, ALU.add,
                                  ["CXs%d" % j, "vq", "acc"], ["acc"])
                        P.cp(mlt[:, fj, :], CXs[:, j, 512:515], ["CXs%d" % j], ["mlt"])
                        P.actf(CAr[:, j, :], acc[:, j, :], AF.Silu, ["acc"], ["CA"])

                    def cons_qk(i, pap, pk):
                        if i < 2:
                            P.cp(Qm[:, i, :], pap, [pk], ["Qm"], eng="act")
                        else:
                            P.ts(Km[:, i - 2, :], pap, 192.0 ** -0.5, None, ALU.mult, None, [pk], ["Km"])
                    proj(w_qk[l, h], [(0, 96, CAr[:, 0, :], "CA"), (96, 96, CAr[:, 1, :], "CA")], [(0, 384)],
                         [(0, 96 * i, 96) for i in range(4)], cons_qk)
                    P.mm(ps[7][0:96, 0:4], sel4(h), DEC, True, True, ["DEC", "cst"], [PK(7)])
                    P.cp(DB, ps[7][0:96, 0:4], [PK(7)], ["DB"])
                    kC, kN = "mlC%d" % h, "mlN%d" % h
                    for ch in range(4):
                        cols = slice(128 * ch, 128 * (ch + 1))
                        P.mm(ps[0][:, 0:128], Km[:, 0, cols], Qm[:, 0, cols], True, False, ["Km", "Qm"], [PK(0)])
                        P.mm(ps[0][:, 0:128], Km[:, 1, cols], Qm[:, 1, cols], False, True, ["Km", "Qm"], [PK(0)])
                        P.stt(PT, ps[0][:, 0:128], AC[:, ch, h:h + 1], m128, ALU.mult, ALU.mult, [PK(0), "AC", "cst"], ["PT"])
                        for j in range(2):
                            P.tr(ps[1][:, 96 * j:96 * (j + 1)], Vh[:, j, cols], ident[0:96, 0:96], ["Vh", "cst"], [PK(1)])
                        P.cp(VTm, ps[1][:, 0:192], [PK(1)], ["VTm"], eng="act")
                        for j in range(2):
                            P.tr(ps[1][:, 192 + 96 * j:192 + 96 * (j + 1)], Km[:, j, cols], ident[0:96, 0:96], ["Km", "cst"], [PK(1)])
                        P.ts(KTa, ps[1][:, 192:384], AC[:, ch, h:h + 1], None, ALU.mult, None, [PK(1), "AC"], ["KTa"])
                        for j in range(2):
                            pn = ps[2 + j]
                            P.mm(pn[0:96, cols], VTm[:, 96 * j:96 * (j + 1)], PT, True, False, ["VTm", "PT"], [PK(2 + j)])
                            P.mm(pn[0:96, cols], mlC[:, h, 0, 96 * j:96 * (j + 1)], Qm[:, 0, cols], False, False, [kC, "Qm"], [PK(2 + j)])
                            P.mm(pn[0:96, cols], mlC[:, h, 1, 96 * j:96 * (j + 1)], Qm[:, 1, cols], False, True, [kC, "Qm"], [PK(2 + j)])
                        P.mm(ps[4][0:96, cols], ones[:, 0:96], PT, True, False, ["cst", "PT"], [PK(4)])
                        P.mm(ps[4][0:96, cols], mlN[:, h, 0, :], Qm[:, 0, cols], False, False, [kN, "Qm"], [PK(4)])
                        P.mm(ps[4][0:96, cols], mlN[:, h, 1, :], Qm[:, 1, cols], False, True, [kN, "Qm"], [PK(4)])
                        for kt in range(2):
                            P.mm(ps[5][0:96, 192 * kt:192 * (kt + 1)], KTa[:, 96 * kt:96 * (kt + 1)], VTm, True, True, ["KTa", "VTm"], [PK(5)])
                            P.mm(ps[6][0:96, 96 * kt:96 * (kt + 1)], KTa[:, 96 * kt:96 * (kt + 1)], ones[:, 0:96], True, True, ["KTa", "cst"], [PK(6)])
                        Cf = mlC[:, h, :, :].rearrange("p a b -> p (a b)")
                        Nf = mlN[:, h, :, :].rearrange("p a b -> p (a b)")
                        P.tt(CTm, ps[5][0:96, 0:384], Cf, ALU.add, [PK(5), kC], ["CTm"])
                        P.ts(Cf, CTm, DB[:, ch:ch + 1], None, ALU.mult, None, ["CTm", "DB"], [kC])
                        P.tt(CTm[:, 0:192], ps[6][0:96, 0:192], Nf, ALU.add, [PK(6), kN], ["CTm"])
                        P.ts(Nf, CTm[:, 0:192], DB[:, ch:ch + 1], None, ALU.mult, None, ["CTm", "DB"], [kN])
                    P.mm(ps[7][0:96, :], sel4(h), IG, True, True, ["IG", "cst"], [PK(7)])
                    P.cp(DS, ps[4][0:96, :], [PK(4)], ["DS"])
                    P.stt(DT2, DS, -1.0, DS, ALU.mult, ALU.max, ["DS"], ["DT2"])
                    P.tt(DT2, DT2, ps[7][0:96, :], ALU.max, ["DT2", PK(7)], ["DT2"])
                    P.add("dve", lambda e, DT2=DT2: e.reciprocal(DT2, DT2), ["DT2"], ["DT2"])
                    for j in range(2):
                        P.tt(HS[:, j, :], ps[2 + j][0:96, :], DT2, ALU.mult, [PK(2 + j), "DT2"], ["HS"])
                        P.actf(acc[:, j, :], HS[:, j, :], AF.Square, ["HS"], ["acc"])
                        P.mm(ps[7][0:96, :], ones[0:96, 0:96], acc[:, j, :], j == 0, j == 1, ["acc", "cst"], [PK(7)])
                    P.actf(DS, ps[7][0:96, :], AF.Sqrt, [PK(7)], ["DS"], bias=epsc[0:96, :], scale=1.0 / 192)
                    P.add("dve", lambda e, DS=DS: e.reciprocal(DS, DS), ["DS"], ["DS"])
                    for j in range(2):
                        fj = 2 * h + j
                        P.stt(HS[:, j, :], HS[:, j, :], vq[:, 40 + fj:41 + fj], DS, ALU.mult, ALU.mult, ["HS", "DS", "vq"], ["HS"])
                        P.tt(Ycr[:, fj, :], HS[:, j, :], OGs[:, j, :], ALU.mult, ["HS", "OGs"], ["Y"])
                P.barrier()
                branch([(96 * i, 96, Ycr[:, i, :], "Y") for i in range(8)], (2, 1280), True)

                for g0, ng in ((0, 6), (6, 6), (12, 4)):
                    def cons_o(i, pap, pk, g0=g0):
                        j = g0 + i
                        P.tt(xt[:, j, :], xt[:, j, :], pap, ALU.add, ["xt%d" % j, pk], ["xt%d" % j])
                    proj(w_out[l], [(128 * c, 128, mergedr[:, c, :], "mgr%d" % c) for c in range(NCH)],
                         [(128 * g0, 128 * ng)], [(0, 128 * i, 128) for i in range(ng)], cons_o)
                P.barrier()

                cv = Carve()
                sq2 = cv.t3(2, T); rs = cv.t2(T)
                rmsnorm(htr, lambda c: vp[:, 16 + c:17 + c], sq2, rs, "ht")
                ringB = cv.h3(6, D)
                actgr = cv.h3(12, T)
                SA = cv.t3(6, T)
                STG = cv.t3(12, 514); accB2 = cv.t3(2, T)

                def evac_to(base):
                    def f(i, pap, pk):
                        P.cp(STG[:, base + i, 2:514], pap, [pk], ["STG%d" % (base + i)], eng=("act" if i % 2 else "dve"))
                    return f

                def conv(slot, cidx, dst, kd):
                    st = STG[:, slot, :]
                    kst = "STG%d" % slot
                    P.cp(st[:, 0:2], ftl[:, cidx, :], ["ftl%d" % cidx], [kst])
                    P.actf(dst, st[:, 2:514], AF.Identity, [kst, "vp"], [kd],
                           bias=vp[:, 46 + cidx:47 + cidx], scale=vp[:, 134 + 88 * 2 + cidx:135 + 88 * 2 + cidx])
                    for k in range(2):
                        P.stt(dst, st[:, k:k + T], vp[:, 134 + 88 * k + cidx:135 + 88 * k + cidx], dst, ALU.mult, ALU.add, [kst, "vp", kd], [kd])
                    P.cp(ftl[:, cidx, :], st[:, 512:514], [kst], ["ftl%d" % cidx])

                def emit_down(g0, ng, ab):
                    for ii in range(ng):
                        P.dma("pool", ringB[:, ii, :], w_dn[l, 128 * (g0 + ii):128 * (g0 + ii + 1), :], (), ["ringB%d" % ii])
                    for j in range(NCH):
                        b = 6 + (j % 2)
                        for ii in range(ng):
                            P.mm(ps[b][:, :], ringB[:, ii, 128 * j:128 * (j + 1)], actgr[:, ab + ii, :], ii == 0, ii == ng - 1,
                                 ["ringB%d" % ii, "actg%d" % (ab + ii)], [PK(b)])
                        P.tt(xt[:, j, :], xt[:, j, :], ps[b][:, :], ALU.add, ["xt%d" % j, PK(b)], ["xt%d" % j])

                pend = None
                gi = 0
                g0 = 0
                while g0 < 44:
                    ng = min(6, 44 - g0)
                    ab = (gi % 2) * 6
                    tl = [(0, 128 * ii, 128) for ii in range(ng)]
                    proj(w_up[l], hk(), [(128 * g0, 128 * ng)], tl, evac_to(0))
                    for i in range(ng):
                        conv(i, g0 + i, SA[:, i, :], "SA%d" % i)
                        P.actf(SA[:, i, :], SA[:, i, :], AF.Silu, ["SA%d" % i], ["SA%d" % i])
                    proj(w_up[l], hk(), [(FFN + 128 * g0, 128 * ng)], tl, evac_to(6))
                    if pend is not None:
                        emit_down(*pend)
                    for i in range(ng):
                        ab_ = accB2[:, i % 2, :]
                        conv(6 + i, 44 + g0 + i, ab_, "accB%d" % (i % 2))
                        P.tt(actgr[:, ab + i, :], SA[:, i, :], ab_, ALU.mult, ["SA%d" % i, "accB%d" % (i % 2)], ["actg%d" % (ab + i)])
                    pend = (g0, ng, ab)
                    g0 += ng
                    gi += 1
                emit_down(*pend)
                P.barrier()

                if l < NL - 1:
                    P.dma("sp", xs_d[t], xt[:].rearrange("p a b -> p (a b)"), ["xt%d" % c for c in range(NCH)], ["xs%d" % t])
                else:
                    cv = Carve()
                    sq2 = cv.t3(2, T); rs = cv.t2(T)
                    xo = cv.t2(D)
                    hf = cv.t3(NCH, T)
                    rmsnorm(hf, lambda c: vp[:, 398 + c:399 + c], sq2, rs, "hf")
                    for tb in range(4):
                        for c in range(NCH):
                            P.tr(ps[c % 8][:, 0:128], hf[:, c, tb * 128:(tb + 1) * 128], ident, ["hf", "cst"], [PK(c % 8)])
                            P.cp(xo[:, c * 128:(c + 1) * 128], ps[c % 8][:, 0:128], [PK(c % 8)], ["xo"], eng=("act" if c % 2 else "dve"))
                        P.dma("sp", y_d[tok0 + tb * 128:tok0 + (tb + 1) * 128, :], xo, ["xo"], ["y"])
                P.barrier()
        P.emit()
    return nc


WNAMES = ["mix_norm", "w_in", "s5_lam_re", "s5_lam_im", "s5_log_dt", "s5_b_re", "s5_b_im", "s5_c_re", "s5_c_im",
          "s5_d", "s5_w_glu", "s5_b_glu", "hg_lower_bounds", "hg_norm", "ml_conv_w", "ml_conv_b", "ml_w_qk",
          "ml_b_ig", "ml_b_fg", "ml_norm", "w_branch", "w_out", "ffn_norm", "ffn_w_up", "ffn_conv_w", "ffn_conv_b",
          "ffn_w_down", "final_norm"]


def run(inputs, NL, NT, ncores):
    nc = build(NL, NT)
    x = np.asarray(inputs["x"], np.float32)
    B = x.shape[0]
    cstv = make_consts()
    shared = {k: np.ascontiguousarray(np.asarray(inputs[k], np.float32)) for k in WNAMES}
    in_maps = []
    for c in range(ncores):
        m = dict(shared)
        m["x"] = np.ascontiguousarray(x[c % B])
        m["cst"] = cstv
        in_maps.append(m)
    res = run_bass_kernel_spmd(nc, in_maps, core_ids=list(range(ncores)))
    return np.stack([res.results[b]["y"] for b in range(B)], axis=0).astype(np.float32)


def kernel(**inputs):
    return run(inputs, 4, 8, 8)
```

```python
import numpy as np
from contextlib import ExitStack
import concourse.bass as bass
import concourse.mybir as mybir

F32 = mybir.dt.float32
F32R = mybir.dt.float32r
BF = mybir.dt.bfloat16
ALU = mybir.AluOpType
AF = mybir.ActivationFunctionType
AX = mybir.AxisListType

ENGS = ("pe", "act", "dve", "pool", "sp")
NDSEM = 24


class Op:
    __slots__ = ("eng", "fn", "waits", "done", "dma", "idx")


class Prog:
    def __init__(self, nc, es):
        self.nc = nc
        self.ops = {e: [] for e in ENGS}
        self.esem = {e: es.enter_context(nc.semaphore("sem_" + e)) for e in ENGS}
        self.ecnt = {e: 0 for e in ENGS}
        self.dsem = {q: [es.enter_context(nc.semaphore("dq_%s_%d" % (q, i))) for i in range(NDSEM)]
                     for q in ("sp", "pool", "act")}
        self.dcnt = {q: [0] * NDSEM for q in ("sp", "pool", "act")}
        self.dnext = {q: 0 for q in ("sp", "pool", "act")}
        self.waited = {e: {} for e in ENGS}
        self.lastw = {}
        self.readers = {}
        self.pending = {e: [] for e in ENGS}
        self.nops = 0

    def _need(self, eng, waits, dep):
        sem, val, deng, ddma = dep
        if (not ddma) and deng == eng and eng == "pe":
            return
        key = id(sem)
        if self.waited[eng].get(key, 0) >= val:
            return
        waits[key] = (sem, max(val, waits.get(key, (sem, 0))[1]))

    def add(self, eng, fn, reads=(), writes=(), dma=False):
        op = Op()
        op.eng = eng
        op.fn = fn
        op.dma = dma
        waits = {}
        for k in reads:
            w = self.lastw.get(k)
            if w is not None:
                self._need(eng, waits, w)
        for k in writes:
            w = self.lastw.get(k)
            if w is not None:
                self._need(eng, waits, w)
            for r in self.readers.get(k, ()):
                self._need(eng, waits, r)
        for dep in self.pending[eng]:
            self._need(eng, waits, dep)
        self.pending[eng] = []
        if dma:
            q = eng
            i = self.dnext[q]
            self.dnext[q] = (i + 1) % NDSEM
            sem = self.dsem[q][i]
            if self.dcnt[q][i] > 0:
                self._need(eng, waits, (sem, self.dcnt[q][i], eng, True))
            self.dcnt[q][i] += 16
            op.done = (sem, self.dcnt[q][i], eng, True)
        else:
            self.ecnt[eng] += 1
            op.done = (self.esem[eng], self.ecnt[eng], eng, False)
        op.waits = list(waits.values())
        for sem, val in op.waits:
            self.waited[eng][id(sem)] = val
        for k in reads:
            self.readers.setdefault(k, []).append(op.done)
        for k in writes:
            self.lastw[k] = op.done
            self.readers[k] = []
        self.ops[eng].append(op)
        self.nops += 1
        return op

    def barrier(self):
        deps = []
        for e in ENGS:
            if self.ecnt[e] > 0:
                deps.append((self.esem[e], self.ecnt[e], e, True))
        for q in self.dsem:
            for i in range(NDSEM):
                if self.dcnt[q][i] > 0:
                    deps.append((self.dsem[q][i], self.dcnt[q][i], q, True))
        for e in ENGS:
            self.pending[e] = list(deps)
        self.lastw = {}
        self.readers = {}

    def emit(self, final_waits_eng="sp"):
        nc = self.nc
        self.barrier()
        fin = self.pending[final_waits_eng]
        with nc.Block() as block:
            def run(e, eng):
                for op in self.ops[e]:
                    for sem, val in op.waits:
                        eng.wait_ge(sem, val)
                    ins = op.fn(eng)
                    sem, val, _, ddma = op.done
                    ins.then_inc(sem, 16 if ddma else 1)
                if e == final_waits_eng:
                    w = {}
                    for sem, val, _, _ in fin:
                        if self.waited[e].get(id(sem), 0) < val:
                            w[id(sem)] = (sem, max(val, w.get(id(sem), (sem, 0))[1]))
                    for sem, val in w.values():
                        eng.wait_ge(sem, val)

            @block.tensor
            def _(eng):
                run("pe", eng)

            @block.scalar
            def _(eng):
                run("act", eng)

            @block.vector
            def _(eng):
                run("dve", eng)

            @block.gpsimd
            def _(eng):
                run("pool", eng)

            @block.sync
            def _(eng):
                run("sp", eng)

    def mm(self, out, lhsT, rhs, start, stop, r, w):
        return self.add("pe", lambda e: e.matmul(out, lhsT, rhs, start=start, stop=stop), r, w)

    def tr(self, out, in_, ident, r, w):
        return self.add("pe", lambda e: e.transpose(out, in_, ident), r, w)

    def actf(self, out, in_, func, r, w, bias=None, scale=None):
        kw = {}
        if bias is not None:
            kw["bias"] = bias
        if scale is not None:
            kw["scale"] = scale
        return self.add("act", lambda e: e.activation(out, in_, func, **kw), r, w)

    def tt(self, out, a, b, op, r, w, eng="dve"):
        return self.add(eng, lambda e: e.tensor_tensor(out, a, b, op), r, w)

    def ts(self, out, a, s1, s2, op0, op1, r, w, eng="dve"):
        if op1 is None:
            return self.add(eng, lambda e: e.tensor_scalar(out, a, s1, None, op0), r, w)
        return self.add(eng, lambda e: e.tensor_scalar(out, a, s1, s2, op0, op1), r, w)

    def stt(self, out, a, s, b, op0, op1, r, w, eng="dve"):
        return self.add(eng, lambda e: e.scalar_tensor_tensor(out, a, s, b, op0, op1), r, w)

    def cp(self, out, a, r, w, eng="dve"):
        if eng == "act":
            return self.add("act", lambda e: e.copy(out, a), r, w)
        return self.add(eng, lambda e: e.tensor_copy(out, a), r, w)

    def scan(self, out, d0, d1, init, op0, op1, r, w):
        return self.add("dve", lambda e: e.tensor_tensor_scan(out, d0, d1, init, op0, op1), r, w)

    def memset(self, ap, val, w, eng="dve"):
        return self.add(eng, lambda e: e.memset(ap, val), (), w)

    def dma(self, q, out, in_, r, w):
        return self.add(q, lambda e: e.dma_start(out=out, in_=in_), r, w, dma=True)


import math
from concourse.bass_utils import run_bass_kernel_spmd

T = 512
D = 2048
NCH = 16
EPS = 1e-6
MAGIC = 12582912.0
TWO_PI = 2.0 * math.pi
GELU_C = math.sqrt(2.0 / math.pi)
FFN = 5632
IN_TOTAL = 12040
C_IDENT, C_ONES, C_M128, C_M64, C_TT, C_MG, C_EPS, C_ZERO, C_SEL = 0, 128, 256, 384, 448, 960, 962, 963, 964
NCST = 964 + 384
RW = 768
NRING = 8
RCOLS = 26624


def make_consts():
    c = np.zeros((128, NCST), np.float32)
    c[:, C_IDENT:C_IDENT + 128] = np.eye(128)
    c[:, C_ONES:C_ONES + 128] = 1.0
    c[:, C_M128:C_M128 + 128] = np.triu(np.ones((128, 128)))
    c[:64, C_M64:C_M64 + 64] = np.triu(np.ones((64, 64)))
    c[:, C_TT:C_TT + 512] = np.arange(1, 513)[None, :]
    c[:64, C_MG] = 1.0
    c[64:, C_MG + 1] = 1.0
    c[:, C_EPS] = EPS
    for h in range(4):
        c[h, C_SEL + h * 96:C_SEL + (h + 1) * 96] = 1.0
    return c


def build(NL, NT):
    nc = bass.Bass("TRN2", target_bir_lowering=False)
    es = ExitStack()

    def din(name, shape):
        return nc.dram_tensor(name, list(shape), F32, kind="ExternalInput").ap()

    x_d = din("x", [NT * T, D])
    cst_d = din("cst", [128, NCST])
    mix_norm = din("mix_norm", [NL, D]); w_in = din("w_in", [NL, D, IN_TOTAL])
    lam_re = din("s5_lam_re", [NL, 32, 64]); lam_im = din("s5_lam_im", [NL, 32, 64]); log_dt = din("s5_log_dt", [NL, 32])
    b_re = din("s5_b_re", [NL, 32, 64, 16]); b_im = din("s5_b_im", [NL, 32, 64, 16])
    c_re = din("s5_c_re", [NL, 32, 16, 64]); c_im = din("s5_c_im", [NL, 32, 16, 64])
    s5_d = din("s5_d", [NL, 512]); w_glu = din("s5_w_glu", [NL, 512, 512]); b_glu = din("s5_b_glu", [NL, 512])
    hg_lb = din("hg_lower_bounds", [NL, 768]); hg_norm = din("hg_norm", [NL, 768])
    ml_cw = din("ml_conv_w", [NL, 4, 768]); ml_cb = din("ml_conv_b", [NL, 768]); w_qk = din("ml_w_qk", [NL, 4, 192, 384])
    b_ig = din("ml_b_ig", [NL, 4]); b_fg = din("ml_b_fg", [NL, 4]); ml_norm = din("ml_norm", [NL, 768])
    w_br = din("w_branch", [NL, D, D]); w_out = din("w_out", [NL, D, D]); ffn_norm = din("ffn_norm", [NL, D])
    w_up = din("ffn_w_up", [NL, D, 2 * FFN]); f_cw = din("ffn_conv_w", [NL, 3, 2 * FFN]); f_cb = din("ffn_conv_b", [NL, 2 * FFN])
    w_dn = din("ffn_w_down", [NL, FFN, D]); fin_norm = din("final_norm", [D])
    y_d = nc.dram_tensor("y", [NT * T, D], F32, kind="ExternalOutput").ap()
    xs_d = nc.dram_tensor("xs_scr", [NT, 128, NCH * T], F32, kind="Internal").ap()
    lbfc_d = nc.dram_tensor("lbfc_scr", [NL, 16, 128, 512], F32, kind="Internal").ap()
    tab_d = nc.dram_tensor("tab_scr", [NL, 16, 128, 1024], F32, kind="Internal").ap()

    with es:
        P = Prog(nc, es)
        sbt = lambda n, s, d=F32: es.enter_context(nc.sbuf_tensor(n + "_sb", s, d))
        cst = sbt("cst", [128, NCST])
        xt = sbt("xt", [128, NCH, T])
        ht = sbt("ht", [128, NCH, T], BF)
        htr = ht[:]
        ring = sbt("ring", [128, NRING, 2, RW], BF)
        R = sbt("R", [128, RCOLS])
        vp = sbt("vp", [128, 420])
        vq = sbt("vq", [96, 48])
        lbs = sbt("lbs", [128, 4 * 6 * 2])
        s5st = sbt("s5st", [128, 2, 16])
        s5p = sbt("s5p", [128, 2, 16])
        hst = sbt("hst", [128, 6, 128])
        mlC = sbt("mlC", [96, 4, 2, 192])
        mlN = sbt("mlN", [96, 4, 2, 96])
        mlt = sbt("mlt", [96, 8, 3])
        ftl = sbt("ftl", [128, 88, 2])
        rc = sbt("rc", [4, 4])
        ps = [es.enter_context(nc.psum_tensor("psb%d" % i, [128, 512], F32)) for i in range(8)]
        PK = lambda b: "ps%d" % b

        ident = cst[:, C_IDENT:C_IDENT + 128]
        ones = cst[:, C_ONES:C_ONES + 128]
        m128 = cst[:, C_M128:C_M128 + 128]
        m64 = cst[0:64, C_M64:C_M64 + 64]
        tt_i = cst[:, C_TT:C_TT + 512]
        epsc = cst[:, C_EPS:C_EPS + 1]

        def sel4(h):
            return cst[0:4, C_SEL + 96 * h:C_SEL + 96 * (h + 1)]

        class Carve:
            def __init__(self, base=0):
                self.o = base

            def t2(self, n, parts=128):
                a = R[0:parts, self.o:self.o + n]
                self.o += n
                assert self.o <= RCOLS, self.o
                return a

            def t3(self, a, b, parts=128):
                return self.t2(a * b, parts).rearrange("p (a b) -> p a b", b=b)

            def h2(self, n, parts=128):
                return self.t2((n + 1) // 2, parts).bitcast(BF)[:, 0:n]

            def h3(self, a, b, parts=128):
                return self.t2(a * b // 2, parts).bitcast(BF).rearrange("p (a b) -> p a b", b=b)

        P.dma("sp", cst[:], cst_d, (), ["cst"])
        ring_i = [0]

        def proj(wsrc, kchunks, pieces, tiles, consume, banks=None, ringB=None):
            banks = banks or list(range(len(tiles)))
            nk = len(kchunks)
            ki = 0
            while ki < nk:
                r0, nr, rhs, rkey = kchunks[ki]
                pack = 1
                if nr == 128 and ki + 1 < nk and kchunks[ki + 1][1] == 128 and kchunks[ki + 1][0] == r0 + 128:
                    pack = 2
                slots = []
                for (c0, wd) in pieces:
                    s = ring_i[0] % NRING
                    ring_i[0] += 1
                    if pack == 2:
                        P.dma("pool", ring[:, s, :, 0:wd], wsrc[r0:r0 + 256, c0:c0 + wd].rearrange("(a p) c -> p a c", p=128),
                              (), ["ring%d" % s])
                    else:
                        P.dma("pool", ring[0:nr, s, 0, 0:wd], wsrc[r0:r0 + nr, c0:c0 + wd], (), ["ring%d" % s])
                    slots.append(s)
                for a in range(pack):
                    _, nr_a, rhs_a, rkey_a = kchunks[ki + a]
                    for ti, (pi, off, M) in enumerate(tiles):
                        s = slots[pi]
                        P.mm(ps[banks[ti]][0:M, :], ring[0:nr_a, s, a, off:off + M], rhs_a, ki + a == 0, ki + a == nk - 1,
                             ["ring%d" % s, rkey_a], [PK(banks[ti])])
                ki += pack
            for ti, (pi, off, M) in enumerate(tiles):
                consume(ti, ps[banks[ti]][0:M, :], PK(banks[ti]))

        def hk(keyprefix="ht"):
            return [(128 * c, 128, htr[:, c, :], "ht") for c in range(NCH)]

        def emit_sin(out, x, tk, kx, kt, kout):
            P.ts(tk, x, 1.0 / TWO_PI, MAGIC, ALU.mult, ALU.add, [kx], [kt])
            P.ts(tk, tk, -MAGIC, None, ALU.add, None, [kt], [kt])
            P.stt(x, tk, -TWO_PI, x, ALU.mult, ALU.add, [kt, kx], [kx])
            P.ts(tk, x, math.pi, TWO_PI, ALU.is_gt, ALU.mult, [kx], [kt])
            P.tt(x, x, tk, ALU.subtract, [kx, kt], [kx])
            P.ts(x, x, math.pi, -math.pi, ALU.min, ALU.max, [kx], [kx])
            P.actf(out, x, AF.Sin, [kx], [kout])

        def load_T(dst, src, nr, w, stage, kst, bank=7):
            P.dma("sp", stage[0:nr, 0:w], src, (), [kst])
            P.tr(ps[bank][0:w, 0:nr], stage[0:nr, 0:w], ident[0:nr, 0:nr], [kst, "cst"], [PK(bank)])
            P.cp(dst, ps[bank][0:w, 0:nr], [PK(bank)], ["vp"])

        def rmsnorm(dst, gcol, sq2, rs, kdst):
            for c in range(NCH):
                sq = sq2[:, c % 2, :]
                P.actf(sq, xt[:, c, :], AF.Square, ["xt%d" % c], ["sq%d" % (c % 2)])
                P.mm(ps[7][:, :], ones, sq, c == 0, c == NCH - 1, ["sq%d" % (c % 2), "cst"], [PK(7)])
            P.actf(rs, ps[7][:, :], AF.Sqrt, [PK(7)], ["rs"], bias=epsc, scale=1.0 / D)
            P.add("dve", lambda e: e.reciprocal(rs, rs), ["rs"], ["rs"])
            for c in range(NCH):
                P.stt(dst[:, c, :], xt[:, c, :], gcol(c), rs, ALU.mult, ALU.mult, ["xt%d" % c, "rs", "vp"], [kdst])

        cv = Carve()
        stg = cv.t2(128)
        lraw = cv.t2(NL * 6)
        for l in range(NL):
            load_T(lraw[:, l * 6:(l + 1) * 6], hg_lb[l].rearrange("(c p) -> c p", p=128), 6, 128, stg, "stg")
        ex = cv.t2(NL * 6)
        tot = cv.t2(6)
        P.actf(ex, lraw, AF.Exp, ["vp"], ["ex"])
        P.cp(tot, ex[:, 0:6], ["ex"], ["tot"])
        for l in range(1, NL):
            P.tt(tot, tot, ex[:, l * 6:(l + 1) * 6], ALU.add, ["tot", "ex"], ["tot"])
        P.add("dve", lambda e: e.reciprocal(tot, tot), ["tot"], ["tot"])
        P.memset(lbs[:, 0:6], 0.0, ["lbs"])
        for l in range(1, NL):
            if l == 1:
                P.cp(lbs[:, 6:12], ex[:, 6:12], ["ex"], ["lbs"])
            else:
                P.tt(lbs[:, l * 6:(l + 1) * 6], lbs[:, (l - 1) * 6:l * 6], ex[:, l * 6:(l + 1) * 6], ALU.add, ["lbs", "ex"], ["lbs"])
        for l in range(NL):
            if l > 0:
                P.tt(lbs[:, l * 6:(l + 1) * 6], lbs[:, l * 6:(l + 1) * 6], tot, ALU.mult, ["lbs", "tot"], ["lbs"])
        for l in range(NL):
            P.ts(lbs[:, 24 + l * 6:24 + (l + 1) * 6], lbs[:, l * 6:(l + 1) * 6], -1.0, 1.0, ALU.mult, ALU.add, ["lbs"], ["lbs"])
        P.barrier()

        for l in range(NL):
            cv = Carve()
            stg = cv.t2(128)
            load_T(vp[:, 0:16], mix_norm[l].rearrange("(c p) -> c p", p=128), 16, 128, stg, "stg")
            load_T(vp[:, 16:32], ffn_norm[l].rearrange("(c p) -> c p", p=128), 16, 128, stg, "stg")
            load_T(vp[:, 32:36], s5_d[l].rearrange("(c p) -> c p", p=128), 4, 128, stg, "stg")
            load_T(vp[:, 36:40], b_glu[l].rearrange("(c p) -> c p", p=128), 4, 128, stg, "stg")
            load_T(vp[:, 40:46], hg_norm[l].rearrange("(c p) -> c p", p=128), 6, 128, stg, "stg")
            load_T(vp[:, 46:134], f_cb[l].rearrange("(c p) -> c p", p=128), 88, 128, stg, "stg")
            for k in range(3):
                load_T(vp[:, 134 + 88 * k:134 + 88 * (k + 1)], f_cw[l, k].rearrange("(c p) -> c p", p=128), 88, 128, stg, "stg")
            load_T(vp[:, 398:414], fin_norm.rearrange("(c p) -> c p", p=128), 16, 128, stg, "stg")
            for k in range(4):
                load_T(vq[:, 8 * k:8 * (k + 1)], ml_cw[l, k].rearrange("(c p) -> c p", p=96), 8, 96, stg, "stg")
            load_T(vq[:, 32:40], ml_cb[l].rearrange("(c p) -> c p", p=96), 8, 96, stg, "stg")
            load_T(vq[:, 40:48], ml_norm[l].rearrange("(c p) -> c p", p=96), 8, 96, stg, "stg")
            P.dma("sp", rc[:, 1:2], b_ig[l].rearrange("(h o) -> h o", o=1), (), ["rc"])
            P.dma("sp", rc[:, 2:3], b_fg[l].rearrange("(h o) -> h o", o=1), (), ["rc"])
            P.memset(s5st[:], 0.0, ["s5st"]); P.memset(hst[:], 0.0, ["hst"]); P.memset(mlC[:], 0.0, ["mlC"])
            P.memset(mlN[:], 0.0, ["mlN"]); P.memset(mlt[:], 0.0, ["mlt"]); P.memset(ftl[:], 0.0, ["ftl"])
            P.memset(rc[:, 0:1], 0.0, ["rc"])

            L16 = cv.t3(3, 128, parts=16)
            LD = cv.t2(2, parts=16)
            P.dma("sp", L16[:, 0, :], lam_re[l].rearrange("(s g) n -> s (g n)", g=2), (), ["L16"])
            P.dma("sp", L16[:, 1, :], lam_im[l].rearrange("(s g) n -> s (g n)", g=2), (), ["L16"])
            P.dma("sp", LD, log_dt[l].rearrange("(s g) -> s g", g=2), (), ["LD"])
            P.cp(L16[:, 2, :].rearrange("p (g n) -> p g n", n=64), LD.unsqueeze(2).to_broadcast([16, 2, 64]), ["LD"], ["L16"])
            sp = cv.t3(12, 16)
            for i in range(3):
                P.tr(ps[7][:, 0:16], L16[:, i, :], ident[0:16, 0:16], ["L16", "cst"], [PK(7)])
                P.cp(sp[:, i, :], ps[7][:, 0:16], [PK(7)], ["sp"])
            lr, li, dt_ = sp[:, 0, :], sp[:, 1, :], sp[:, 2, :]
            tmpa, tmpb = sp[:, 3, :], sp[:, 4, :]
            cosv, sinv, ar, ai, den, cr, ci = (sp[:, i, :] for i in range(5, 12))
            P.actf(dt_, dt_, AF.Exp, ["sp"], ["sp"])
            P.tt(tmpa, lr, dt_, ALU.mult, ["sp"], ["sp"])
            P.actf(s5p[:, 0, :], tmpa, AF.Exp, ["sp"], ["s5p"])
            P.tt(s5p[:, 1, :], li, dt_, ALU.mult, ["sp"], ["s5p"])
            P.cp(tmpa, s5p[:, 1, :], ["s5p"], ["sp"])
            emit_sin(sinv, tmpa, tmpb, "sp", "sp", "sp")
            P.ts(tmpa, s5p[:, 1, :], math.pi / 2, None, ALU.add, None, ["s5p", "sp"], ["sp"])
            emit_sin(cosv, tmpa, tmpb, "sp", "sp", "sp")
            P.tt(ar, s5p[:, 0, :], cosv, ALU.mult, ["sp", "s5p"], ["sp"])
            P.tt(ai, s5p[:, 0, :], sinv, ALU.mult, ["sp", "s5p"], ["sp"])
            P.tt(den, lr, lr, ALU.mult, ["sp"], ["sp"])
            P.tt(tmpa, li, li, ALU.mult, ["sp"], ["sp"])
            P.tt(den, den, tmpa, ALU.add, ["sp"], ["sp"])
            P.add("dve", lambda e: e.reciprocal(den, den), ["sp"], ["sp"])
            P.ts(ar, ar, -1.0, None, ALU.add, None, ["sp"], ["sp"])
            P.tt(cr, ar, lr, ALU.mult, ["sp"], ["sp"])
            P.tt(tmpa, ai, li, ALU.mult, ["sp"], ["sp"])
            P.tt(cr, cr, tmpa, ALU.add, ["sp"], ["sp"])
            P.tt(cr, cr, den, ALU.mult, ["sp"], ["sp"])
            P.tt(ci, ai, lr, ALU.mult, ["sp"], ["sp"])
            P.tt(tmpa, ar, li, ALU.mult, ["sp"], ["sp"])
            P.tt(ci, ci, tmpa, ALU.subtract, ["sp"], ["sp"])
            P.tt(ci, ci, den, ALU.mult, ["sp"], ["sp"])
            TB = cv.t3(2, 1024); tx = cv.t2(T); tk_ = cv.t2(T)
            for s in range(16):
                tb = TB[:, s % 2, :]
                P.ts(tx, tt_i, s5p[:, 1, s:s + 1], math.pi / 2, ALU.mult, ALU.add, ["cst", "s5p"], ["tx"])
                emit_sin(tb[:, 0:T], tx, tk_, "tx", "tk", "TB%d" % (s % 2))
                P.ts(tx, tt_i, s5p[:, 1, s:s + 1], None, ALU.mult, None, ["cst", "s5p"], ["tx"])
                emit_sin(tb[:, T:2 * T], tx, tk_, "tx", "tk", "TB%d" % (s % 2))
                P.dma("sp", tab_d[l, s], tb, ["TB%d" % (s % 2)], ["tab%d" % s])
            BR = cv.t3(16, 16); BI = cv.t3(16, 16); bbr = cv.t3(16, 16); bbi = cv.t3(16, 16); btmp = cv.t3(16, 16)
            P.dma("sp", BR, b_re[l].rearrange("(s g) n p -> (g n) s p", g=2), (), ["BR"])
            P.dma("sp", BI, b_im[l].rearrange("(s g) n p -> (g n) s p", g=2), (), ["BI"])
            crb = cr.unsqueeze(2).to_broadcast([128, 16, 16]); cib = ci.unsqueeze(2).to_broadcast([128, 16, 16])
            P.tt(bbr, BR, crb, ALU.mult, ["BR", "sp"], ["bbr"])
            P.tt(btmp, BI, cib, ALU.mult, ["BI", "sp"], ["btmp"])
            P.tt(bbr, bbr, btmp, ALU.subtract, ["bbr", "btmp"], ["bbr"])
            P.tt(bbi, BI, crb, ALU.mult, ["BI", "sp"], ["bbi"])
            P.tt(btmp, BR, cib, ALU.mult, ["BR", "sp"], ["btmp"])
            P.tt(bbi, bbi, btmp, ALU.add, ["bbi", "btmp"], ["bbi"])
            CI = cv.t3(2 * 16, 128, parts=16)
            cre = cv.t3(16, 16); cim = cv.t3(16, 16)
            P.dma("sp", CI[:, 0:16, :].rearrange("p s (g n) -> p s g n", g=2), c_re[l].rearrange("(s g) p n -> p s g n", g=2), (), ["CI"])
            P.dma("sp", CI[:, 16:32, :].rearrange("p s (g n) -> p s g n", g=2), c_im[l].rearrange("(s g) p n -> p s g n", g=2), (), ["CI"])
            for s in range(32):
                P.tr(ps[6][:, (s % 16) * 16:(s % 16 + 1) * 16], CI[:, s, :], ident[0:16, 0:16], ["CI", "cst"], [PK(6)])
                if s == 15:
                    P.cp(cre.rearrange("p a b -> p (a b)"), ps[6][:, 0:256], [PK(6)], ["cre"])
                if s == 31:
                    P.ts(cim.rearrange("p a b -> p (a b)"), ps[6][:, 0:256], -1.0, None, ALU.mult, None, [PK(6)], ["cim"])
            Fm = cv.t3(16, 128)
            Fst = cv.t3(4, 128)
            for mi, (src, ksrc, needT) in enumerate([(bbr, "bbr", True), (bbi, "bbi", True), (cre, "cre", False), (cim, "cim", False)]):
                P.memset(Fm, 0.0, ["Fm"])
                F4 = Fm.rearrange("p (a m) r -> p a m r", m=4)
                s4 = src.rearrange("p (a m) q -> p a m q", m=4)
                for m in range(4):
                    for gl in range(2):
                        P.ts(F4[:, :, m, 32 * m + 16 * gl:32 * m + 16 * gl + 16], s4[:, :, m, :],
                             cst[:, C_MG + gl:C_MG + gl + 1], None, ALU.mult, None, [ksrc, "cst"], ["Fm"])
                for s in range(16):
                    if needT:
                        P.tr(ps[s % 2][:, 0:128], Fm[:, s, :], ident, ["Fm", "cst"], [PK(s % 2)])
                        P.cp(Fst[:, s % 4, :], ps[s % 2][:, 0:128], [PK(s % 2)], ["Fst%d" % (s % 4)])
                        P.dma("sp", lbfc_d[l, s, :, 128 * mi:128 * (mi + 1)], Fst[:, s % 4, :], ["Fst%d" % (s % 4)], ["lbfc%d" % s])
                    else:
                        P.dma("sp", lbfc_d[l, s, :, 128 * mi:128 * (mi + 1)], Fm[:, s, :], ["Fm"], ["lbfc%d" % s])
            P.barrier()

            for t in range(NT):
                tok0 = t * T
                if l == 0:
                    cv = Carve()
                    xin = cv.t2(D)
                    for tb in range(4):
                        P.dma("sp", xin, x_d[tok0 + tb * 128:tok0 + (tb + 1) * 128, :], (), ["xin"])
                        for c in range(NCH):
                            P.tr(ps[c % 8][:, 0:128], xin[:, c * 128:(c + 1) * 128], ident, ["xin", "cst"], [PK(c % 8)])
                            P.cp(xt[:, c, tb * 128:(tb + 1) * 128], ps[c % 8][:, 0:128], [PK(c % 8)], ["xt%d" % c],
                                 eng=("act" if c % 2 else "dve"))
                else:
                    P.dma("sp", xt[:].rearrange("p a b -> p (a b)"), xs_d[t], ["xs%d" % t], ["xt%d" % c for c in range(NCH)])
                P.barrier()

                cv = Carve()
                merged = cv.t3(NCH, T)
                mergedr = cv.h3(NCH, T)
                Ybr = cv.h3(8, T)
                abase = cv.o
                SG = Carve(abase).t3(6, T)

                ca_ = Carve(abase)
                sq2 = ca_.t3(2, T); rs = ca_.t2(T)
                rmsnorm(htr, lambda c: vp[:, c:c + 1], sq2, rs, "ht")
                P.barrier()

                def branch(kchunks, first, last_br):
                    gcol0 = {0: 5896, 1: 5896 + D, 2: 5896 + 2 * D}[first[0]]
                    row0 = first[1]
                    for g0, ng in ((0, 6), (6, 6), (12, 4)):
                        def cons_g(i, pap, pk):
                            P.actf(SG[:, i, :], pap, AF.Sigmoid, [pk], ["SG%d" % i])
                        proj(w_in[l], hk(), [(gcol0 + 128 * g0, 128 * ng)], [(0, 128 * i, 128) for i in range(ng)], cons_g)

                        def cons_z(i, pap, pk, g0=g0):
                            j = g0 + i
                            if first[0] == 0:
                                P.tt(merged[:, j, :], SG[:, i, :], pap, ALU.mult, ["SG%d" % i, pk], ["mg%d" % j])
                            else:
                                P.tt(SG[:, i, :], SG[:, i, :], pap, ALU.mult, ["SG%d" % i, pk], ["SG%d" % i])
                                dstm = mergedr if last_br else merged
                                P.tt(dstm[:, j, :], merged[:, j, :], SG[:, i, :], ALU.add, ["SG%d" % i, "mg%d" % j],
                                     ["mgr%d" % j if last_br else "mg%d" % j])
                        kc = [(row0 + r0, nr, rhs, rk) for (r0, nr, rhs, rk) in kchunks]
                        proj(w_br[l], kc, [(128 * g0, 128 * ng)], [(0, 128 * i, 128) for i in range(ng)], cons_z)

                ca_ = Carve(abase)
                U = ca_.t3(4, T); G = ca_.t3(4, T); Gr_ = ca_.h3(4, T)
                CSN = ca_.t3(2, 2 * T); zr = ca_.t2(T); zi = ca_.t2(T); wr = ca_.t2(T); wi = ca_.t2(T)
                t1 = ca_.t2(T); t2_ = ca_.t2(T)
                LF = ca_.t3(2, 512)

                def cons_u(i, pap, pk):
                    P.cp(U[:, i, :], pap, [pk], ["U"], eng="act")
                proj(w_in[l], hk(), [(0, 512)], [(0, 128 * i, 128) for i in range(4)], cons_u, banks=[4, 5, 6, 7])
                for s in range(16):
                    c = s // 4
                    lf = LF[:, s % 2, :]
                    lfk = "LF%d" % (s % 2)
                    P.dma("sp", lf, lbfc_d[l, s], ["lbfc%d" % s], [lfk])
                    cs = CSN[:, s % 2, 0:T]; sn = CSN[:, s % 2, T:2 * T]
                    kcs = "csn%d" % (s % 2)
                    P.dma("sp", CSN[:, s % 2, :], tab_d[l, s], ["tab%d" % s], [kcs])
                    bre, bim = ps[4 + 2 * (s % 2)], ps[5 + 2 * (s % 2)]
                    kre, kim = PK(4 + 2 * (s % 2)), PK(5 + 2 * (s % 2))
                    P.mm(bre[:, :], lf[:, 0:128], U[:, c, :], True, True, [lfk, "U"], [kre])
                    P.mm(bim[:, :], lf[:, 128:256], U[:, c, :], True, True, [lfk, "U"], [kim])
                    P.tt(t1, bre[:, :], cs, ALU.mult, [kre, kcs], ["t1"])
                    P.tt(t2_, bim[:, :], sn, ALU.mult, [kim, kcs], ["t2"])
                    P.tt(zr, t1, t2_, ALU.add, ["t1", "t2"], ["zr"])
                    P.tt(t1, bim[:, :], cs, ALU.mult, [kim, kcs], ["t1"])
                    P.tt(t2_, bre[:, :], sn, ALU.mult, [kre, kcs], ["t2"])
                    P.tt(zi, t1, t2_, ALU.subtract, ["t1", "t2"], ["zi"])
                    magb = s5p[:, 0, s:s + 1].to_broadcast([128, T])
                    P.scan(wr, magb, zr, s5st[:, 0, s:s + 1], ALU.mult, ALU.add, ["zr", "s5p", "s5st"], ["wr"])
                    P.scan(wi, magb, zi, s5st[:, 1, s:s + 1], ALU.mult, ALU.add, ["zi", "s5p", "s5st"], ["wi"])
                    P.tt(t1, wr, cs, ALU.mult, ["wr", kcs], ["t1"])
                    P.tt(t2_, wi, sn, ALU.mult, ["wi", kcs], ["t2"])
                    P.tt(zr, t1, t2_, ALU.subtract, ["t1", "t2"], ["zr"])
                    P.tt(t1, wr, sn, ALU.mult, ["wr", kcs], ["t1"])
                    P.tt(t2_, wi, cs, ALU.mult, ["wi", kcs], ["t2"])
                    P.tt(zi, t1, t2_, ALU.add, ["t1", "t2"], ["zi"])
                    P.cp(s5st[:, 0, s:s + 1], zr[:, T - 1:T], ["zr"], ["s5st"])
                    P.cp(s5st[:, 1, s:s + 1], zi[:, T - 1:T], ["zi"], ["s5st"])
                    P.mm(ps[c][:, :], lf[:, 256:384], zr, s % 4 == 0, False, [lfk, "zr"], [PK(c)])
                    P.mm(ps[c][:, :], lf[:, 384:512], zi, False, s % 4 == 3, [lfk, "zi"], [PK(c)])
                for c in range(4):
                    P.stt(t1, U[:, c, :], vp[:, 32 + c:33 + c], ps[c][:, :], ALU.mult, ALU.add, ["U", "vp", PK(c)], ["t1"])
                    P.tt(t2_, t1, t1, ALU.mult, ["t1"], ["t2"])
                    P.ts(t2_, t2_, 0.044715, 1.0, ALU.mult, ALU.add, ["t2"], ["t2"])
                    P.tt(t2_, t2_, t1, ALU.mult, ["t1", "t2"], ["t2"])
                    P.actf(t2_, t2_, AF.Sigmoid, ["t2"], ["t2"], scale=2.0 * GELU_C)
                    P.tt(G[:, c, :], t1, t2_, ALU.mult, ["t1", "t2"], ["G"])
                    P.cp(Gr_[:, c, :], G[:, c, :], ["G"], ["Gr"], eng="act")

                def cons_glu(i, pap, pk):
                    P.actf(t1, pap, AF.Sigmoid, [pk], ["t1"], bias=vp[:, 36 + i:37 + i], scale=1.0)
                    P.tt(Ybr[:, i, :], G[:, i, :], t1, ALU.mult, ["G", "t1"], ["Y"])
                proj(w_glu[l], [(128 * c, 128, Gr_[:, c, :], "Gr") for c in range(4)], [(0, 512)],
                     [(0, 128 * i, 128) for i in range(4)], cons_glu)
                P.barrier()
                branch([(128 * c, 128, Ybr[:, c, :], "Y") for c in range(4)], (0, 0), False)
                P.barrier()

                for half in range(2):
                    ca_ = Carve(abase)
                    Qb = ca_.t3(3, T); Fb = ca_.t3(3, T); Vb = ca_.t3(3, T); OGb = ca_.t3(3, T)
                    T1 = ca_.t2(T); T2 = ca_.t2(T); CM = ca_.t2(T); D3 = ca_.t2(T)
                    AB = ca_.t3(6, T)
                    VT3 = ca_.t3(3, 128, parts=64); KT3 = ca_.t3(3, 128, parts=64); SM3 = ca_.t3(3, 64, parts=64)
                    EL = ca_.t3(3, 8)

                    def cons1(i, pap, pk):
                        if i < 3:
                            P.actf(Qb[:, i, :], pap, AF.Silu, [pk], ["Qb%d" % i])
                        else:
                            P.actf(Fb[:, i - 3, :], pap, AF.Sigmoid, [pk], ["Fb%d" % (i - 3)])
                    proj(w_in[l], hk(), [(512 + 384 * half, 384), (1280 + 384 * half, 384)],
                         [(0, 0, 128), (0, 128, 128), (0, 256, 128), (1, 0, 128), (1, 128, 128), (1, 256, 128)], cons1)

                    def cons2(i, pap, pk):
                        if i < 3:
                            P.cp(Vb[:, i, :], pap, [pk], ["Vb%d" % i], eng="act")
                        else:
                            P.actf(OGb[:, i - 3, :], pap, AF.Silu, [pk], ["OGb%d" % (i - 3)])
                    proj(w_in[l], hk(), [(2048 + 384 * half, 384), (2816 + 384 * half, 384)],
                         [(0, 0, 128), (0, 128, 128), (0, 256, 128), (1, 0, 128), (1, 128, 128), (1, 256, 128)], cons2)
                    for hh in range(3):
                        h = 3 * half + hh
                        q = Qb[:, hh, :]; f = Fb[:, hh, :]
                        kq, kf = "Qb%d" % hh, "Fb%d" % hh
                        A = AB[:, 2 * hh, :]; Bm = AB[:, 2 * hh + 1, :]
                        kA, kB = "A%d" % hh, "Bm%d" % hh
                        P.ts(f, f, lbs[:, 24 + l * 6 + h:24 + l * 6 + h + 1], lbs[:, l * 6 + h:l * 6 + h + 1], ALU.mult, ALU.add, [kf, "lbs"], [kf])
                        P.actf(T1, f, AF.Ln, [kf], ["T1"])
                        P.ts(f, f, -1.0, 1.0, ALU.mult, ALU.add, [kf], [kf])
                        P.scan(T2, ones[:, 0:1].to_broadcast([128, T]), T1, 0.0, ALU.mult, ALU.add, ["T1", "cst"], ["T2"])
                        T23 = T2.rearrange("p (a b) -> p a b", b=64); CM3 = CM.rearrange("p (a b) -> p a b", b=64)
                        P.cp(CM3[:, 0, :], T23[:, 0, :], ["T2"], ["CM"])
                        P.tt(CM3[:, 1:8, :], T23[:, 1:8, :], T23[:, 0:7, 63:64].to_broadcast([128, 7, 64]), ALU.subtract, ["T2"], ["CM"])
                        lastb = CM3[:, :, 63:64].to_broadcast([128, 8, 64])
                        D33 = D3.rearrange("p (a b) -> p a b", b=64)
                        P.stt(D33, lastb, -0.5, CM3, ALU.mult, ALU.add, ["CM"], ["D3"])
                        P.actf(T1, CM, AF.Exp, ["CM"], ["T1"])
                        P.tt(A, q, T1, ALU.mult, [kq, "T1"], [kA])
                        P.actf(T1, D3, AF.Exp, ["D3"], ["T1"])
                        P.tt(q, q, T1, ALU.mult, [kq, "T1"], [kq])
                        P.actf(T2, D3, AF.Exp, ["D3"], ["T2"], scale=-1.0)
                        P.tt(Bm, f, T2, ALU.mult, [kf, "T2"], [kB])
                        P.tt(D33, lastb, CM3, ALU.subtract, ["CM"], ["D3"])
                        P.actf(T1, D3, AF.Exp, ["D3"], ["T1"])
                        P.tt(f, f, T1, ALU.mult, [kf, "T1"], [kf])
                        P.actf(EL[:, hh, :], CM3[:, :, 63], AF.Exp, ["CM"], ["EL%d" % hh])
                    OSb = [T1, T2, CM]
                    for c in range(8):
                        cols = slice(64 * c, 64 * (c + 1))
                        for stage in range(10):
                            for hh in range(3):
                                h = 3 * half + hh
                                Ash = Qb[:, hh, :]; Kd = Fb[:, hh, :]; v = Vb[:, hh, :]
                                A = AB[:, 2 * hh, :]; Bm = AB[:, 2 * hh + 1, :]
                                kq, kf, kv = "Qb%d" % hh, "Fb%d" % hh, "Vb%d" % hh
                                kA, kB = "A%d" % hh, "Bm%d" % hh
                                VT = VT3[:, hh, :]; KT = KT3[:, hh, :]; SM = SM3[:, hh, :]
                                kVT, kKT, kSM = "VT%d" % hh, "KT%d" % hh, "SM%d" % hh
                                X = ps[2 * hh]; Yp = ps[2 * hh + 1]
                                kX, kY = PK(2 * hh), PK(2 * hh + 1)
                                S = hst[:, h, :]
                                ks = "hst%d" % h
                                OS = OSb[hh]; kOS = "OS%d" % hh
                                if stage == 0:
                                    P.mm(X[0:64, 0:64], Bm[:, cols], Ash[:, cols], True, True, [kB, kq], [kX])
                                    P.tr(Yp[0:64, 0:128], v[:, cols], ident, [kv, "cst"], [kY])
                                elif stage == 1:
                                    P.tt(SM, X[0:64, 0:64], m64, ALU.mult, [kX, "cst"], [kSM])
                                    P.cp(VT, Yp[0:64, 0:128], [kY], [kVT], eng="act")
                                elif stage == 2:
                                    P.tr(Yp[0:64, 0:128], Kd[:, cols], ident, [kf, "cst"], [kY])
                                elif stage == 3:
                                    P.cp(KT, Yp[0:64, 0:128], [kY], [kKT], eng="act")
                                elif stage == 4:
                                    P.mm(X[:, 0:64], VT, SM, True, False, [kVT, kSM], [kX])
                                    P.mm(X[:, 0:64], S, A[:, cols], False, True, [ks, kA], [kX])
                                elif stage == 5:
                                    P.cp(OS[:, cols], X[:, 0:64], [kX], [kOS] + (["T1", "T2", "CM"] if c == 0 else []), eng="act")
                                elif stage == 6:
                                    P.mm(X[:, 0:128], KT, VT, True, True, [kKT, kVT], [kX])
                                elif stage == 7:
                                    P.stt(S, S, EL[:, hh, c:c + 1], X[:, 0:128], ALU.mult, ALU.add, [ks, "EL%d" % hh, kX], [ks])
                    for hh in range(3):
                        h = 3 * half + hh
                        og = OGb[:, hh, :]; ko = "OGb%d" % hh
                        OS = OSb[hh]; kOS = "OS%d" % hh
                        P.actf(D3, OS, AF.Square, [kOS], ["D3"])
                        P.mm(ps[6][:, :], ones, D3, True, True, ["D3", "cst"], [PK(6)])
                        P.actf(D3, ps[6][:, :], AF.Sqrt, [PK(6)], ["D3"], bias=epsc, scale=1.0 / 128)
                        P.add("dve", lambda e, D3=D3: e.reciprocal(D3, D3), ["D3"], ["D3"])
                        P.stt(OS, OS, vp[:, 40 + h:41 + h], D3, ALU.mult, ALU.mult, [kOS, "D3", "vp"], [kOS])
                        P.tt(Ybr[:, h, :], OS, og, ALU.mult, [kOS, ko], ["Y"])
                P.barrier()
                branch([(128 * c, 128, Ybr[:, c, :], "Y") for c in range(6)], (1, 512), False)
                P.barrier()

                ca_ = Carve(abase)
                IG = ca_.t2(T, parts=4); LFr = ca_.t2(T, parts=4); Bc = ca_.t2(T, parts=4); Rr = ca_.t2(T, parts=4)
                AC = ca_.t3(4, 4); CS = ca_.t2(4, parts=4); DEC = ca_.t2(4, parts=4); DB = ca_.t2(4, parts=96)
                CXs = ca_.t3(2, 515, parts=96); Vh = ca_.t3(2, T, parts=96); OGs = ca_.t3(2, T, parts=96)
                acc = ca_.t3(2, T, parts=96); CAr = ca_.h3(2, T, parts=96)
                Qm = ca_.t3(2, T, parts=96); Km = ca_.t3(2, T, parts=96); HS = ca_.t3(2, T, parts=96)
                DS = ca_.t2(T, parts=96); DT2 = ca_.t2(T, parts=96)
                PT = ca_.t2(128); VTm = ca_.t2(192); KTa = ca_.t2(192); CTm = ca_.t2(384, parts=96)
                Ycr = Ybr[0:96, :, :]

                def cons_g4(i, pap, pk):
                    if i == 0:
                        P.actf(IG, pap, AF.Identity, [pk], ["IG"], bias=rc[:, 1:2], scale=1.0)
                    else:
                        P.actf(LFr, pap, AF.Sigmoid, [pk], ["LFr"], bias=rc[:, 2:3], scale=1.0)
                        P.actf(LFr, LFr, AF.Ln, ["LFr"], ["LFr"])
                proj(w_in[l], hk(), [(5888, 8)], [(0, 0, 4), (0, 4, 4)], cons_g4)
                ones4 = ones[0:4, 0:1].to_broadcast([4, T])
                P.scan(Bc, ones4, LFr, 0.0, ALU.mult, ALU.add, ["LFr", "cst"], ["Bc"])
                P.tt(IG, IG, Bc, ALU.subtract, ["IG", "Bc"], ["IG"])
                P.scan(Rr, ones4, IG, rc[:, 0:1], ALU.mult, ALU.max, ["IG", "rc", "cst"], ["Rr"])
                R3 = Rr.rearrange("p (a b) -> p a b", b=128)
                P.cp(CS[:, 0:1], rc[:, 0:1], ["rc"], ["CS"])
                P.cp(CS[:, 1:4], R3[:, 0:3, 127], ["Rr"], ["CS"])
                P.tt(DEC, CS, R3[:, :, 127], ALU.subtract, ["CS", "Rr"], ["DEC"])
                P.actf(DEC, DEC, AF.Exp, ["DEC"], ["DEC"])
                csb = CS.unsqueeze(2).to_broadcast([4, 4, 128])
                P.tt(LFr.rearrange("p (a b) -> p a b", b=128), IG.rearrange("p (a b) -> p a b", b=128), csb, ALU.subtract, ["IG", "CS"], ["LFr"])
                P.actf(LFr, LFr, AF.Exp, ["LFr"], ["LFr"])
                P.tt(IG.rearrange("p (a b) -> p a b", b=128), Bc.rearrange("p (a b) -> p a b", b=128), csb, ALU.add, ["Bc", "CS", "IG"], ["IG"])
                P.actf(IG, IG, AF.Exp, ["IG"], ["IG"], scale=-1.0)
                P.tt(rc[:, 0:1], Bc[:, T - 1:T], Rr[:, T - 1:T], ALU.add, ["Bc", "Rr"], ["rc"])
                for ch in range(4):
                    P.mm(ps[7][:, 4 * ch:4 * ch + 4], LFr[:, 128 * ch:128 * (ch + 1)], ident[0:4, 0:4], True, True, ["LFr", "cst"], [PK(7)])
                P.cp(AC.rearrange("p a b -> p (a b)"), ps[7][:, 0:16], [PK(7)], ["AC"])
                for h in range(4):
                    def cons_m(i, pap, pk, h=h):
                        j = i % 2
                        if i < 2:
                            P.cp(CXs[:, j, 3:515], pap, [pk], ["CXs%d" % j], eng="act")
                        elif i < 4:
                            P.cp(Vh[:, j, :], pap, [pk], ["Vh"], eng="act")
                        else:
                            P.actf(OGs[:, j, :], pap, AF.Sigmoid, [pk], ["OGs"])
                    proj(w_in[l], hk(), [(3584 + 192 * h, 192), (4352 + 192 * h, 192), (5120 + 192 * h, 192)],
                         [(0, 0, 96), (0, 96, 96), (1, 0, 96), (1, 96, 96), (2, 0, 96), (2, 96, 96)], cons_m)
                    for j in range(2):
                        fj = 2 * h + j
                        P.cp(CXs[:, j, 0:3], mlt[:, fj, :], ["mlt"], ["CXs%d" % j])
                        P.actf(acc[:, j, :], CXs[:, j, 3:515], AF.Identity, ["CXs%d" % j, "vq"], ["acc"],
                               bias=vq[:, 32 + fj:33 + fj], scale=vq[:, 24 + fj:25 + fj])
                        for k in range(3):
                            P.stt(acc[:, j, :], CXs[:, j, k:k + T], vq[:, 8 * k + fj:8 * k + fj + 1], acc[:, j, :], ALU.mult, ALU.add,
                                  ["CXs%d" % j, "vq", "acc"], ["acc"])
                        P.cp(mlt[:, fj, :], CXs[:, j, 512:515], ["CXs%d" % j], ["mlt"])
                        P.actf(CAr[:, j, :], acc[:, j, :], AF.Silu, ["acc"], ["CA"])

                    def cons_qk(i, pap, pk):
                        if i < 2:
                            P.cp(Qm[:, i, :], pap, [pk], ["Qm"], eng="act")
                        else:
                            P.ts(Km[:, i - 2, :], pap, 192.0 ** -0.5, None, ALU.mult, None, [pk], ["Km"])
                    proj(w_qk[l, h], [(0, 96, CAr[:, 0, :], "CA"), (96, 96, CAr[:, 1, :], "CA")], [(0, 384)],
                         [(0, 96 * i, 96) for i in range(4)], cons_qk)
                    P.mm(ps[7][0:96, 0:4], sel4(h), DEC, True, True, ["DEC", "cst"], [PK(7)])
                    P.cp(DB, ps[7][0:96, 0:4], [PK(7)], ["DB"])
                    kC, kN = "mlC%d" % h, "mlN%d" % h
                    for ch in range(4):
                        cols = slice(128 * ch, 128 * (ch + 1))
                        P.mm(ps[0][:, 0:128], Km[:, 0, cols], Qm[:, 0, cols], True, False, ["Km", "Qm"], [PK(0)])
                        P.mm(ps[0][:, 0:128], Km[:, 1, cols], Qm[:, 1, cols], False, True, ["Km", "Qm"], [PK(0)])
                        P.stt(PT, ps[0][:, 0:128], AC[:, ch, h:h + 1], m128, ALU.mult, ALU.mult, [PK(0), "AC", "cst"], ["PT"])
                        for j in range(2):
                            P.tr(ps[1][:, 96 * j:96 * (j + 1)], Vh[:, j, cols], ident[0:96, 0:96], ["Vh", "cst"], [PK(1)])
                        P.cp(VTm, ps[1][:, 0:192], [PK(1)], ["VTm"], eng="act")
                        for j in range(2):
                            P.tr(ps[1][:, 192 + 96 * j:192 + 96 * (j + 1)], Km[:, j, cols], ident[0:96, 0:96], ["Km", "cst"], [PK(1)])
                        P.ts(KTa, ps[1][:, 192:384], AC[:, ch, h:h + 1], None, ALU.mult, None, [PK(1), "AC"], ["KTa"])
                        for j in range(2):
                            pn = ps[2 + j]
                            P.mm(pn[0:96, cols], VTm[:, 96 * j:96 * (j + 1)], PT, True, False, ["VTm", "PT"], [PK(2 + j)])
                            P.mm(pn[0:96, cols], mlC[:, h, 0, 96 * j:96 * (j + 1)], Qm[:, 0, cols], False, False, [kC, "Qm"], [PK(2 + j)])
                            P.mm(pn[0:96, cols], mlC[:, h, 1, 96 * j:96 * (j + 1)], Qm[:, 1, cols], False, True, [kC, "Qm"], [PK(2 + j)])
                        P.mm(ps[4][0:96, cols], ones[:, 0:96], PT, True, False, ["cst", "PT"], [PK(4)])
                        P.mm(ps[4][0:96, cols], mlN[:, h, 0, :], Qm[:, 0, cols], False, False, [kN, "Qm"], [PK(4)])
                        P.mm(ps[4][0:96, cols], mlN[:, h, 1, :], Qm[:, 1, cols], False, True, [kN, "Qm"], [PK(4)])
                        for kt in range(2):
                            P.mm(ps[5][0:96, 192 * kt:192 * (kt + 1)], KTa[:, 96 * kt:96 * (kt + 1)], VTm, True, True, ["KTa", "VTm"], [PK(5)])
                            P.mm(ps[6][0:96, 96 * kt:96 * (kt + 1)], KTa[:, 96 * kt:96 * (kt + 1)], ones[:, 0:96], True, True, ["KTa", "cst"], [PK(6)])
                        Cf = mlC[:, h, :, :].rearrange("p a b -> p (a b)")
                        Nf = mlN[:, h, :, :].rearrange("p a b -> p (a b)")
                        P.tt(CTm, ps[5][0:96, 0:384], Cf, ALU.add, [PK(5), kC], ["CTm"])
                        P.ts(Cf, CTm, DB[:, ch:ch + 1], None, ALU.mult, None, ["CTm", "DB"], [kC])
                        P.tt(CTm[:, 0:192], ps[6][0:96, 0:192], Nf, ALU.add, [PK(6), kN], ["CTm"])
                        P.ts(Nf, CTm[:, 0:192], DB[:, ch:ch + 1], None, ALU.mult, None, ["CTm", "DB"], [kN])
                    P.mm(ps[7][0:96, :], sel4(h), IG, True, True, ["IG", "cst"], [PK(7)])
                    P.cp(DS, ps[4][0:96, :], [PK(4)], ["DS"])
                    P.stt(DT2, DS, -1.0, DS, ALU.mult, ALU.max, ["DS"], ["DT2"])
                    P.tt(DT2, DT2, ps[7][0:96, :], ALU.max, ["DT2", PK(7)], ["DT2"])
                    P.add("dve", lambda e, DT2=DT2: e.reciprocal(DT2, DT2), ["DT2"], ["DT2"])
                    for j in range(2):
                        P.tt(HS[:, j, :], ps[2 + j][0:96, :], DT2, ALU.mult, [PK(2 + j), "DT2"], ["HS"])
                        P.actf(acc[:, j, :], HS[:, j, :], AF.Square, ["HS"], ["acc"])
                        P.mm(ps[7][0:96, :], ones[0:96, 0:96], acc[:, j, :], j == 0, j == 1, ["acc", "cst"], [PK(7)])
                    P.actf(DS, ps[7][0:96, :], AF.Sqrt, [PK(7)], ["DS"], bias=epsc[0:96, :], scale=1.0 / 192)
                    P.add("dve", lambda e, DS=DS: e.reciprocal(DS, DS), ["DS"], ["DS"])
                    for j in range(2):
                        fj = 2 * h + j
                        P.stt(HS[:, j, :], HS[:, j, :], vq[:, 40 + fj:41 + fj], DS, ALU.mult, ALU.mult, ["HS", "DS", "vq"], ["HS"])
                        P.tt(Ycr[:, fj, :], HS[:, j, :], OGs[:, j, :], ALU.mult, ["HS", "OGs"], ["Y"])
                P.barrier()
                branch([(96 * i, 96, Ycr[:, i, :], "Y") for i in range(8)], (2, 1280), True)

                for g0, ng in ((0, 6), (6, 6), (12, 4)):
                    def cons_o(i, pap, pk, g0=g0):
                        j = g0 + i
                        P.tt(xt[:, j, :], xt[:, j, :], pap, ALU.add, ["xt%d" % j, pk], ["xt%d" % j])
                    proj(w_out[l], [(128 * c, 128, mergedr[:, c, :], "mgr%d" % c) for c in range(NCH)],
                         [(128 * g0, 128 * ng)], [(0, 128 * i, 128) for i in range(ng)], cons_o)
                P.barrier()

                cv = Carve()
                sq2 = cv.t3(2, T); rs = cv.t2(T)
                rmsnorm(htr, lambda c: vp[:, 16 + c:17 + c], sq2, rs, "ht")
                ringB = cv.h3(6, D)
                actgr = cv.h3(12, T)
                SA = cv.t3(6, T)
                STG = cv.t3(12, 514); accB2 = cv.t3(2, T)

                def evac_to(base):
                    def f(i, pap, pk):
                        P.cp(STG[:, base + i, 2:514], pap, [pk], ["STG%d" % (base + i)], eng=("act" if i % 2 else "dve"))
                    return f

                def conv(slot, cidx, dst, kd):
                    st = STG[:, slot, :]
                    kst = "STG%d" % slot
                    P.cp(st[:, 0:2], ftl[:, cidx, :], ["ftl%d" % cidx], [kst])
                    P.actf(dst, st[:, 2:514], AF.Identity, [kst, "vp"], [kd],
                           bias=vp[:, 46 + cidx:47 + cidx], scale=vp[:, 134 + 88 * 2 + cidx:135 + 88 * 2 + cidx])
                    for k in range(2):
                        P.stt(dst, st[:, k:k + T], vp[:, 134 + 88 * k + cidx:135 + 88 * k + cidx], dst, ALU.mult, ALU.add, [kst, "vp", kd], [kd])
                    P.cp(ftl[:, cidx, :], st[:, 512:514], [kst], ["ftl%d" % cidx])

                def emit_down(g0, ng, ab):
                    for ii in range(ng):
                        P.dma("pool", ringB[:, ii, :], w_dn[l, 128 * (g0 + ii):128 * (g0 + ii + 1), :], (), ["ringB%d" % ii])
                    for j in range(NCH):
                        b = 6 + (j % 2)
                        for ii in range(ng):
                            P.mm(ps[b][:, :], ringB[:, ii, 128 * j:128 * (j + 1)], actgr[:, ab + ii, :], ii == 0, ii == ng - 1,
                                 ["ringB%d" % ii, "actg%d" % (ab + ii)], [PK(b)])
                        P.tt(xt[:, j, :], xt[:, j, :], ps[b][:, :], ALU.add, ["xt%d" % j, PK(b)], ["xt%d" % j])

                pend = None
                gi = 0
                g0 = 0
                while g0 < 44:
                    ng = min(6, 44 - g0)
                    ab = (gi % 2) * 6
                    tl = [(0, 128 * ii, 128) for ii in range(ng)]
                    proj(w_up[l], hk(), [(128 * g0, 128 * ng)], tl, evac_to(0))
                    for i in range(ng):
                        conv(i, g0 + i, SA[:, i, :], "SA%d" % i)
                        P.actf(SA[:, i, :], SA[:, i, :], AF.Silu, ["SA%d" % i], ["SA%d" % i])
                    proj(w_up[l], hk(), [(FFN + 128 * g0, 128 * ng)], tl, evac_to(6))
                    if pend is not None:
                        emit_down(*pend)
                    for i in range(ng):
                        ab_ = accB2[:, i % 2, :]
                        conv(6 + i, 44 + g0 + i, ab_, "accB%d" % (i % 2))
                        P.tt(actgr[:, ab + i, :], SA[:, i, :], ab_, ALU.mult, ["SA%d" % i, "accB%d" % (i % 2)], ["actg%d" % (ab + i)])
                    pend = (g0, ng, ab)
                    g0 += ng
                    gi += 1
                emit_down(*pend)
                P.barrier()

                if l < NL - 1:
                    P.dma("sp", xs_d[t], xt[:].rearrange("p a b -> p (a b)"), ["xt%d" % c for c in range(NCH)], ["xs%d" % t])
                else:
                    cv = Carve()
                    sq2 = cv.t3(2, T); rs = cv.t2(T)
                    xo = cv.t2(D)
                    hf = cv.t3(NCH, T)
                    rmsnorm(hf, lambda c: vp[:, 398 + c:399 + c], sq2, rs, "hf")
                    for tb in range(4):
                        for c in range(NCH):
                            P.tr(ps[c % 8][:, 0:128], hf[:, c, tb * 128:(tb + 1) * 128], ident, ["hf", "cst"], [PK(c % 8)])
                            P.cp(xo[:, c * 128:(c + 1) * 128], ps[c % 8][:, 0:128], [PK(c % 8)], ["xo"], eng=("act" if c % 2 else "dve"))
                        P.dma("sp", y_d[tok0 + tb * 128:tok0 + (tb + 1) * 128, :], xo, ["xo"], ["y"])
                P.barrier()
        P.emit()
    return nc


WNAMES = ["mix_norm", "w_in", "s5_lam_re", "s5_lam_im", "s5_log_dt", "s5_b_re", "s5_b_im", "s5_c_re", "s5_c_im",
          "s5_d", "s5_w_glu", "s5_b_glu", "hg_lower_bounds", "hg_norm", "ml_conv_w", "ml_conv_b", "ml_w_qk",
          "ml_b_ig", "ml_b_fg", "ml_norm", "w_branch", "w_out", "ffn_norm", "ffn_w_up", "ffn_conv_w", "ffn_conv_b",
          "ffn_w_down", "final_norm"]


def run(inputs, NL, NT, ncores):
    nc = build(NL, NT)
    x = np.asarray(inputs["x"], np.float32)
    B = x.shape[0]
    cstv = make_consts()
    shared = {k: np.ascontiguousarray(np.asarray(inputs[k], np.float32)) for k in WNAMES}
    in_maps = []
    for c in range(ncores):
        m = dict(shared)
        m["x"] = np.ascontiguousarray(x[c % B])
        m["cst"] = cstv
        in_maps.append(m)
    res = run_bass_kernel_spmd(nc, in_maps, core_ids=list(range(ncores)))
    return np.stack([res.results[b]["y"] for b in range(B)], axis=0).astype(np.float32)


def kernel(**inputs):
    return run(inputs, 4, 8, 8)
```

```python
import numpy as np
from contextlib import ExitStack
import concourse.bass as bass
import concourse.mybir as mybir

F32 = mybir.dt.float32
F32R = mybir.dt.float32r
BF = mybir.dt.bfloat16
ALU = mybir.AluOpType
AF = mybir.ActivationFunctionType
AX = mybir.AxisListType

ENGS = ("pe", "act", "dve", "pool", "sp")
NDSEM = 24


class Op:
    __slots__ = ("eng", "fn", "waits", "done", "dma", "idx")


class Prog:
    def __init__(self, nc, es):
        self.nc = nc
        self.ops = {e: [] for e in ENGS}
        self.esem = {e: es.enter_context(nc.semaphore("sem_" + e)) for e in ENGS}
        self.ecnt = {e: 0 for e in ENGS}
        self.dsem = {q: [es.enter_context(nc.semaphore("dq_%s_%d" % (q, i))) for i in range(NDSEM)]
                     for q in ("sp", "pool", "act")}
        self.dcnt = {q: [0] * NDSEM for q in ("sp", "pool", "act")}
        self.dnext = {q: 0 for q in ("sp", "pool", "act")}
        self.waited = {e: {} for e in ENGS}
        self.lastw = {}
        self.readers = {}
        self.pending = {e: [] for e in ENGS}
        self.nops = 0

    def _need(self, eng, waits, dep):
        sem, val, deng, ddma = dep
        if (not ddma) and deng == eng and eng == "pe":
            return
        key = id(sem)
        if self.waited[eng].get(key, 0) >= val:
            return
        waits[key] = (sem, max(val, waits.get(key, (sem, 0))[1]))

    def add(self, eng, fn, reads=(), writes=(), dma=False):
        op = Op()
        op.eng = eng
        op.fn = fn
        op.dma = dma
        waits = {}
        for k in reads:
            w = self.lastw.get(k)
            if w is not None:
                self._need(eng, waits, w)
        for k in writes:
            w = self.lastw.get(k)
            if w is not None:
                self._need(eng, waits, w)
            for r in self.readers.get(k, ()):
                self._need(eng, waits, r)
        for dep in self.pending[eng]:
            self._need(eng, waits, dep)
        self.pending[eng] = []
        if dma:
            q = eng
            i = self.dnext[q]
            self.dnext[q] = (i + 1) % NDSEM
            sem = self.dsem[q][i]
            if self.dcnt[q][i] > 0:
                self._need(eng, waits, (sem, self.dcnt[q][i], eng, True))
            self.dcnt[q][i] += 16
            op.done = (sem, self.dcnt[q][i], eng, True)
        else:
            self.ecnt[eng] += 1
            op.done = (self.esem[eng], self.ecnt[eng], eng, False)
        op.waits = list(waits.values())
        for sem, val in op.waits:
            self.waited[eng][id(sem)] = val
        for k in reads:
            self.readers.setdefault(k, []).append(op.done)
        for k in writes:
            self.lastw[k] = op.done
            self.readers[k] = []
        self.ops[eng].append(op)
        self.nops += 1
        return op

    def barrier(self):
        deps = []
        for e in ENGS:
            if self.ecnt[e] > 0:
                deps.append((self.esem[e], self.ecnt[e], e, True))
        for q in self.dsem:
            for i in range(NDSEM):
                if self.dcnt[q][i] > 0:
                    deps.append((self.dsem[q][i], self.dcnt[q][i], q, True))
        for e in ENGS:
            self.pending[e] = list(deps)
        self.lastw = {}
        self.readers = {}

    def emit(self, final_waits_eng="sp"):
        nc = self.nc
        self.barrier()
        fin = self.pending[final_waits_eng]
        with nc.Block() as block:
            def run(e, eng):
                for op in self.ops[e]:
                    for sem, val in op.waits:
                        eng.wait_ge(sem, val)
                    ins = op.fn(eng)
                    sem, val, _, ddma = op.done
                    ins.then_inc(sem, 16 if ddma else 1)
                if e == final_waits_eng:
                    w = {}
                    for sem, val, _, _ in fin:
                        if self.waited[e].get(id(sem), 0) < val:
                            w[id(sem)] = (sem, max(val, w.get(id(sem), (sem, 0))[1]))
                    for sem, val in w.values():
                        eng.wait_ge(sem, val)

            @block.tensor
            def _(eng):
                run("pe", eng)

            @block.scalar
            def _(eng):
                run("act", eng)

            @block.vector
            def _(eng):
                run("dve", eng)

            @block.gpsimd
            def _(eng):
                run("pool", eng)

            @block.sync
            def _(eng):
                run("sp", eng)

    def mm(self, out, lhsT, rhs, start, stop, r, w):
        return self.add("pe", lambda e: e.matmul(out, lhsT, rhs, start=start, stop=stop), r, w)

    def tr(self, out, in_, ident, r, w):
        return self.add("pe", lambda e: e.transpose(out, in_, ident), r, w)

    def actf(self, out, in_, func, r, w, bias=None, scale=None):
        kw = {}
        if bias is not None:
            kw["bias"] = bias
        if scale is not None:
            kw["scale"] = scale
        return self.add("act", lambda e: e.activation(out, in_, func, **kw), r, w)

    def tt(self, out, a, b, op, r, w, eng="dve"):
        return self.add(eng, lambda e: e.tensor_tensor(out, a, b, op), r, w)

    def ts(self, out, a, s1, s2, op0, op1, r, w, eng="dve"):
        if op1 is None:
            return self.add(eng, lambda e: e.tensor_scalar(out, a, s1, None, op0), r, w)
        return self.add(eng, lambda e: e.tensor_scalar(out, a, s1, s2, op0, op1), r, w)

    def stt(self, out, a, s, b, op0, op1, r, w, eng="dve"):
        return self.add(eng, lambda e: e.scalar_tensor_tensor(out, a, s, b, op0, op1), r, w)

    def cp(self, out, a, r, w, eng="dve"):
        if eng == "act":
            return self.add("act", lambda e: e.copy(out, a), r, w)
        return self.add(eng, lambda e: e.tensor_copy(out, a), r, w)

    def scan(self, out, d0, d1, init, op0, op1, r, w):
        return self.add("dve", lambda e: e.tensor_tensor_scan(out, d0, d1, init, op0, op1), r, w)

    def memset(self, ap, val, w, eng="dve"):
        return self.add(eng, lambda e: e.memset(ap, val), (), w)

    def dma(self, q, out, in_, r, w):
        return self.add(q, lambda e: e.dma_start(out=out, in_=in_), r, w, dma=True)


import math
from concourse.bass_utils import run_bass_kernel_spmd

T = 512
D = 2048
NCH = 16
EPS = 1e-6
MAGIC = 12582912.0
TWO_PI = 2.0 * math.pi
GELU_C = math.sqrt(2.0 / math.pi)
FFN = 5632
IN_TOTAL = 12040
C_IDENT, C_ONES, C_M128, C_M64, C_TT, C_MG, C_EPS, C_ZERO, C_SEL = 0, 128, 256, 384, 448, 960, 962, 963, 964
NCST = 964 + 384
RW = 768
NRING = 8
RCOLS = 28160


def make_consts():
    c = np.zeros((128, NCST), np.float32)
    c[:, C_IDENT:C_IDENT + 128] = np.eye(128)
    c[:, C_ONES:C_ONES + 128] = 1.0
    c[:, C_M128:C_M128 + 128] = np.triu(np.ones((128, 128)))
    c[:64, C_M64:C_M64 + 64] = np.triu(np.ones((64, 64)))
    c[:, C_TT:C_TT + 512] = np.arange(1, 513)[None, :]
    c[:64, C_MG] = 1.0
    c[64:, C_MG + 1] = 1.0
    c[:, C_EPS] = EPS
    for h in range(4):
        c[h, C_SEL + h * 96:C_SEL + (h + 1) * 96] = 1.0
    return c


def build(NL, NT):
    nc = bass.Bass("TRN2", target_bir_lowering=False)
    es = ExitStack()

    def din(name, shape):
        return nc.dram_tensor(name, list(shape), F32, kind="ExternalInput").ap()

    x_d = din("x", [NT * T, D])
    cst_d = din("cst", [128, NCST])
    mix_norm = din("mix_norm", [NL, D]); w_in = din("w_in", [NL, D, IN_TOTAL])
    lam_re = din("s5_lam_re", [NL, 32, 64]); lam_im = din("s5_lam_im", [NL, 32, 64]); log_dt = din("s5_log_dt", [NL, 32])
    b_re = din("s5_b_re", [NL, 32, 64, 16]); b_im = din("s5_b_im", [NL, 32, 64, 16])
    c_re = din("s5_c_re", [NL, 32, 16, 64]); c_im = din("s5_c_im", [NL, 32, 16, 64])
    s5_d = din("s5_d", [NL, 512]); w_glu = din("s5_w_glu", [NL, 512, 512]); b_glu = din("s5_b_glu", [NL, 512])
    hg_lb = din("hg_lower_bounds", [NL, 768]); hg_norm = din("hg_norm", [NL, 768])
    ml_cw = din("ml_conv_w", [NL, 4, 768]); ml_cb = din("ml_conv_b", [NL, 768]); w_qk = din("ml_w_qk", [NL, 4, 192, 384])
    b_ig = din("ml_b_ig", [NL, 4]); b_fg = din("ml_b_fg", [NL, 4]); ml_norm = din("ml_norm", [NL, 768])
    w_br = din("w_branch", [NL, D, D]); w_out = din("w_out", [NL, D, D]); ffn_norm = din("ffn_norm", [NL, D])
    w_up = din("ffn_w_up", [NL, D, 2 * FFN]); f_cw = din("ffn_conv_w", [NL, 3, 2 * FFN]); f_cb = din("ffn_conv_b", [NL, 2 * FFN])
    w_dn = din("ffn_w_down", [NL, FFN, D]); fin_norm = din("final_norm", [D])
    y_d = nc.dram_tensor("y", [NT * T, D], F32, kind="ExternalOutput").ap()
    xs_d = nc.dram_tensor("xs_scr", [NT, 128, NCH * T], F32, kind="Internal").ap()
    lbfc_d = nc.dram_tensor("lbfc_scr", [NL, 16, 128, 512], F32, kind="Internal").ap()
    tab_d = nc.dram_tensor("tab_scr", [NL, 16, 128, 1024], F32, kind="Internal").ap()

    with es:
        P = Prog(nc, es)
        sbt = lambda n, s, d=F32: es.enter_context(nc.sbuf_tensor(n + "_sb", s, d))
        cst = sbt("cst", [128, NCST])
        xt = sbt("xt", [128, NCH, T])
        ht = sbt("ht", [128, NCH, T], BF)
        htr = ht[:]
        ring = sbt("ring", [128, NRING, 2, RW], BF)
        R = sbt("R", [128, RCOLS])
        vp = sbt("vp", [128, 420])
        vq = sbt("vq", [96, 48])
        lbs = sbt("lbs", [128, 4 * 6 * 2])
        s5st = sbt("s5st", [128, 2, 16])
        s5p = sbt("s5p", [128, 2, 16])
        hst = sbt("hst", [128, 6, 128])
        mlC = sbt("mlC", [96, 4, 2, 192])
        mlN = sbt("mlN", [96, 4, 2, 96])
        mlt = sbt("mlt", [96, 8, 3])
        ftl = sbt("ftl", [128, 88, 2])
        rc = sbt("rc", [4, 4])
        ps = [es.enter_context(nc.psum_tensor("psb%d" % i, [128, 512], F32)) for i in range(8)]
        PK = lambda b: "ps%d" % b

        ident = cst[:, C_IDENT:C_IDENT + 128]
        ones = cst[:, C_ONES:C_ONES + 128]
        m128 = cst[:, C_M128:C_M128 + 128]
        m64 = cst[0:64, C_M64:C_M64 + 64]
        tt_i = cst[:, C_TT:C_TT + 512]
        epsc = cst[:, C_EPS:C_EPS + 1]

        def sel4(h):
            return cst[0:4, C_SEL + 96 * h:C_SEL + 96 * (h + 1)]

        class Carve:
            def __init__(self, base=0):
                self.o = base

            def t2(self, n, parts=128):
                a = R[0:parts, self.o:self.o + n]
                self.o += n
                assert self.o <= RCOLS, self.o
                return a

            def t3(self, a, b, parts=128):
                return self.t2(a * b, parts).rearrange("p (a b) -> p a b", b=b)

            def h2(self, n, parts=128):
                return self.t2((n + 1) // 2, parts).bitcast(BF)[:, 0:n]

            def h3(self, a, b, parts=128):
                return self.t2(a * b // 2, parts).bitcast(BF).rearrange("p (a b) -> p a b", b=b)

        P.dma("sp", cst[:], cst_d, (), ["cst"])
        ring_i = [0]

        def proj(wsrc, kchunks, pieces, tiles, consume, banks=None, ringB=None):
            banks = banks or list(range(len(tiles)))
            nk = len(kchunks)
            ki = 0
            while ki < nk:
                r0, nr, rhs, rkey = kchunks[ki]
                pack = 1
                if nr == 128 and ki + 1 < nk and kchunks[ki + 1][1] == 128 and kchunks[ki + 1][0] == r0 + 128:
                    pack = 2
                slots = []
                for (c0, wd) in pieces:
                    s = ring_i[0] % NRING
                    ring_i[0] += 1
                    if pack == 2:
                        P.dma("pool", ring[:, s, :, 0:wd], wsrc[r0:r0 + 256, c0:c0 + wd].rearrange("(a p) c -> p a c", p=128),
                              (), ["ring%d" % s])
                    else:
                        P.dma("pool", ring[0:nr, s, 0, 0:wd], wsrc[r0:r0 + nr, c0:c0 + wd], (), ["ring%d" % s])
                    slots.append(s)
                for a in range(pack):
                    _, nr_a, rhs_a, rkey_a = kchunks[ki + a]
                    for ti, (pi, off, M) in enumerate(tiles):
                        s = slots[pi]
                        P.mm(ps[banks[ti]][0:M, :], ring[0:nr_a, s, a, off:off + M], rhs_a, ki + a == 0, ki + a == nk - 1,
                             ["ring%d" % s, rkey_a], [PK(banks[ti])])
                ki += pack
            for ti, (pi, off, M) in enumerate(tiles):
                consume(ti, ps[banks[ti]][0:M, :], PK(banks[ti]))

        def hk(keyprefix="ht"):
            return [(128 * c, 128, htr[:, c, :], "ht") for c in range(NCH)]

        def emit_sin(out, x, tk, kx, kt, kout):
            P.ts(tk, x, 1.0 / TWO_PI, MAGIC, ALU.mult, ALU.add, [kx], [kt])
            P.ts(tk, tk, -MAGIC, None, ALU.add, None, [kt], [kt])
            P.stt(x, tk, -TWO_PI, x, ALU.mult, ALU.add, [kt, kx], [kx])
            P.ts(tk, x, math.pi, TWO_PI, ALU.is_gt, ALU.mult, [kx], [kt])
            P.tt(x, x, tk, ALU.subtract, [kx, kt], [kx])
            P.ts(x, x, math.pi, -math.pi, ALU.min, ALU.max, [kx], [kx])
            P.actf(out, x, AF.Sin, [kx], [kout])

        def load_T(dst, src, nr, w, stage, kst, bank=7):
            P.dma("sp", stage[0:nr, 0:w], src, (), [kst])
            P.tr(ps[bank][0:w, 0:nr], stage[0:nr, 0:w], ident[0:nr, 0:nr], [kst, "cst"], [PK(bank)])
            P.cp(dst, ps[bank][0:w, 0:nr], [PK(bank)], ["vp"])

        def rmsnorm(dst, gcol, sq2, rs, kdst):
            for c in range(NCH):
                sq = sq2[:, c % 2, :]
                P.actf(sq, xt[:, c, :], AF.Square, ["xt%d" % c], ["sq%d" % (c % 2)])
                P.mm(ps[7][:, :], ones, sq, c == 0, c == NCH - 1, ["sq%d" % (c % 2), "cst"], [PK(7)])
            P.actf(rs, ps[7][:, :], AF.Sqrt, [PK(7)], ["rs"], bias=epsc, scale=1.0 / D)
            P.add("dve", lambda e: e.reciprocal(rs, rs), ["rs"], ["rs"])
            for c in range(NCH):
                P.stt(dst[:, c, :], xt[:, c, :], gcol(c), rs, ALU.mult, ALU.mult, ["xt%d" % c, "rs", "vp"], [kdst])

        cv = Carve()
        stg = cv.t2(128)
        lraw = cv.t2(NL * 6)
        for l in range(NL):
            load_T(lraw[:, l * 6:(l + 1) * 6], hg_lb[l].rearrange("(c p) -> c p", p=128), 6, 128, stg, "stg")
        ex = cv.t2(NL * 6)
        tot = cv.t2(6)
        P.actf(ex, lraw, AF.Exp, ["vp"], ["ex"])
        P.cp(tot, ex[:, 0:6], ["ex"], ["tot"])
        for l in range(1, NL):
            P.tt(tot, tot, ex[:, l * 6:(l + 1) * 6], ALU.add, ["tot", "ex"], ["tot"])
        P.add("dve", lambda e: e.reciprocal(tot, tot), ["tot"], ["tot"])
        P.memset(lbs[:, 0:6], 0.0, ["lbs"])
        for l in range(1, NL):
            if l == 1:
                P.cp(lbs[:, 6:12], ex[:, 6:12], ["ex"], ["lbs"])
            else:
                P.tt(lbs[:, l * 6:(l + 1) * 6], lbs[:, (l - 1) * 6:l * 6], ex[:, l * 6:(l + 1) * 6], ALU.add, ["lbs", "ex"], ["lbs"])
        for l in range(NL):
            if l > 0:
                P.tt(lbs[:, l * 6:(l + 1) * 6], lbs[:, l * 6:(l + 1) * 6], tot, ALU.mult, ["lbs", "tot"], ["lbs"])
        for l in range(NL):
            P.ts(lbs[:, 24 + l * 6:24 + (l + 1) * 6], lbs[:, l * 6:(l + 1) * 6], -1.0, 1.0, ALU.mult, ALU.add, ["lbs"], ["lbs"])
        P.barrier()

        for l in range(NL):
            cv = Carve()
            stg = cv.t2(128)
            load_T(vp[:, 0:16], mix_norm[l].rearrange("(c p) -> c p", p=128), 16, 128, stg, "stg")
            load_T(vp[:, 16:32], ffn_norm[l].rearrange("(c p) -> c p", p=128), 16, 128, stg, "stg")
            load_T(vp[:, 32:36], s5_d[l].rearrange("(c p) -> c p", p=128), 4, 128, stg, "stg")
            load_T(vp[:, 36:40], b_glu[l].rearrange("(c p) -> c p", p=128), 4, 128, stg, "stg")
            load_T(vp[:, 40:46], hg_norm[l].rearrange("(c p) -> c p", p=128), 6, 128, stg, "stg")
            load_T(vp[:, 46:134], f_cb[l].rearrange("(c p) -> c p", p=128), 88, 128, stg, "stg")
            for k in range(3):
                load_T(vp[:, 134 + 88 * k:134 + 88 * (k + 1)], f_cw[l, k].rearrange("(c p) -> c p", p=128), 88, 128, stg, "stg")
            load_T(vp[:, 398:414], fin_norm.rearrange("(c p) -> c p", p=128), 16, 128, stg, "stg")
            for k in range(4):
                load_T(vq[:, 8 * k:8 * (k + 1)], ml_cw[l, k].rearrange("(c p) -> c p", p=96), 8, 96, stg, "stg")
            load_T(vq[:, 32:40], ml_cb[l].rearrange("(c p) -> c p", p=96), 8, 96, stg, "stg")
            load_T(vq[:, 40:48], ml_norm[l].rearrange("(c p) -> c p", p=96), 8, 96, stg, "stg")
            P.dma("sp", rc[:, 1:2], b_ig[l].rearrange("(h o) -> h o", o=1), (), ["rc"])
            P.dma("sp", rc[:, 2:3], b_fg[l].rearrange("(h o) -> h o", o=1), (), ["rc"])
            P.memset(s5st[:], 0.0, ["s5st"]); P.memset(hst[:], 0.0, ["hst"]); P.memset(mlC[:], 0.0, ["mlC"])
            P.memset(mlN[:], 0.0, ["mlN"]); P.memset(mlt[:], 0.0, ["mlt"]); P.memset(ftl[:], 0.0, ["ftl"])
            P.memset(rc[:, 0:1], 0.0, ["rc"])

            L16 = cv.t3(3, 128, parts=16)
            LD = cv.t2(2, parts=16)
            P.dma("sp", L16[:, 0, :], lam_re[l].rearrange("(s g) n -> s (g n)", g=2), (), ["L16"])
            P.dma("sp", L16[:, 1, :], lam_im[l].rearrange("(s g) n -> s (g n)", g=2), (), ["L16"])
            P.dma("sp", LD, log_dt[l].rearrange("(s g) -> s g", g=2), (), ["LD"])
            P.cp(L16[:, 2, :].rearrange("p (g n) -> p g n", n=64), LD.unsqueeze(2).to_broadcast([16, 2, 64]), ["LD"], ["L16"])
            sp = cv.t3(12, 16)
            for i in range(3):
                P.tr(ps[7][:, 0:16], L16[:, i, :], ident[0:16, 0:16], ["L16", "cst"], [PK(7)])
                P.cp(sp[:, i, :], ps[7][:, 0:16], [PK(7)], ["sp"])
            lr, li, dt_ = sp[:, 0, :], sp[:, 1, :], sp[:, 2, :]
            tmpa, tmpb = sp[:, 3, :], sp[:, 4, :]
            cosv, sinv, ar, ai, den, cr, ci = (sp[:, i, :] for i in range(5, 12))
            P.actf(dt_, dt_, AF.Exp, ["sp"], ["sp"])
            P.tt(tmpa, lr, dt_, ALU.mult, ["sp"], ["sp"])
            P.actf(s5p[:, 0, :], tmpa, AF.Exp, ["sp"], ["s5p"])
            P.tt(s5p[:, 1, :], li, dt_, ALU.mult, ["sp"], ["s5p"])
            P.cp(tmpa, s5p[:, 1, :], ["s5p"], ["sp"])
            emit_sin(sinv, tmpa, tmpb, "sp", "sp", "sp")
            P.ts(tmpa, s5p[:, 1, :], math.pi / 2, None, ALU.add, None, ["s5p", "sp"], ["sp"])
            emit_sin(cosv, tmpa, tmpb, "sp", "sp", "sp")
            P.tt(ar, s5p[:, 0, :], cosv, ALU.mult, ["sp", "s5p"], ["sp"])
            P.tt(ai, s5p[:, 0, :], sinv, ALU.mult, ["sp", "s5p"], ["sp"])
            P.tt(den, lr, lr, ALU.mult, ["sp"], ["sp"])
            P.tt(tmpa, li, li, ALU.mult, ["sp"], ["sp"])
            P.tt(den, den, tmpa, ALU.add, ["sp"], ["sp"])
            P.add("dve", lambda e: e.reciprocal(den, den), ["sp"], ["sp"])
            P.ts(ar, ar, -1.0, None, ALU.add, None, ["sp"], ["sp"])
            P.tt(cr, ar, lr, ALU.mult, ["sp"], ["sp"])
            P.tt(tmpa, ai, li, ALU.mult, ["sp"], ["sp"])
            P.tt(cr, cr, tmpa, ALU.add, ["sp"], ["sp"])
            P.tt(cr, cr, den, ALU.mult, ["sp"], ["sp"])
            P.tt(ci, ai, lr, ALU.mult, ["sp"], ["sp"])
            P.tt(tmpa, ar, li, ALU.mult, ["sp"], ["sp"])
            P.tt(ci, ci, tmpa, ALU.subtract, ["sp"], ["sp"])
            P.tt(ci, ci, den, ALU.mult, ["sp"], ["sp"])
            TB = cv.t3(2, 1024); tx = cv.t2(T); tk_ = cv.t2(T)
            for s in range(16):
                tb = TB[:, s % 2, :]
                P.ts(tx, tt_i, s5p[:, 1, s:s + 1], math.pi / 2, ALU.mult, ALU.add, ["cst", "s5p"], ["tx"])
                emit_sin(tb[:, 0:T], tx, tk_, "tx", "tk", "TB%d" % (s % 2))
                P.ts(tx, tt_i, s5p[:, 1, s:s + 1], None, ALU.mult, None, ["cst", "s5p"], ["tx"])
                emit_sin(tb[:, T:2 * T], tx, tk_, "tx", "tk", "TB%d" % (s % 2))
                P.dma("sp", tab_d[l, s], tb, ["TB%d" % (s % 2)], ["tab%d" % s])
            BR = cv.t3(16, 16); BI = cv.t3(16, 16); bbr = cv.t3(16, 16); bbi = cv.t3(16, 16); btmp = cv.t3(16, 16)
            P.dma("sp", BR, b_re[l].rearrange("(s g) n p -> (g n) s p", g=2), (), ["BR"])
            P.dma("sp", BI, b_im[l].rearrange("(s g) n p -> (g n) s p", g=2), (), ["BI"])
            crb = cr.unsqueeze(2).to_broadcast([128, 16, 16]); cib = ci.unsqueeze(2).to_broadcast([128, 16, 16])
            P.tt(bbr, BR, crb, ALU.mult, ["BR", "sp"], ["bbr"])
            P.tt(btmp, BI, cib, ALU.mult, ["BI", "sp"], ["btmp"])
            P.tt(bbr, bbr, btmp, ALU.subtract, ["bbr", "btmp"], ["bbr"])
            P.tt(bbi, BI, crb, ALU.mult, ["BI", "sp"], ["bbi"])
            P.tt(btmp, BR, cib, ALU.mult, ["BR", "sp"], ["btmp"])
            P.tt(bbi, bbi, btmp, ALU.add, ["bbi", "btmp"], ["bbi"])
            CI = cv.t3(2 * 16, 128, parts=16)
            cre = cv.t3(16, 16); cim = cv.t3(16, 16)
            P.dma("sp", CI[:, 0:16, :].rearrange("p s (g n) -> p s g n", g=2), c_re[l].rearrange("(s g) p n -> p s g n", g=2), (), ["CI"])
            P.dma("sp", CI[:, 16:32, :].rearrange("p s (g n) -> p s g n", g=2), c_im[l].rearrange("(s g) p n -> p s g n", g=2), (), ["CI"])
            for s in range(32):
                P.tr(ps[6][:, (s % 16) * 16:(s % 16 + 1) * 16], CI[:, s, :], ident[0:16, 0:16], ["CI", "cst"], [PK(6)])
                if s == 15:
                    P.cp(cre.rearrange("p a b -> p (a b)"), ps[6][:, 0:256], [PK(6)], ["cre"])
                if s == 31:
                    P.ts(cim.rearrange("p a b -> p (a b)"), ps[6][:, 0:256], -1.0, None, ALU.mult, None, [PK(6)], ["cim"])
            Fm = cv.t3(16, 128)
            Fst = cv.t3(4, 128)
            for mi, (src, ksrc, needT) in enumerate([(bbr, "bbr", True), (bbi, "bbi", True), (cre, "cre", False), (cim, "cim", False)]):
                P.memset(Fm, 0.0, ["Fm"])
                F4 = Fm.rearrange("p (a m) r -> p a m r", m=4)
                s4 = src.rearrange("p (a m) q -> p a m q", m=4)
                for m in range(4):
                    for gl in range(2):
                        P.ts(F4[:, :, m, 32 * m + 16 * gl:32 * m + 16 * gl + 16], s4[:, :, m, :],
                             cst[:, C_MG + gl:C_MG + gl + 1], None, ALU.mult, None, [ksrc, "cst"], ["Fm"])
                for s in range(16):
                    if needT:
                        P.tr(ps[s % 2][:, 0:128], Fm[:, s, :], ident, ["Fm", "cst"], [PK(s % 2)])
                        P.cp(Fst[:, s % 4, :], ps[s % 2][:, 0:128], [PK(s % 2)], ["Fst%d" % (s % 4)])
                        P.dma("sp", lbfc_d[l, s, :, 128 * mi:128 * (mi + 1)], Fst[:, s % 4, :], ["Fst%d" % (s % 4)], ["lbfc%d" % s])
                    else:
                        P.dma("sp", lbfc_d[l, s, :, 128 * mi:128 * (mi + 1)], Fm[:, s, :], ["Fm"], ["lbfc%d" % s])
            P.barrier()

            for t in range(NT):
                tok0 = t * T
                if l == 0:
                    cv = Carve()
                    xin = cv.t2(D)
                    for tb in range(4):
                        P.dma("sp", xin, x_d[tok0 + tb * 128:tok0 + (tb + 1) * 128, :], (), ["xin"])
                        for c in range(NCH):
                            P.tr(ps[c % 8][:, 0:128], xin[:, c * 128:(c + 1) * 128], ident, ["xin", "cst"], [PK(c % 8)])
                            P.cp(xt[:, c, tb * 128:(tb + 1) * 128], ps[c % 8][:, 0:128], [PK(c % 8)], ["xt%d" % c],
                                 eng=("act" if c % 2 else "dve"))
                else:
                    P.dma("sp", xt[:].rearrange("p a b -> p (a b)"), xs_d[t], ["xs%d" % t], ["xt%d" % c for c in range(NCH)])
                P.barrier()

                cv = Carve()
                merged = cv.t3(NCH, T)
                mergedr = cv.h3(NCH, T)
                Ybr = cv.h3(8, T)
                abase = cv.o
                SG = Carve(abase).t3(6, T)

                ca_ = Carve(abase)
                sq2 = ca_.t3(2, T); rs = ca_.t2(T)
                rmsnorm(htr, lambda c: vp[:, c:c + 1], sq2, rs, "ht")
                P.barrier()

                def branch(kchunks, first, last_br):
                    gcol0 = {0: 5896, 1: 5896 + D, 2: 5896 + 2 * D}[first[0]]
                    row0 = first[1]
                    for g0, ng in ((0, 6), (6, 6), (12, 4)):
                        def cons_g(i, pap, pk):
                            P.actf(SG[:, i, :], pap, AF.Sigmoid, [pk], ["SG%d" % i])
                        proj(w_in[l], hk(), [(gcol0 + 128 * g0, 128 * ng)], [(0, 128 * i, 128) for i in range(ng)], cons_g)

                        def cons_z(i, pap, pk, g0=g0):
                            j = g0 + i
                            if first[0] == 0:
                                P.tt(merged[:, j, :], SG[:, i, :], pap, ALU.mult, ["SG%d" % i, pk], ["mg%d" % j])
                            else:
                                P.tt(SG[:, i, :], SG[:, i, :], pap, ALU.mult, ["SG%d" % i, pk], ["SG%d" % i])
                                dstm = mergedr if last_br else merged
                                P.tt(dstm[:, j, :], merged[:, j, :], SG[:, i, :], ALU.add, ["SG%d" % i, "mg%d" % j],
                                     ["mgr%d" % j if last_br else "mg%d" % j])
                        kc = [(row0 + r0, nr, rhs, rk) for (r0, nr, rhs, rk) in kchunks]
                        proj(w_br[l], kc, [(128 * g0, 128 * ng)], [(0, 128 * i, 128) for i in range(ng)], cons_z)

                ca_ = Carve(abase)
                U = ca_.t3(4, T); G = ca_.t3(4, T); Gr_ = ca_.h3(4, T)
                CSN = ca_.t3(2, 2 * T); zr = ca_.t2(T); zi = ca_.t2(T); wr = ca_.t2(T); wi = ca_.t2(T)
                t1 = ca_.t2(T); t2_ = ca_.t2(T)
                LF = ca_.t3(2, 512)

                def cons_u(i, pap, pk):
                    P.cp(U[:, i, :], pap, [pk], ["U"], eng="act")
                proj(w_in[l], hk(), [(0, 512)], [(0, 128 * i, 128) for i in range(4)], cons_u, banks=[4, 5, 6, 7])
                for s in range(16):
                    c = s // 4
                    lf = LF[:, s % 2, :]
                    lfk = "LF%d" % (s % 2)
                    P.dma("sp", lf, lbfc_d[l, s], ["lbfc%d" % s], [lfk])
                    cs = CSN[:, s % 2, 0:T]; sn = CSN[:, s % 2, T:2 * T]
                    kcs = "csn%d" % (s % 2)
                    P.dma("sp", CSN[:, s % 2, :], tab_d[l, s], ["tab%d" % s], [kcs])
                    bre, bim = ps[4 + 2 * (s % 2)], ps[5 + 2 * (s % 2)]
                    kre, kim = PK(4 + 2 * (s % 2)), PK(5 + 2 * (s % 2))
                    P.mm(bre[:, :], lf[:, 0:128], U[:, c, :], True, True, [lfk, "U"], [kre])
                    P.mm(bim[:, :], lf[:, 128:256], U[:, c, :], True, True, [lfk, "U"], [kim])
                    P.tt(t1, bre[:, :], cs, ALU.mult, [kre, kcs], ["t1"])
                    P.tt(t2_, bim[:, :], sn, ALU.mult, [kim, kcs], ["t2"])
                    P.tt(zr, t1, t2_, ALU.add, ["t1", "t2"], ["zr"])
                    P.tt(t1, bim[:, :], cs, ALU.mult, [kim, kcs], ["t1"])
                    P.tt(t2_, bre[:, :], sn, ALU.mult, [kre, kcs], ["t2"])
                    P.tt(zi, t1, t2_, ALU.subtract, ["t1", "t2"], ["zi"])
                    magb = s5p[:, 0, s:s + 1].to_broadcast([128, T])
                    P.scan(wr, magb, zr, s5st[:, 0, s:s + 1], ALU.mult, ALU.add, ["zr", "s5p", "s5st"], ["wr"])
                    P.scan(wi, magb, zi, s5st[:, 1, s:s + 1], ALU.mult, ALU.add, ["zi", "s5p", "s5st"], ["wi"])
                    P.tt(t1, wr, cs, ALU.mult, ["wr", kcs], ["t1"])
                    P.tt(t2_, wi, sn, ALU.mult, ["wi", kcs], ["t2"])
                    P.tt(zr, t1, t2_, ALU.subtract, ["t1", "t2"], ["zr"])
                    P.tt(t1, wr, sn, ALU.mult, ["wr", kcs], ["t1"])
                    P.tt(t2_, wi, cs, ALU.mult, ["wi", kcs], ["t2"])
                    P.tt(zi, t1, t2_, ALU.add, ["t1", "t2"], ["zi"])
                    P.cp(s5st[:, 0, s:s + 1], zr[:, T - 1:T], ["zr"], ["s5st"])
                    P.cp(s5st[:, 1, s:s + 1], zi[:, T - 1:T], ["zi"], ["s5st"])
                    P.mm(ps[c][:, :], lf[:, 256:384], zr, s % 4 == 0, False, [lfk, "zr"], [PK(c)])
                    P.mm(ps[c][:, :], lf[:, 384:512], zi, False, s % 4 == 3, [lfk, "zi"], [PK(c)])
                for c in range(4):
                    P.stt(t1, U[:, c, :], vp[:, 32 + c:33 + c], ps[c][:, :], ALU.mult, ALU.add, ["U", "vp", PK(c)], ["t1"])
                    P.tt(t2_, t1, t1, ALU.mult, ["t1"], ["t2"])
                    P.ts(t2_, t2_, 0.044715, 1.0, ALU.mult, ALU.add, ["t2"], ["t2"])
                    P.tt(t2_, t2_, t1, ALU.mult, ["t1", "t2"], ["t2"])
                    P.actf(t2_, t2_, AF.Sigmoid, ["t2"], ["t2"], scale=2.0 * GELU_C)
                    P.tt(G[:, c, :], t1, t2_, ALU.mult, ["t1", "t2"], ["G"])
                    P.cp(Gr_[:, c, :], G[:, c, :], ["G"], ["Gr"], eng="act")

                def cons_glu(i, pap, pk):
                    P.actf(t1, pap, AF.Sigmoid, [pk], ["t1"], bias=vp[:, 36 + i:37 + i], scale=1.0)
                    P.tt(Ybr[:, i, :], G[:, i, :], t1, ALU.mult, ["G", "t1"], ["Y"])
                proj(w_glu[l], [(128 * c, 128, Gr_[:, c, :], "Gr") for c in range(4)], [(0, 512)],
                     [(0, 128 * i, 128) for i in range(4)], cons_glu)
                P.barrier()
                branch([(128 * c, 128, Ybr[:, c, :], "Y") for c in range(4)], (0, 0), False)
                P.barrier()

                for half in range(2):
                    ca_ = Carve(abase)
                    Qb = ca_.t3(3, T); Fb = ca_.t3(3, T); Vb = ca_.t3(3, T); OGb = ca_.t3(3, T)
                    T1 = ca_.t2(T); T2 = ca_.t2(T); CM = ca_.t2(T); D3 = ca_.t2(T)
                    AB = ca_.t3(6, T)
                    VT3 = ca_.t3(3, 128, parts=64); KT3 = ca_.t3(3, 128, parts=64); SM3 = ca_.t3(3, 64, parts=64)
                    EL = ca_.t3(3, 8)

                    def cons1(i, pap, pk):
                        if i < 3:
                            P.actf(Qb[:, i, :], pap, AF.Silu, [pk], ["Qb%d" % i])
                        else:
                            P.actf(Fb[:, i - 3, :], pap, AF.Sigmoid, [pk], ["Fb%d" % (i - 3)])
                    proj(w_in[l], hk(), [(512 + 1536 * half, 768)], [(0, 128 * i, 128) for i in range(6)], cons1)

                    def cons2(i, pap, pk):
                        if i < 3:
                            P.cp(Vb[:, i, :], pap, [pk], ["Vb%d" % i], eng="act")
                        else:
                            P.actf(OGb[:, i - 3, :], pap, AF.Silu, [pk], ["OGb%d" % (i - 3)])
                    proj(w_in[l], hk(), [(512 + 1536 * half + 768, 768)], [(0, 128 * i, 128) for i in range(6)], cons2)
                    for hh in range(3):
                        h = 3 * half + hh
                        q = Qb[:, hh, :]; f = Fb[:, hh, :]
                        kq, kf = "Qb%d" % hh, "Fb%d" % hh
                        A = AB[:, 2 * hh, :]; Bm = AB[:, 2 * hh + 1, :]
                        kA, kB = "A%d" % hh, "Bm%d" % hh
                        P.ts(f, f, lbs[:, 24 + l * 6 + h:24 + l * 6 + h + 1], lbs[:, l * 6 + h:l * 6 + h + 1], ALU.mult, ALU.add, [kf, "lbs"], [kf])
                        P.actf(T1, f, AF.Ln, [kf], ["T1"])
                        P.ts(f, f, -1.0, 1.0, ALU.mult, ALU.add, [kf], [kf])
                        P.scan(T2, ones[:, 0:1].to_broadcast([128, T]), T1, 0.0, ALU.mult, ALU.add, ["T1", "cst"], ["T2"])
                        T23 = T2.rearrange("p (a b) -> p a b", b=64); CM3 = CM.rearrange("p (a b) -> p a b", b=64)
                        P.cp(CM3[:, 0, :], T23[:, 0, :], ["T2"], ["CM"])
                        P.tt(CM3[:, 1:8, :], T23[:, 1:8, :], T23[:, 0:7, 63:64].to_broadcast([128, 7, 64]), ALU.subtract, ["T2"], ["CM"])
                        lastb = CM3[:, :, 63:64].to_broadcast([128, 8, 64])
                        D33 = D3.rearrange("p (a b) -> p a b", b=64)
                        P.stt(D33, lastb, -0.5, CM3, ALU.mult, ALU.add, ["CM"], ["D3"])
                        P.actf(T1, CM, AF.Exp, ["CM"], ["T1"])
                        P.tt(A, q, T1, ALU.mult, [kq, "T1"], [kA])
                        P.actf(T1, D3, AF.Exp, ["D3"], ["T1"])
                        P.tt(q, q, T1, ALU.mult, [kq, "T1"], [kq])
                        P.actf(T2, D3, AF.Exp, ["D3"], ["T2"], scale=-1.0)
                        P.tt(Bm, f, T2, ALU.mult, [kf, "T2"], [kB])
                        P.tt(D33, lastb, CM3, ALU.subtract, ["CM"], ["D3"])
                        P.actf(T1, D3, AF.Exp, ["D3"], ["T1"])
                        P.tt(f, f, T1, ALU.mult, [kf, "T1"], [kf])
                        P.actf(EL[:, hh, :], CM3[:, :, 63], AF.Exp, ["CM"], ["EL%d" % hh])
                    OSb = [T1, T2, CM]
                    for c in range(8):
                        cols = slice(64 * c, 64 * (c + 1))
                        for stage in range(10):
                            for hh in range(3):
                                h = 3 * half + hh
                                Ash = Qb[:, hh, :]; Kd = Fb[:, hh, :]; v = Vb[:, hh, :]
                                A = AB[:, 2 * hh, :]; Bm = AB[:, 2 * hh + 1, :]
                                kq, kf, kv = "Qb%d" % hh, "Fb%d" % hh, "Vb%d" % hh
                                kA, kB = "A%d" % hh, "Bm%d" % hh
                                VT = VT3[:, hh, :]; KT = KT3[:, hh, :]; SM = SM3[:, hh, :]
                                kVT, kKT, kSM = "VT%d" % hh, "KT%d" % hh, "SM%d" % hh
                                X = ps[2 * hh]; Yp = ps[2 * hh + 1]
                                kX, kY = PK(2 * hh), PK(2 * hh + 1)
                                S = hst[:, h, :]
                                ks = "hst%d" % h
                                OS = OSb[hh]; kOS = "OS%d" % hh
                                if stage == 0:
                                    P.mm(X[0:64, 0:64], Bm[:, cols], Ash[:, cols], True, True, [kB, kq], [kX])
                                    P.tr(Yp[0:64, 0:128], v[:, cols], ident, [kv, "cst"], [kY])
                                elif stage == 1:
                                    P.tt(SM, X[0:64, 0:64], m64, ALU.mult, [kX, "cst"], [kSM])
                                    P.cp(VT, Yp[0:64, 0:128], [kY], [kVT], eng="act")
                                elif stage == 2:
                                    P.tr(Yp[0:64, 0:128], Kd[:, cols], ident, [kf, "cst"], [kY])
                                elif stage == 3:
                                    P.cp(KT, Yp[0:64, 0:128], [kY], [kKT], eng="act")
                                elif stage == 4:
                                    P.mm(X[:, 0:64], VT, SM, True, False, [kVT, kSM], [kX])
                                    P.mm(X[:, 0:64], S, A[:, cols], False, True, [ks, kA], [kX])
                                elif stage == 5:
                                    P.cp(OS[:, cols], X[:, 0:64], [kX], [kOS] + (["T1", "T2", "CM"] if c == 0 else []), eng="act")
                                elif stage == 6:
                                    P.mm(X[:, 0:128], KT, VT, True, True, [kKT, kVT], [kX])
                                elif stage == 7:
                                    P.stt(S, S, EL[:, hh, c:c + 1], X[:, 0:128], ALU.mult, ALU.add, [ks, "EL%d" % hh, kX], [ks])
                    for hh in range(3):
                        h = 3 * half + hh
                        og = OGb[:, hh, :]; ko = "OGb%d" % hh
                        OS = OSb[hh]; kOS = "OS%d" % hh
                        P.actf(D3, OS, AF.Square, [kOS], ["D3"])
                        P.mm(ps[6][:, :], ones, D3, True, True, ["D3", "cst"], [PK(6)])
                        P.actf(D3, ps[6][:, :], AF.Sqrt, [PK(6)], ["D3"], bias=epsc, scale=1.0 / 128)
                        P.add("dve", lambda e, D3=D3: e.reciprocal(D3, D3), ["D3"], ["D3"])
                        P.stt(OS, OS, vp[:, 40 + h:41 + h], D3, ALU.mult, ALU.mult, [kOS, "D3", "vp"], [kOS])
                        P.tt(Ybr[:, h, :], OS, og, ALU.mult, [kOS, ko], ["Y"])
                P.barrier()
                branch([(128 * c, 128, Ybr[:, c, :], "Y") for c in range(6)], (1, 512), False)
                P.barrier()

                ca_ = Carve(abase)
                IG = ca_.t2(T, parts=4); LFr = ca_.t2(T, parts=4); Bc = ca_.t2(T, parts=4); Rr = ca_.t2(T, parts=4)
                AC = ca_.t3(4, 4); CS = ca_.t2(4, parts=4); DEC = ca_.t2(4, parts=4); DB = ca_.t2(4, parts=96)
                CXs = ca_.t3(2, 515, parts=96); Vh = ca_.t3(2, T, parts=96); OGs = ca_.t3(2, T, parts=96)
                acc = ca_.t3(2, T, parts=96); CAr = ca_.h3(2, T, parts=96)
                Qm = ca_.t3(2, T, parts=96); Km = ca_.t3(2, T, parts=96); HS = ca_.t3(2, T, parts=96)
                DS = ca_.t2(T, parts=96); DT2 = ca_.t2(T, parts=96)
                PT4 = ca_.t3(4, 128); VT4 = ca_.t3(4, 192); KT4 = ca_.t3(4, 192); CTm = ca_.t2(384, parts=96)
                Ycr = Ybr[0:96, :, :]

                def cons_g4(i, pap, pk):
                    if i == 0:
                        P.actf(IG, pap, AF.Identity, [pk], ["IG"], bias=rc[:, 1:2], scale=1.0)
                    else:
                        P.actf(LFr, pap, AF.Sigmoid, [pk], ["LFr"], bias=rc[:, 2:3], scale=1.0)
                        P.actf(LFr, LFr, AF.Ln, ["LFr"], ["LFr"])
                proj(w_in[l], hk(), [(5888, 8)], [(0, 0, 4), (0, 4, 4)], cons_g4)
                ones4 = ones[0:4, 0:1].to_broadcast([4, T])
                P.scan(Bc, ones4, LFr, 0.0, ALU.mult, ALU.add, ["LFr", "cst"], ["Bc"])
                P.tt(IG, IG, Bc, ALU.subtract, ["IG", "Bc"], ["IG"])
                P.scan(Rr, ones4, IG, rc[:, 0:1], ALU.mult, ALU.max, ["IG", "rc", "cst"], ["Rr"])
                R3 = Rr.rearrange("p (a b) -> p a b", b=128)
                P.cp(CS[:, 0:1], rc[:, 0:1], ["rc"], ["CS"])
                P.cp(CS[:, 1:4], R3[:, 0:3, 127], ["Rr"], ["CS"])
                P.tt(DEC, CS, R3[:, :, 127], ALU.subtract, ["CS", "Rr"], ["DEC"])
                P.actf(DEC, DEC, AF.Exp, ["DEC"], ["DEC"])
                csb = CS.unsqueeze(2).to_broadcast([4, 4, 128])
                P.tt(LFr.rearrange("p (a b) -> p a b", b=128), IG.rearrange("p (a b) -> p a b", b=128), csb, ALU.subtract, ["IG", "CS"], ["LFr"])
                P.actf(LFr, LFr, AF.Exp, ["LFr"], ["LFr"])
                P.tt(IG.rearrange("p (a b) -> p a b", b=128), Bc.rearrange("p (a b) -> p a b", b=128), csb, ALU.add, ["Bc", "CS", "IG"], ["IG"])
                P.actf(IG, IG, AF.Exp, ["IG"], ["IG"], scale=-1.0)
                P.tt(rc[:, 0:1], Bc[:, T - 1:T], Rr[:, T - 1:T], ALU.add, ["Bc", "Rr"], ["rc"])
                for ch in range(4):
                    P.mm(ps[7][:, 4 * ch:4 * ch + 4], LFr[:, 128 * ch:128 * (ch + 1)], ident[0:4, 0:4], True, True, ["LFr", "cst"], [PK(7)])
                P.cp(AC.rearrange("p a b -> p (a b)"), ps[7][:, 0:16], [PK(7)], ["AC"])
                for h in range(4):
                    def cons_m(i, pap, pk, h=h):
                        j = i % 2
                        if i < 2:
                            P.cp(CXs[:, j, 3:515], pap, [pk], ["CXs%d" % j], eng="act")
                        elif i < 4:
                            P.cp(Vh[:, j, :], pap, [pk], ["Vh"], eng="act")
                        else:
                            P.actf(OGs[:, j, :], pap, AF.Sigmoid, [pk], ["OGs"])
                    proj(w_in[l], hk(), [(3584 + 576 * h, 576)], [(0, 96 * i, 96) for i in range(6)], cons_m)
                    for j in range(2):
                        fj = 2 * h + j
                        P.cp(CXs[:, j, 0:3], mlt[:, fj, :], ["mlt"], ["CXs%d" % j])
                        P.actf(acc[:, j, :], CXs[:, j, 3:515], AF.Identity, ["CXs%d" % j, "vq"], ["acc"],
                               bias=vq[:, 32 + fj:33 + fj], scale=vq[:, 24 + fj:25 + fj])
                        for k in range(3):
                            P.stt(acc[:, j, :], CXs[:, j, k:k + T], vq[:, 8 * k + fj:8 * k + fj + 1], acc[:, j, :], ALU.mult, ALU.add,
                                  ["CXs%d" % j, "vq", "acc"], ["acc"])
                        P.cp(mlt[:, fj, :], CXs[:, j, 512:515], ["CXs%d" % j], ["mlt"])
                        P.actf(CAr[:, j, :], acc[:, j, :], AF.Silu, ["acc"], ["CA"])

                    def cons_qk(i, pap, pk):
                        if i < 2:
                            P.cp(Qm[:, i, :], pap, [pk], ["Qm"], eng="act")
                        else:
                            P.ts(Km[:, i - 2, :], pap, 192.0 ** -0.5, None, ALU.mult, None, [pk], ["Km"])
                    proj(w_qk[l, h], [(0, 96, CAr[:, 0, :], "CA"), (96, 96, CAr[:, 1, :], "CA")], [(0, 384)],
                         [(0, 96 * i, 96) for i in range(4)], cons_qk)
                    P.mm(ps[7][0:96, 0:4], sel4(h), DEC, True, True, ["DEC", "cst"], [PK(7)])
                    P.cp(DB, ps[7][0:96, 0:4], [PK(7)], ["DB"])
                    kC, kN = "mlC%d" % h, "mlN%d" % h
                    for ch in range(4):
                        cols = slice(128 * ch, 128 * (ch + 1))
                        PT = PT4[:, ch, :]; VTm = VT4[:, ch, :]; KTa = KT4[:, ch, :]
                        kPT, kVT, kKT = "PT%d" % ch, "VTm%d" % ch, "KTa%d" % ch
                        P.mm(ps[0][:, 0:128], Km[:, 0, cols], Qm[:, 0, cols], True, False, ["Km", "Qm"], [PK(0)])
                        P.mm(ps[0][:, 0:128], Km[:, 1, cols], Qm[:, 1, cols], False, True, ["Km", "Qm"], [PK(0)])
                        P.stt(PT, ps[0][:, 0:128], AC[:, ch, h:h + 1], m128, ALU.mult, ALU.mult, [PK(0), "AC", "cst"], [kPT])
                        for j in range(2):
                            P.tr(ps[1][:, 96 * j:96 * (j + 1)], Vh[:, j, cols], ident[0:96, 0:96], ["Vh", "cst"], [PK(1)])
                        P.cp(VTm, ps[1][:, 0:192], [PK(1)], [kVT], eng="act")
                        for j in range(2):
                            P.tr(ps[1][:, 192 + 96 * j:192 + 96 * (j + 1)], Km[:, j, cols], ident[0:96, 0:96], ["Km", "cst"], [PK(1)])
                        P.ts(KTa, ps[1][:, 192:384], AC[:, ch, h:h + 1], None, ALU.mult, None, [PK(1), "AC"], [kKT])
                    for ch in range(4):
                        cols = slice(128 * ch, 128 * (ch + 1))
                        PT = PT4[:, ch, :]; VTm = VT4[:, ch, :]; KTa = KT4[:, ch, :]
                        kPT, kVT, kKT = "PT%d" % ch, "VTm%d" % ch, "KTa%d" % ch
                        for j in range(2):
                            pn = ps[2 + j]
                            P.mm(pn[0:96, cols], VTm[:, 96 * j:96 * (j + 1)], PT, True, False, [kVT, kPT], [PK(2 + j)])
                            P.mm(pn[0:96, cols], mlC[:, h, 0, 96 * j:96 * (j + 1)], Qm[:, 0, cols], False, False, [kC, "Qm"], [PK(2 + j)])
                            P.mm(pn[0:96, cols], mlC[:, h, 1, 96 * j:96 * (j + 1)], Qm[:, 1, cols], False, True, [kC, "Qm"], [PK(2 + j)])
                        P.mm(ps[4][0:96, cols], ones[:, 0:96], PT, True, False, ["cst", kPT], [PK(4)])
                        P.mm(ps[4][0:96, cols], mlN[:, h, 0, :], Qm[:, 0, cols], False, False, [kN, "Qm"], [PK(4)])
                        P.mm(ps[4][0:96, cols], mlN[:, h, 1, :], Qm[:, 1, cols], False, True, [kN, "Qm"], [PK(4)])
                        for kt in range(2):
                            P.mm(ps[5][0:96, 192 * kt:192 * (kt + 1)], KTa[:, 96 * kt:96 * (kt + 1)], VTm, True, True, [kKT, kVT], [PK(5)])
                            P.mm(ps[6][0:96, 96 * kt:96 * (kt + 1)], KTa[:, 96 * kt:96 * (kt + 1)], ones[:, 0:96], True, True, [kKT, "cst"], [PK(6)])
                        Cf = mlC[:, h, :, :].rearrange("p a b -> p (a b)")
                        Nf = mlN[:, h, :, :].rearrange("p a b -> p (a b)")
                        P.tt(CTm, ps[5][0:96, 0:384], Cf, ALU.add, [PK(5), kC], ["CTm"])
                        P.ts(Cf, CTm, DB[:, ch:ch + 1], None, ALU.mult, None, ["CTm", "DB"], [kC])
                        P.tt(CTm[:, 0:192], ps[6][0:96, 0:192], Nf, ALU.add, [PK(6), kN], ["CTm"])
                        P.ts(Nf, CTm[:, 0:192], DB[:, ch:ch + 1], None, ALU.mult, None, ["CTm", "DB"], [kN])
                    P.mm(ps[7][0:96, :], sel4(h), IG, True, True, ["IG", "cst"], [PK(7)])
                    P.cp(DS, ps[4][0:96, :], [PK(4)], ["DS"])
                    P.stt(DT2, DS, -1.0, DS, ALU.mult, ALU.max, ["DS"], ["DT2"])
                    P.tt(DT2, DT2, ps[7][0:96, :], ALU.max, ["DT2", PK(7)], ["DT2"])
                    P.add("dve", lambda e, DT2=DT2: e.reciprocal(DT2, DT2), ["DT2"], ["DT2"])
                    for j in range(2):
                        P.tt(HS[:, j, :], ps[2 + j][0:96, :], DT2, ALU.mult, [PK(2 + j), "DT2"], ["HS"])
                        P.actf(acc[:, j, :], HS[:, j, :], AF.Square, ["HS"], ["acc"])
                        P.mm(ps[7][0:96, :], ones[0:96, 0:96], acc[:, j, :], j == 0, j == 1, ["acc", "cst"], [PK(7)])
                    P.actf(DS, ps[7][0:96, :], AF.Sqrt, [PK(7)], ["DS"], bias=epsc[0:96, :], scale=1.0 / 192)
                    P.add("dve", lambda e, DS=DS: e.reciprocal(DS, DS), ["DS"], ["DS"])
                    for j in range(2):
                        fj = 2 * h + j
                        P.stt(HS[:, j, :], HS[:, j, :], vq[:, 40 + fj:41 + fj], DS, ALU.mult, ALU.mult, ["HS", "DS", "vq"], ["HS"])
                        P.tt(Ycr[:, fj, :], HS[:, j, :], OGs[:, j, :], ALU.mult, ["HS", "OGs"], ["Y"])
                P.barrier()
                branch([(96 * i, 96, Ycr[:, i, :], "Y") for i in range(8)], (2, 1280), True)

                for g0, ng in ((0, 6), (6, 6), (12, 4)):
                    def cons_o(i, pap, pk, g0=g0):
                        j = g0 + i
                        P.tt(xt[:, j, :], xt[:, j, :], pap, ALU.add, ["xt%d" % j, pk], ["xt%d" % j])
                    proj(w_out[l], [(128 * c, 128, mergedr[:, c, :], "mgr%d" % c) for c in range(NCH)],
                         [(128 * g0, 128 * ng)], [(0, 128 * i, 128) for i in range(ng)], cons_o)
                P.barrier()

                cv = Carve()
                sq2 = cv.t3(2, T); rs = cv.t2(T)
                rmsnorm(htr, lambda c: vp[:, 16 + c:17 + c], sq2, rs, "ht")
                ringB = cv.h3(6, D)
                actgr = cv.h3(12, T)
                SA = cv.t3(6, T)
                STG = cv.t3(12, 514); accB2 = cv.t3(2, T)

                def evac_to(base):
                    def f(i, pap, pk):
                        P.cp(STG[:, base + i, 2:514], pap, [pk], ["STG%d" % (base + i)], eng=("act" if i % 2 else "dve"))
                    return f

                def conv(slot, cidx, dst, kd):
                    st = STG[:, slot, :]
                    kst = "STG%d" % slot
                    P.cp(st[:, 0:2], ftl[:, cidx, :], ["ftl%d" % cidx], [kst])
                    P.actf(dst, st[:, 2:514], AF.Identity, [kst, "vp"], [kd],
                           bias=vp[:, 46 + cidx:47 + cidx], scale=vp[:, 134 + 88 * 2 + cidx:135 + 88 * 2 + cidx])
                    for k in range(2):
                        P.stt(dst, st[:, k:k + T], vp[:, 134 + 88 * k + cidx:135 + 88 * k + cidx], dst, ALU.mult, ALU.add, [kst, "vp", kd], [kd])
                    P.cp(ftl[:, cidx, :], st[:, 512:514], [kst], ["ftl%d" % cidx])

                def emit_down(g0, ng, ab):
                    for ii in range(ng):
                        P.dma("pool", ringB[:, ii, :], w_dn[l, 128 * (g0 + ii):128 * (g0 + ii + 1), :], (), ["ringB%d" % ii])
                    for j in range(NCH):
                        b = 6 + (j % 2)
                        for ii in range(ng):
                            P.mm(ps[b][:, :], ringB[:, ii, 128 * j:128 * (j + 1)], actgr[:, ab + ii, :], ii == 0, ii == ng - 1,
                                 ["ringB%d" % ii, "actg%d" % (ab + ii)], [PK(b)])
                        P.tt(xt[:, j, :], xt[:, j, :], ps[b][:, :], ALU.add, ["xt%d" % j, PK(b)], ["xt%d" % j])

                pend = None
                gi = 0
                g0 = 0
                while g0 < 44:
                    ng = min(6, 44 - g0)
                    ab = (gi % 2) * 6
                    tl = [(0, 128 * ii, 128) for ii in range(ng)]
                    proj(w_up[l], hk(), [(128 * g0, 128 * ng)], tl, evac_to(0))
                    for i in range(ng):
                        conv(i, g0 + i, SA[:, i, :], "SA%d" % i)
                        P.actf(SA[:, i, :], SA[:, i, :], AF.Silu, ["SA%d" % i], ["SA%d" % i])
                    proj(w_up[l], hk(), [(FFN + 128 * g0, 128 * ng)], tl, evac_to(6))
                    if pend is not None:
                        emit_down(*pend)
                    for i in range(ng):
                        ab_ = accB2[:, i % 2, :]
                        conv(6 + i, 44 + g0 + i, ab_, "accB%d" % (i % 2))
                        P.tt(actgr[:, ab + i, :], SA[:, i, :], ab_, ALU.mult, ["SA%d" % i, "accB%d" % (i % 2)], ["actg%d" % (ab + i)])
                    pend = (g0, ng, ab)
                    g0 += ng
                    gi += 1
                emit_down(*pend)
                P.barrier()

                if l < NL - 1:
                    P.dma("sp", xs_d[t], xt[:].rearrange("p a b -> p (a b)"), ["xt%d" % c for c in range(NCH)], ["xs%d" % t])
                else:
                    cv = Carve()
                    sq2 = cv.t3(2, T); rs = cv.t2(T)
                    xo = cv.t2(D)
                    hf = cv.t3(NCH, T)
                    rmsnorm(hf, lambda c: vp[:, 398 + c:399 + c], sq2, rs, "hf")
                    for tb in range(4):
                        for c in range(NCH):
                            P.tr(ps[c % 8][:, 0:128], hf[:, c, tb * 128:(tb + 1) * 128], ident, ["hf", "cst"], [PK(c % 8)])
                            P.cp(xo[:, c * 128:(c + 1) * 128], ps[c % 8][:, 0:128], [PK(c % 8)], ["xo"], eng=("act" if c % 2 else "dve"))
                        P.dma("sp", y_d[tok0 + tb * 128:tok0 + (tb + 1) * 128, :], xo, ["xo"], ["y"])
                P.barrier()
        P.emit()
    return nc


WNAMES = ["mix_norm", "w_in", "s5_lam_re", "s5_lam_im", "s5_log_dt", "s5_b_re", "s5_b_im", "s5_c_re", "s5_c_im",
          "s5_d", "s5_w_glu", "s5_b_glu", "hg_lower_bounds", "hg_norm", "ml_conv_w", "ml_conv_b", "ml_w_qk",
          "ml_b_ig", "ml_b_fg", "ml_norm", "w_branch", "w_out", "ffn_norm", "ffn_w_up", "ffn_conv_w", "ffn_conv_b",
          "ffn_w_down", "final_norm"]


def w_in_perm():
    idx = list(range(512))
    for r in range(2):
        for base in (512, 1280, 2048, 2816):
            idx += list(range(base + 384 * r, base + 384 * (r + 1)))
    for h in range(4):
        for base in (3584, 4352, 5120):
            idx += list(range(base + 192 * h, base + 192 * (h + 1)))
    idx += list(range(5888, IN_TOTAL))
    assert len(idx) == IN_TOTAL and sorted(idx) == list(range(IN_TOTAL))
    return np.asarray(idx, np.int64)


def prep_shared(inputs):
    shared = {k: np.ascontiguousarray(np.asarray(inputs[k], np.float32)) for k in WNAMES}
    shared["w_in"] = np.ascontiguousarray(np.take(shared["w_in"], w_in_perm(), axis=2))
    return shared


def run(inputs, NL, NT, ncores):
    nc = build(NL, NT)
    x = np.asarray(inputs["x"], np.float32)
    B = x.shape[0]
    cstv = make_consts()
    shared = prep_shared(inputs)
    in_maps = []
    for c in range(ncores):
        m = dict(shared)
        m["x"] = np.ascontiguousarray(x[c % B])
        m["cst"] = cstv
        in_maps.append(m)
    res = run_bass_kernel_spmd(nc, in_maps, core_ids=list(range(ncores)))
    return np.stack([res.results[b]["y"] for b in range(B)], axis=0).astype(np.float32)


def kernel(**inputs):
    return run(inputs, 4, 8, 8)
```
